# Optimizing a Trainium2 kernel written in Bass

```python
import math
import jax, jax.numpy as jnp
from jax import lax
import numpy as np

D_MODEL = 1024
BATCH = 2
SEQ = 8192
DEPTH = 1

N_HEADS = 8
HEAD_DIM = 64
ATTN_WIDTH = N_HEADS * HEAD_DIM
IDX_HEADS = 8
IDX_DIM = 64
TOPK_MAX = 256
Q_BLOCK = 128
N_BUCKETS = 32
MAX_DISTANCE = 128
CONV_WIDTH = 512
CONV_K = 3
N_BRANCH = 2
N_GROUPS = 4
EXPERTS_PER_GROUP = 8
N_EXPERTS = N_GROUPS * EXPERTS_PER_GROUP
TOP_K_EXPERTS = 2
D_EXPERT = 512
EPS = 1e-6

SPLITS = (ATTN_WIDTH, ATTN_WIDTH, ATTN_WIDTH,
          IDX_HEADS * IDX_DIM, IDX_DIM, IDX_HEADS,
          CONV_WIDTH, CONV_WIDTH, CONV_WIDTH,
          N_BRANCH * D_MODEL)
IN_COLS = sum(SPLITS)

kernel_name = "hybrid_dsa_shortconv_hmoe_block"


def rms_norm(x, g):
    xf = x.astype(jnp.float32)
    y = xf * lax.rsqrt(jnp.mean(xf * xf, axis=-1, keepdims=True) + EPS)
    return y * g.astype(jnp.float32)


def t5_bucket(rel):
    n = jnp.maximum(rel, 0)
    max_exact = N_BUCKETS // 2
    nf = jnp.maximum(n, 1).astype(jnp.float32)
    large = max_exact + (jnp.log(nf / max_exact) / math.log(MAX_DISTANCE / max_exact)
                         * (N_BUCKETS - max_exact)).astype(jnp.int32)
    large = jnp.minimum(large, N_BUCKETS - 1)
    return jnp.where(n < max_exact, n, large)


def dsa_attention(q, k, v, q_idx, k_idx, w_idx, rel_bias):
    B, S, H, Dh = q.shape
    K = min(TOPK_MAX, S // 4)
    nb = S // Q_BLOCK
    key_pos = jnp.arange(S)
    k_idx_f = k_idx.astype(jnp.float32)
    neg = jnp.finfo(jnp.float32).min

    def to_blocks(a):
        return a.reshape(B, nb, Q_BLOCK, *a.shape[2:]).swapaxes(0, 1)

    def block(args):
        qb, qib, wb, start = args
        qpos = start + jnp.arange(Q_BLOCK)
        dots = jnp.einsum('bqhd,bsd->bqhs', qib.astype(jnp.float32), k_idx_f) * (IDX_DIM ** -0.5)
        score = jnp.einsum('bqh,bqhs->bqs', wb.astype(jnp.float32), jax.nn.relu(dots)) * (IDX_HEADS ** -0.5)
        causal = key_pos[None, :] <= qpos[:, None]
        score = jnp.where(causal[None], score, neg)
        _, idx = lax.top_k(score, K)
        gather = jax.vmap(lambda kb, ib: kb[ib])
        kg = gather(k, idx).astype(jnp.float32)
        vg = gather(v, idx).astype(jnp.float32)
        logits = jnp.einsum('bqhd,bqkhd->bqhk', qb.astype(jnp.float32), kg) * (Dh ** -0.5)
        rel = qpos[None, :, None] - idx
        bias = rel_bias.astype(jnp.float32)[t5_bucket(rel)]
        logits = logits + bias.transpose(0, 1, 3, 2)
        logits = jnp.where((rel >= 0)[:, :, None, :], logits, -jnp.inf)
        p = jax.nn.softmax(logits, axis=-1)
        return jnp.einsum('bqhk,bqkhd->bqhd', p, vg)

    outs = lax.map(block, (to_blocks(q), to_blocks(q_idx), to_blocks(w_idx),
                           jnp.arange(nb, dtype=jnp.int32) * Q_BLOCK))
    return outs.swapaxes(0, 1).reshape(B, S, H * Dh)


def causal_short_conv(u, w):
    S = u.shape[1]
    up = jnp.pad(u, ((0, 0), (CONV_K - 1, 0), (0, 0)))
    return sum(w[j] * up[:, j:j + S] for j in range(CONV_K))


def hier_moe(h, wg, bg, we, be, w_gate, w_up, w_down):
    B, S, D = h.shape
    t = h.reshape(-1, D)
    T = t.shape[0]
    g_logits = t @ wg.astype(jnp.float32) + bg.astype(jnp.float32)
    g_prob = jax.nn.softmax(g_logits, axis=-1)
    g_sel = jnp.argmax(g_logits, axis=-1)
    g_w = jnp.take_along_axis(g_prob, g_sel[:, None], axis=1)[:, 0]
    e_logits = (t @ we.astype(jnp.float32) + be.astype(jnp.float32)).reshape(T, N_GROUPS, EXPERTS_PER_GROUP)
    e_sel_logits = e_logits[jnp.arange(T), g_sel]
    e_prob = jax.nn.softmax(e_sel_logits, axis=-1)
    top_p, top_i = lax.top_k(e_prob, TOP_K_EXPERTS)
    top_p = top_p / jnp.sum(top_p, axis=-1, keepdims=True)
    expert_id = g_sel[:, None] * EXPERTS_PER_GROUP + top_i
    weights = g_w[:, None] * top_p
    gate_dense = jnp.einsum('tk,tke->te', weights, jax.nn.one_hot(expert_id, N_EXPERTS, dtype=jnp.float32))

    def expert_step(acc, params):
        wg_e, wu_e, wd_e, gcol = params
        hid = jax.nn.silu(t @ wg_e.astype(jnp.float32)) * (t @ wu_e.astype(jnp.float32))
        return acc + gcol[:, None] * (hid @ wd_e.astype(jnp.float32)), None

    acc, _ = lax.scan(expert_step, jnp.zeros((T, D), jnp.float32),
                      (w_gate, w_up, w_down, gate_dense.T))
    return acc.reshape(B, S, D)


def setup_inputs(seed: int = 0) -> dict:
    key = jax.random.key(seed)
    ks = jax.random.split(key, 24)
    f32 = jnp.float32
    D = D_MODEL
    nrm = lambda k, shape, s: jax.random.normal(k, shape, f32) * s
    return {
        "x": nrm(ks[0], (BATCH, SEQ, D), 1.0),
        "c": nrm(ks[1], (BATCH, D), 1.0),
        "w_ada": nrm(ks[2], (DEPTH, D, 6 * D), 0.5 * D ** -0.5),
        "b_ada": nrm(ks[3], (DEPTH, 6 * D), 0.01),
        "norm1_g": 1.0 + nrm(ks[4], (DEPTH, D), 0.01),
        "w_in": nrm(ks[5], (DEPTH, D, IN_COLS), D ** -0.5),
        "rel_bias": nrm(ks[6], (N_BUCKETS, N_HEADS), 0.5),
        "conv_w": nrm(ks[7], (DEPTH, CONV_K, CONV_WIDTH), CONV_K ** -0.5),
        "w_attn_branch": nrm(ks[8], (DEPTH, ATTN_WIDTH, D), ATTN_WIDTH ** -0.5),
        "w_conv_branch": nrm(ks[9], (DEPTH, CONV_WIDTH, D), CONV_WIDTH ** -0.5),
        "w_out": nrm(ks[10], (DEPTH, D, D), D ** -0.5),
        "norm2_g": 1.0 + nrm(ks[11], (DEPTH, D), 0.01),
        "w_router_group": nrm(ks[12], (DEPTH, D, N_GROUPS), D ** -0.5),
        "b_router_group": nrm(ks[13], (DEPTH, N_GROUPS), 0.01),
        "w_router_expert": nrm(ks[14], (DEPTH, D, N_EXPERTS), D ** -0.5),
        "b_router_expert": nrm(ks[15], (DEPTH, N_EXPERTS), 0.01),
        "w_gate_e": nrm(ks[16], (DEPTH, N_EXPERTS, D, D_EXPERT), D ** -0.5),
        "w_up_e": nrm(ks[17], (DEPTH, N_EXPERTS, D, D_EXPERT), D ** -0.5),
        "w_down_e": nrm(ks[18], (DEPTH, N_EXPERTS, D_EXPERT, D), D_EXPERT ** -0.5),
        "norm_f_g": 1.0 + nrm(ks[19], (D,), 0.01),
    }


def reference(x, c, w_ada, b_ada, norm1_g, w_in, rel_bias, conv_w, w_attn_branch, w_conv_branch,
              w_out, norm2_g, w_router_group, b_router_group, w_router_expert, b_router_expert,
              w_gate_e, w_up_e, w_down_e, norm_f_g):
    B, S, D = x.shape
    offsets = np.cumsum(SPLITS)[:-1].tolist()
    h_res = x.astype(jnp.float32)
    c_act = jax.nn.silu(c.astype(jnp.float32))
    for l in range(DEPTH):
        mod = c_act @ w_ada[l].astype(jnp.float32) + b_ada[l].astype(jnp.float32)
        shift1, scale1, gate1, shift2, scale2, gate2 = [m[:, None, :] for m in jnp.split(mod, 6, axis=-1)]

        h = rms_norm(h_res, norm1_g[l]) * (1.0 + scale1) + shift1
        proj = h @ w_in[l].astype(jnp.float32)
        q, k, v, qi, ki, wi, cb, cc, cx, gl = jnp.split(proj, offsets, axis=-1)
        attn = dsa_attention(q.reshape(B, S, N_HEADS, HEAD_DIM), k.reshape(B, S, N_HEADS, HEAD_DIM),
                             v.reshape(B, S, N_HEADS, HEAD_DIM), qi.reshape(B, S, IDX_HEADS, IDX_DIM),
                             ki, wi, rel_bias)
        y_attn = attn @ w_attn_branch[l].astype(jnp.float32)
        conv = causal_short_conv(cc * cx, conv_w[l].astype(jnp.float32))
        y_conv = (cb * conv) @ w_conv_branch[l].astype(jnp.float32)
        gates = jax.nn.sigmoid(gl).reshape(B, S, N_BRANCH, D)
        merged = gates[:, :, 0] * y_attn + gates[:, :, 1] * y_conv
        h_res = h_res + gate1 * (merged @ w_out[l].astype(jnp.float32))

        h2 = rms_norm(h_res, norm2_g[l]) * (1.0 + scale2) + shift2
        h_res = h_res + gate2 * hier_moe(h2, w_router_group[l], b_router_group[l], w_router_expert[l],
                                         b_router_expert[l], w_gate_e[l], w_up_e[l], w_down_e[l])
    return rms_norm(h_res, norm_f_g).astype(x.dtype)
```

```python
from contextlib import ExitStack
import numpy as np
import concourse.bass as bass
import concourse.mybir as mybir
from concourse.bass_utils import run_bass_kernel_spmd

F32 = mybir.dt.float32
BF16 = mybir.dt.bfloat16
ALU = mybir.AluOpType
AF = mybir.ActivationFunctionType
AX = mybir.AxisListType

D = 1024
S = 8192
NT = S // 128
NSLOT = 16
IN_COLS = 5704
C_Q, C_K, C_V, C_QI, C_KI, C_WI, C_CB, C_CC, C_CX, C_GL = 0, 512, 1024, 1536, 2048, 2112, 2120, 2632, 3144, 3656
EPS = 1e-6
NEG = -1.0e30
NIT = 18
TABW = 5 * 256
SEM_ROT = 30000


class Res:
    __slots__ = ("w", "r", "name")

    def __init__(self, name=""):
        self.w = None
        self.r = {}
        self.name = name


class Slot:
    def __init__(self, kb, name):
        self.kb = kb
        self.name = name
        self.sem = kb.nc.alloc_semaphore(name)
        self.val = 0
        kb.slots.append(self)

    def bump(self):
        if self.val + 16 > SEM_ROT:
            self.sem = self.kb.nc.alloc_semaphore(self.name + "_r%d" % self.kb.uid())
            self.val = 0
        self.val += 16
        return self.sem, self.val


class KB:
    def __init__(self, nc):
        self.nc = nc
        self.eng = {"pe": nc.tensor, "act": nc.scalar, "dve": nc.vector, "pool": nc.gpsimd, "sp": nc.sync}
        self._uid = 0
        self.sem = {k: nc.alloc_semaphore("s_" + k) for k in self.eng}
        self.cnt = {k: 0 for k in self.eng}
        self.seen = {k: {} for k in self.eng}
        self.nins = 0
        self.slots = []
        self.pool = {}
        self.pool_i = {}

    def barrier(self):
        evs = [(self.sem[k], self.cnt[k], k) for k in self.eng if self.cnt[k] > 0]
        evs += [(sl.sem, sl.val, "dma") for sl in self.slots if sl.val > 0]
        for e in self.eng:
            for sem, val, src in evs:
                if self.seen[e].get(sem.num, 0) >= val:
                    continue
                self.eng[e].wait_ge(sem, val)
                self.seen[e][sem.num] = val

    def uid(self):
        self._uid += 1
        return self._uid

    def _wait(self, e, ev):
        sem, val, src = ev
        if src == "pe" and e == "pe":
            return
        if self.seen[e].get(sem.num, 0) >= val:
            return
        self.eng[e].wait_ge(sem, val)
        self.seen[e][sem.num] = val

    def deps(self, e, reads, writes):
        for r in reads:
            if r.w is not None:
                self._wait(e, r.w)
        for w in writes:
            if w.w is not None:
                self._wait(e, w.w)
            for ev in w.r.values():
                self._wait(e, ev)

    def _record(self, ev, reads, writes):
        for r in reads:
            r.r[ev[0].num] = ev
        for w in writes:
            w.w = ev
            w.r = {}

    def op(self, e, fn, reads=(), writes=()):
        self.deps(e, reads, writes)
        ins = fn(self.eng[e])
        if self.cnt[e] + 1 > SEM_ROT:
            self.sem[e] = self.nc.alloc_semaphore("s_%s_r%d" % (e, self.uid()))
            self.cnt[e] = 0
        self.cnt[e] += 1
        ins.then_inc(self.sem[e], 1)
        self.nins += 1
        ev = (self.sem[e], self.cnt[e], e)
        self._record(ev, reads, writes)
        return ev

    def dma(self, q, out, in_, slot, reads=(), writes=(), **kw):
        self.deps(q, reads, writes)
        if slot is None:
            if q not in self.pool:
                self.pool[q] = [Slot(self, "dq_%s%d" % (q, k)) for k in range(16)]
                self.pool_i[q] = 0
            slot = self.pool[q][self.pool_i[q] % 16]
            self.pool_i[q] += 1
            if slot.val > 0:
                self._wait(q, (slot.sem, slot.val, "dma"))
        ins = self.eng[q].dma_start(out=out, in_=in_, **kw)
        sem, val = slot.bump()
        ins.then_inc(sem, 16)
        self.nins += 1
        ev = (sem, val, "dma")
        self._record(ev, reads, writes)
        return ev

    def slot(self, name):
        return None

    def wait_all(self, e, resources):
        for r in resources:
            if r.w is not None:
                self._wait(e, r.w)
            for ev in r.r.values():
                self._wait(e, ev)


class Ring:
    def __init__(self, kb, name, n, make):
        self.items = []
        for i in range(n):
            self.items.append((make("%s%d" % (name, i)), Res("%s%d" % (name, i)), LazySlot(kb, "dq_%s%d" % (name, i))))
        self.i = 0

    def next(self):
        it = self.items[self.i % len(self.items)]
        self.i += 1
        return it


class LazySlot:
    def __init__(self, kb, name):
        self.kb = kb
        self.name = name
        self.s = None

    def bump(self):
        if self.s is None:
            self.s = Slot(self.kb, self.name)
        return self.s.bump()


def build_nc(stop=None):
    nc = bass.Bass("TRN2", target_bir_lowering=False)
    kb = KB(nc)
    dbg_row = [0]

    class Scope(ExitStack):
        def __exit__(self, *a):
            r = super().__exit__(*a)
            if a[0] is None:
                kb.barrier()
            return r

    def finish(items):
        sl = kb.slot("dq_dbg")
        rs = []
        for ap, res, p, n in items:
            r0 = dbg_row[0]
            kb.dma("pool", y_d.ap()[r0:r0 + p, 0:n], ap, sl, reads=[res])
            dbg_row[0] += 128
            rs.append(res)
        kb.wait_all("pool", rs)
        return nc

    def din(name, shape):
        return nc.dram_tensor(name, list(shape), F32, kind="ExternalInput")

    xb_d = din("xb", [S, D])
    xo_d = din("xo", [NSLOT, 130, D])
    c8_d = din("c8", [128, 8])
    wada_d = din("w_ada", [D, 6 * D])
    badaT_d = din("b_adaT", [128, 48])
    bada_d = din("b_ada", [1, 6 * D])
    g1T_d = din("g1T", [128, 8])
    g2T_d = din("g2T", [128, 8])
    gf_d = din("gf", [1, D])
    win_d = din("w_in", [D, IN_COLS])
    relb_d = din("relb", [32, 8])
    ej_d = din("ej", [32, TABW])
    convw_d = din("convw", [128, 4, 3])
    wab_d = din("w_ab", [512, D])
    wcbr_d = din("w_cbr", [512, D])
    wout_d = din("w_out", [D, D])
    wr_d = din("wr", [D, 36])
    br_d = din("br", [1, 36])
    wge_d = din("wge", [32, D, 512])
    wue_d = din("wue", [32, D, 512])
    wde_d = din("wde", [32, 512, D])
    tq_d = din("tq", [128, 1])
    hmask_d = din("hmask", [128, NSLOT])
    y_d = nc.dram_tensor("y", [NSLOT * 128, D], F32, kind="ExternalOutput")
    scr_tab = nc.dram_tensor("scr_tab", [8, TABW], F32, kind="Internal")
    scr_q = nc.dram_tensor("scr_q", [NSLOT, 128, 4 * 128], BF16, kind="Internal")
    scr_qi = nc.dram_tensor("scr_qi", [NSLOT, 128, 4 * 128], BF16, kind="Internal")
    scr_o = nc.dram_tensor("scr_o", [NSLOT, 128, 512], BF16, kind="Internal")
    r_scr_tab, r_scr_q, r_scr_qi, r_scr_o = Res(), Res(), Res(), Res()

    win_v = win_d.ap().rearrange("(k p) n -> p k n", p=128)

    PB = [nc.alloc_psum_tensor("pb%d" % i, [128, 512], F32) for i in range(8)]
    rPB = [Res("pb%d" % i) for i in range(8)]

    def pbf(i):
        return PB[i][:].bitcast(BF16)

    es_all = ExitStack()

    def SB(es, name, shape, dt):
        return es.enter_context(nc.sbuf_tensor("sb_" + name, list(shape), dt))

    ID32 = SB(es_all, "ID32", [128, 128], F32)
    ID = SB(es_all, "ID", [128, 128], BF16)
    JJ = SB(es_all, "JJ", [128, 128], F32)
    JK = SB(es_all, "JK", [128, 8], F32)
    junk_ap = JK[:].ap
    rJK = Res("junk")

    def junk(p, n, col=0):
        return bass.AP(JK, col, [[junk_ap[0][0], p], [0, n]])

    modT = SB(es_all, "modT", [128, 48], F32)
    s1 = SB(es_all, "s1", [128, 8], F32)
    s2 = SB(es_all, "s2", [128, 8], F32)
    cact = SB(es_all, "cact", [128, 8], F32)
    epsb = SB(es_all, "epsb", [128, 1], F32)
    tq = SB(es_all, "tq", [128, 1], F32)
    hmask = SB(es_all, "hmask", [128, NSLOT], F32)
    wi_all = SB(es_all, "wi_all", [128, NSLOT, 8], F32)
    sg_all = SB(es_all, "sg_all", [128, NSLOT, 8], F32)
    rC = Res("consts")
    rMOD = Res("mod")
    cs = kb.slot("dq_const")

    kb.op("pool", lambda e: e.memset(ID32[:], 0.0), writes=[rC])
    kb.op("pool", lambda e: e.affine_select(out=ID32[:], in_=ID32[:], compare_op=ALU.not_equal, fill=1.0, base=0,
                                            pattern=[[-1, 128]], channel_multiplier=1), reads=[rC], writes=[rC])
    kb.op("pool", lambda e: e.memset(JJ[:], 0.0), reads=[rC], writes=[rC])
    kb.op("pool", lambda e: e.affine_select(out=JJ[:], in_=JJ[:], compare_op=ALU.not_equal, fill=1.0, base=-127,
                                            pattern=[[1, 128]], channel_multiplier=1), reads=[rC], writes=[rC])
    kb.op("dve", lambda e: e.tensor_copy(out=ID[:], in_=ID32[:]), reads=[rC], writes=[rC])
    kb.op("pool", lambda e: e.memset(epsb[:], EPS), reads=[rC], writes=[rC])
    kb.dma("sp", tq[:], tq_d.ap(), cs, writes=[rC])
    kb.dma("sp", hmask[:], hmask_d.ap(), cs, writes=[rC])
    kb.dma("sp", cact[:], c8_d.ap(), cs, writes=[rC])
    kb.dma("sp", modT[:], badaT_d.ap(), cs, writes=[rMOD])
    kb.dma("sp", s1[:], g1T_d.ap(), cs, writes=[rMOD])
    kb.dma("sp", s2[:], g2T_d.ap(), cs, writes=[rMOD])
    kb.op("act", lambda e: e.activation(out=cact[:], in_=cact[:], func=AF.Silu), reads=[rC], writes=[rC])

    if stop == "c0":
        return finish([(cact[:], rC, 128, 8), (ID32[:], rC, 128, 128), (JJ[:], rC, 128, 128), (modT[:], rMOD, 128, 48)])
    wada_v = wada_d.ap().rearrange("(k p) n -> k p n", p=128)

    def gate_bc(gb, rgb, part):
        kb.barrier()
        with Scope() as es:
            wa = Ring(kb, "wg%d" % part, 2, lambda n: SB(es, n, [128, D], F32))
            cbr = Ring(kb, "cb%d" % part, 2, lambda n: SB(es, n, [128, 128], F32))
            kb.dma("sp", gb[:], bada_d.ap()[0:1, part * D:(part + 1) * D].partition_broadcast(128), cs, writes=[rgb])
            for k in range(8):
                wt, rw, sl = wa.next()
                kb.dma("sp", wt[:], wada_v[k][:, part * D:(part + 1) * D], sl, writes=[rw])
                cbk, rcbk, _ = cbr.next()
                kb.op("dve", lambda e: e.tensor_copy(out=cbk[:], in_=cact[:, k:k + 1].to_broadcast([128, 128])), reads=[rC], writes=[rcbk])
                for n in range(2):
                    kb.op("pe", lambda e: e.matmul(PB[n][:], lhsT=cbk[:], rhs=wt[:, n * 512:(n + 1) * 512], start=(k == 0), stop=(k == 7)),
                          reads=[rw, rcbk], writes=[rPB[n]])
            for n in range(2):
                kb.op("dve", lambda e: e.tensor_tensor(out=gb[:, n * 512:(n + 1) * 512], in0=gb[:, n * 512:(n + 1) * 512], in1=PB[n][:], op=ALU.add),
                      reads=[rPB[n], rgb], writes=[rgb])

    kb.barrier()
    with Scope() as es:
        gtmp = SB(es, "gtmp", [128, D], F32)
        dtmp = SB(es, "dtmp", [128, 8, 128], F32)
        rgt = Res("gtmp")
        for part in (0, 1, 3, 4):
            gate_bc(gtmp, rgt, part)
            kb.op("dve", lambda e: e.tensor_tensor(out=dtmp[:], in0=gtmp[:].rearrange("p (c q) -> p c q", c=8),
                                                   in1=ID32[:].unsqueeze(1).to_broadcast([128, 8, 128]), op=ALU.mult), reads=[rgt, rC], writes=[rgt])
            kb.op("dve", lambda e: e.tensor_reduce(out=modT[:, part * 8:(part + 1) * 8], in_=dtmp[:], axis=AX.X, op=ALU.add), reads=[rgt], writes=[rMOD])
        kb.op("dve", lambda e: e.scalar_tensor_tensor(out=s1[:], in0=modT[:, 8:16], scalar=1.0, in1=s1[:], op0=ALU.add, op1=ALU.mult),
              reads=[rMOD], writes=[rMOD])
        kb.op("dve", lambda e: e.scalar_tensor_tensor(out=s2[:], in0=modT[:, 32:40], scalar=1.0, in1=s2[:], op0=ALU.add, op1=ALU.mult),
              reads=[rMOD], writes=[rMOD])
    if stop == "p0":
        return finish([(modT[:], rMOD, 128, 48), (s1[:], rMOD, 128, 8), (s2[:], rMOD, 128, 8), (cact[:], rC, 128, 8)])
    sh1 = modT[:, 0:8]
    sh2 = modT[:, 24:32]

    def norm_stats(X, rX, p, xn, rxn, st, rst, on_pool=False):
        kb.op("act", lambda e: e.activation(out=xn[0:p, :], in_=X, func=AF.Square, accum_out=st[0:p, 0:1]),
              reads=[rX], writes=[rst, rxn])
        kb.op("act", lambda e: e.activation(out=st[0:p, 1:2], in_=st[0:p, 0:1], func=AF.Ln, scale=1.0 / D, bias=epsb[0:p, 0:1]), reads=[rst, rC], writes=[rst])
        kb.op("act", lambda e: e.activation(out=st[0:p, 2:3], in_=st[0:p, 1:2], func=AF.Exp, scale=-0.5), reads=[rst], writes=[rst])
        kb.op("dve", lambda e: e.tensor_scalar(out=xn[0:p, :], in0=X, scalar1=st[0:p, 2:3], scalar2=None, op0=ALU.mult),
              reads=[rX, rst], writes=[rxn])

    def norm_T(X, rX, p, xn, rxn, st, rst, hT_out, rhT, sc, sh, pbank, on_pool=False):
        norm_stats(X, rX, p, xn, rxn, st, rst, on_pool)
        norm_tr(xn, rxn, p, hT_out, rhT, sc, sh, pbank)

    def norm_tr(xn, rxn, p, hT_out, rhT, sc, sh, pbank):
        for c in range(8):
            if p == 128:
                kb.op("pe", lambda e: e.transpose(out=pbf(pbank)[:, c * 128:(c + 1) * 128], in_=xn[:, c * 128:(c + 1) * 128], identity=ID[:]),
                      reads=[rxn, rC], writes=[rPB[pbank]])
        for c in range(8):
            src = pbf(pbank)[:, c * 128:(c + 1) * 128]
            if c % 2 == 0:
                kb.op("act", lambda e: e.activation(out=hT_out(c), in_=src, func=AF.Identity, scale=sc[:, c:c + 1], bias=sh[:, c:c + 1]),
                      reads=[rPB[pbank], rMOD], writes=[rhT])
            else:
                kb.op("dve", lambda e: e.tensor_scalar(out=hT_out(c), in0=src, scalar1=sc[:, c:c + 1], scalar2=sh[:, c:c + 1], op0=ALU.mult, op1=ALU.add),
                      reads=[rPB[pbank], rMOD], writes=[rhT])

    TAB = SB(es_all, "TAB", [128, 5, 8, 128], BF16)
    rTAB = Res("TAB")
    PEN = SB(es_all, "PEN", [128, 512], BF16)
    kb.barrier()
    with Scope() as es2:
        relb = SB(es2, "relb", [32, 8], F32)
        ej = SB(es2, "ej", [32, TABW], F32)
        rtab = SB(es2, "rtab", [8, TABW], F32)
        XZ = SB(es2, "XZ", [128, 8, 128], F32)
        rT0, rXZ = Res(), Res()
        ts_ = kb.slot("dq_tab")
        kb.dma("sp", relb[:], relb_d.ap(), ts_, writes=[rT0])
        kb.dma("sp", ej[:], ej_d.ap(), ts_, writes=[rT0])
        for c0 in range(0, TABW, 512):
            w = min(512, TABW - c0)
            kb.op("pe", lambda e: e.matmul(PB[0][0:8, 0:w], lhsT=relb[:], rhs=ej[:, c0:c0 + w], start=True, stop=True),
                  reads=[rT0], writes=[rPB[0]])
            kb.op("act", lambda e: e.activation(out=rtab[:, c0:c0 + w], in_=PB[0][0:8, 0:w], func=AF.Exp), reads=[rPB[0]], writes=[rT0])
        kb.dma("sp", scr_tab.ap(), rtab[:], ts_, reads=[rT0], writes=[r_scr_tab])
        for zi in range(5):
            toe = bass.AP(scr_tab, zi * 256, [[1, 128], [TABW, 8], [1, 128]])
            kb.dma("sp", XZ[:], toe, ts_, reads=[r_scr_tab], writes=[rXZ])
            for n in range(2):
                kb.op("pe", lambda e: e.matmul(PB[1 + n][:], lhsT=JJ[:], rhs=XZ[:].rearrange("p h q -> p (h q)")[:, n * 512:(n + 1) * 512], start=True, stop=True),
                      reads=[rXZ, rC], writes=[rPB[1 + n]])
                kb.op("dve", lambda e: e.tensor_copy(out=TAB[:, zi, n * 4:(n + 1) * 4, :], in_=PB[1 + n][:].rearrange("p (h q) -> p h q", h=4)),
                      reads=[rPB[1 + n]], writes=[rTAB])
        IOT = SB(es2, "IOT", [128, 512], F32)
        kb.op("pool", lambda e: e.iota(IOT[:], pattern=[[1, 512]], base=0, channel_multiplier=0, allow_small_or_imprecise_dtypes=True), writes=[rTAB])
        kb.op("dve", lambda e: e.tensor_scalar(out=PEN[:], in0=IOT[:], scalar1=tq[:, 0:1], scalar2=NEG, op0=ALU.is_gt, op1=ALU.mult),
              reads=[rTAB, rC], writes=[rTAB])


    kb.barrier()
    es_att = ExitStack()
    KT = SB(es_att, "KT", [128, 4, S], BF16)
    V = SB(es_att, "V", [128, NT, 8, 65], BF16)
    KI = SB(es_att, "KI", [128, S], BF16)
    rKT, rV, rKI = Res("KT"), Res("V"), Res("KI")
    kb.op("pool", lambda e: e.memset(V[:, :, :, 64:65], 1.0), writes=[rV])

    kb.barrier()
    with Scope() as es:
        Wk = SB(es, "Wk", [128, 8, 512], BF16)
        Wv = SB(es, "Wv", [128, 8, 512], BF16)
        Wki = SB(es, "Wki", [128, 8, 128], BF16)
        rW = Res("W1a")
        ws = kb.slot("dq_w1a")
        kb.dma("pool", Wk[:], win_v[:, :, C_K:C_K + 512], ws, writes=[rW])
        kb.dma("pool", Wv[:], win_v[:, :, C_V:C_V + 512], ws, writes=[rW])
        kb.dma("pool", Wki[:, :, 0:64], win_v[:, :, C_KI:C_KI + 64], ws, writes=[rW])
        kb.dma("pool", Wki[:, :, 64:128], win_v[:, :, C_KI:C_KI + 64], ws, writes=[rW])
        xr = Ring(kb, "x", 2, lambda n: SB(es, n, [128, D], F32))
        xnr = Ring(kb, "xn", 2, lambda n: SB(es, n, [128, D], BF16))
        str_ = Ring(kb, "st", 2, lambda n: SB(es, n, [128, 4], F32))
        h4r = Ring(kb, "h4", 2, lambda n: SB(es, n, [128, 8, 512], BF16))
        xb_v = xb_d.ap().rearrange("(t p) n -> t p n", p=128)
        ev_i = 0
        pre = {}

        def do_stats(t):
            X, rX, sl = xr.next()
            kb.dma("sp", X[:], xb_v[t], sl, writes=[rX])
            xn, rxn, _ = xnr.next()
            st, rst, _ = str_.next()
            norm_stats(X[:], rX, 128, xn, rxn, st, rst, on_pool=(t % 2 == 1))
            pre[t] = (xn, rxn)

        do_stats(0)
        for g in range(NT // 4):
            h4, rh4, _ = h4r.next()
            for tt in range(4):
                t = g * 4 + tt
                if t + 1 < NT:
                    do_stats(t + 1)
                xn, rxn = pre.pop(t)
                norm_tr(xn, rxn, 128, lambda c: h4[:, c, tt * 128:(tt + 1) * 128], rh4, s1, sh1, 6 + (t % 2))
            for fc in range(5):
                bank = fc % 4
                for kc in range(8):
                    lw = Wk[:, kc, fc * 128:(fc + 1) * 128] if fc < 4 else Wki[:, kc, :]
                    kb.op("pe", lambda e: e.matmul(PB[bank][:], lhsT=lw, rhs=h4[:, kc, :], start=(kc == 0), stop=(kc == 7)),
                          reads=[rW, rh4], writes=[rPB[bank]])
                dst = KT[:, fc, g * 512:(g + 1) * 512] if fc < 4 else KI[:, g * 512:(g + 1) * 512]
                rd = rKT if fc < 4 else rKI
                if ev_i % 2 == 0:
                    kb.op("act", lambda e: e.copy(out=dst, in_=PB[bank][:]), reads=[rPB[bank]], writes=[rd])
                else:
                    kb.op("dve", lambda e: e.tensor_copy(out=dst, in_=PB[bank][:]), reads=[rPB[bank]], writes=[rd])
                ev_i += 1
            for tt in range(4):
                t = g * 4 + tt
                bank = 4 + (tt % 2)
                for kc in range(8):
                    kb.op("pe", lambda e: e.matmul(PB[bank][:], lhsT=h4[:, kc, tt * 128:(tt + 1) * 128], rhs=Wv[:, kc, :],
                                                   start=(kc == 0), stop=(kc == 7)), reads=[rW, rh4], writes=[rPB[bank]])
                src = PB[bank][:].rearrange("p (h d) -> p h d", h=8)
                if ev_i % 2 == 0:
                    kb.op("act", lambda e: e.copy(out=V[:, t, :, 0:64], in_=src), reads=[rPB[bank]], writes=[rV])
                else:
                    kb.op("dve", lambda e: e.tensor_copy(out=V[:, t, :, 0:64], in_=src), reads=[rPB[bank]], writes=[rV])
                ev_i += 1

    if stop == "1a":
        return finish([(TAB[:, 1, 0, :], rTAB, 128, 128), (TAB[:, 0, 3, :], rTAB, 128, 128), (PEN[:], rTAB, 128, 512),
                       (KT[:, 0, 0:1024], rKT, 128, 1024), (KT[:, 3, 7168:8192], rKT, 128, 1024),
                       (V[:, 5, :, :].rearrange("p h d -> p (h d)"), rV, 128, 520), (KI[:, 0:1024], rKI, 128, 1024)])
    kb.barrier()
    with Scope() as es:
        Wq = SB(es, "Wq", [128, 8, 512], BF16)
        Wqi = SB(es, "Wqi", [128, 8, 512], BF16)
        Wwi = SB(es, "Wwi", [128, 8, 8], BF16)
        rW = Res("W1b")
        ws = kb.slot("dq_w1b")
        kb.dma("pool", Wq[:], win_v[:, :, C_Q:C_Q + 512], ws, writes=[rW])
        kb.dma("pool", Wqi[:], win_v[:, :, C_QI:C_QI + 512], ws, writes=[rW])
        kb.dma("pool", Wwi[:], win_v[:, :, C_WI:C_WI + 8], ws, writes=[rW])
        xr = Ring(kb, "xo", 2, lambda n: SB(es, n, [128, D], F32))
        xnr = Ring(kb, "xno", 2, lambda n: SB(es, n, [128, D], BF16))
        str_ = Ring(kb, "sto", 2, lambda n: SB(es, n, [128, 4], F32))
        htr = Ring(kb, "hto", 2, lambda n: SB(es, n, [128, 8, 128], BF16))
        qsr = Ring(kb, "qs", 2, lambda n: SB(es, n, [128, 2, 512], BF16))
        ss = kb.slot("dq_spill")
        pre1b = {}

        def stats1b(i):
            X, rX, sl = xr.next()
            kb.dma("sp", X[:], xo_d.ap()[i, 2:130, :], sl, writes=[rX])
            xn, rxn, _ = xnr.next()
            st, rst, _ = str_.next()
            norm_stats(X[:], rX, 128, xn, rxn, st, rst)
            pre1b[i] = (xn, rxn)

        stats1b(0)
        for i in range(NSLOT):
            if i + 1 < NSLOT:
                stats1b(i + 1)
            xn, rxn = pre1b.pop(i)
            hT, rhT, _ = htr.next()
            norm_tr(xn, rxn, 128, lambda c: hT[:, c, :], rhT, s1, sh1, 6 + (i % 2))
            qs, rqs, _ = qsr.next()
            for wi_, (Wt, scl) in enumerate(((Wq, 0.125), (Wqi, 1.0))):
                bank = wi_
                for fc in range(4):
                    for kc in range(8):
                        kb.op("pe", lambda e: e.matmul(PB[bank][:, fc * 128:(fc + 1) * 128], lhsT=Wt[:, kc, fc * 128:(fc + 1) * 128],
                                                       rhs=hT[:, kc, :], start=(kc == 0), stop=(kc == 7)), reads=[rW, rhT], writes=[rPB[bank]])
                kb.op("act", lambda e: e.activation(out=qs[:, wi_, :], in_=PB[bank][:], func=AF.Copy, scale=scl),
                      reads=[rPB[bank]], writes=[rqs])
            kb.dma("sp", scr_q.ap()[i], qs[:, 0, :], ss, reads=[rqs], writes=[r_scr_q])
            kb.dma("sp", scr_qi.ap()[i], qs[:, 1, :], ss, reads=[rqs], writes=[r_scr_qi])
            for kc in range(8):
                kb.op("pe", lambda e: e.matmul(PB[2][:, 0:8], lhsT=hT[:, kc, :], rhs=Wwi[:, kc, :], start=(kc == 0), stop=(kc == 7)),
                      reads=[rW, rhT], writes=[rPB[2]])
            kb.op("act", lambda e: e.activation(out=wi_all[:, i, :], in_=PB[2][:, 0:8], func=AF.Abs, scale=0.125 * (8 ** -0.5)),
                  reads=[rPB[2]], writes=[rC])
            kb.op("dve", lambda e: e.tensor_scalar(out=sg_all[:, i, :], in0=PB[2][:, 0:8], scalar1=0.0, scalar2=2.0,
                                                   op0=ALU.is_ge, op1=ALU.mult), reads=[rPB[2]], writes=[rC])
            kb.op("dve", lambda e: e.tensor_scalar(out=sg_all[:, i, :], in0=sg_all[:, i, :], scalar1=-1.0, scalar2=None, op0=ALU.add),
                  reads=[rC], writes=[rC])

    if stop == "1b":
        return finish([(wi_all[:, 1, :], rC, 128, 8), (sg_all[:, 1, :], rC, 128, 8)])
    kb.barrier()
    with Scope() as es:
        SC = SB(es, "SC", [128, S], F32)
        rSC = Res("SC")
        qTr = Ring(kb, "qT", 1, lambda n: SB(es, n, [128, 4, 128], BF16))
        qiTr = Ring(kb, "qiT", 1, lambda n: SB(es, n, [128, 4, 128], BF16))
        DG = SB(es, "DG", [128, 8, 128], BF16)
        rDG = Res("DG")
        yr = Ring(kb, "yh", 2, lambda n: SB(es, n, [128, 512], BF16))
        BS = SB(es, "BS", [128, 16], F32)
        rBS = Res("BS")
        THD = SB(es, "THD", [128, 128], F32)
        THB = SB(es, "THB", [128, 128], F32)
        ONES = SB(es, "ONES", [128, 128], F32)
        rTH = Res("TH")
        kb.op("pool", lambda e: e.memset(ONES[:], 1.0), writes=[rTH])
        THR = SB(es, "THR", [128, 1], F32)
        BA = SB(es, "BA", [128, 16], F32)
        rTHR, rBA = Res("THR"), Res("BA")
        P2 = SB(es, "P2", [128, NIT + 2], F32)
        RN = SB(es, "RN", [128, NIT + 2], F32)
        for m_ in range(NIT + 2):
            kb.op("pool", lambda e: e.memset(P2[:, m_:m_ + 1], 2.0 ** -m_), writes=[rTH])
        ZR = SB(es, "ZR", [128, 260], BF16)
        kb.op("pool", lambda e: e.memset(ZR[:], 0.0), writes=[rTH])
        selr = Ring(kb, "sel", 3, lambda n: SB(es, n, [128, 128], BF16))
        EB = SB(es, "EB", [128, 2, 8, 128], BF16)
        rEB = [Res("E0"), Res("E1")]
        CJ = EB[:].rearrange("p a h q -> p (a h q)").bitcast(mybir.dt.uint8)

        class _ER:
            n = 0

            def next(self):
                k = self.n % 2
                self.n += 1
                return EB[:, k], rEB[k], None
        Er = _ER()
        OS = SB(es, "OS", [128, 8, 64], BF16)
        RS = SB(es, "RS", [128, 8], F32)
        rOS = Res("OS")

        for i in range(NSLOT):
            nch = i + 1
            Sc = nch * 512
            ntile = 4 * nch
            qT, rqT, sl1 = qTr.next()
            qiT, rqiT, sl2 = qiTr.next()
            kb.dma("sp", qT[:], scr_q.ap()[i].rearrange("p (c q) -> p c q", c=4), sl1, reads=[r_scr_q], writes=[rqT])
            kb.dma("sp", qiT[:], scr_qi.ap()[i].rearrange("p (c q) -> p c q", c=4), sl2, reads=[r_scr_qi], writes=[rqiT])
            for h in range(8):
                kb.op("pool", lambda e: e.tensor_scalar(out=DG[:, h, :], in0=ID[:], scalar1=sg_all[:, i, h:h + 1], scalar2=None, op0=ALU.mult),
                      reads=[rC], writes=[rDG])
            def dots_mm(ch, h):
                bank = h % 4
                hp = (h % 2) * 64
                kb.op("pe", lambda e: e.matmul(PB[bank][:], lhsT=qiT[hp:hp + 64, h // 2, :], rhs=KI[hp:hp + 64, ch * 512:(ch + 1) * 512],
                                               start=True, stop=True), reads=[rqiT, rKI], writes=[rPB[bank]])

            for ch in range(nch):
                ab = 6 + ch % 2
                dots_mm(ch, 0)
                dots_mm(ch, 1)
                for h in range(8):
                    bank = h % 4
                    y, ry, _ = yr.next()
                    if h % 2 == 0:
                        kb.op("act", lambda e: e.activation(out=y[:], in_=PB[bank][:], func=AF.Relu, scale=wi_all[:, i, h:h + 1]),
                              reads=[rPB[bank], rC], writes=[ry])
                    else:
                        kb.op("dve", lambda e: e.tensor_scalar(out=y[:], in0=PB[bank][:], scalar1=0.0, scalar2=wi_all[:, i, h:h + 1],
                                                               op0=ALU.max, op1=ALU.mult), reads=[rPB[bank], rC], writes=[ry])
                    if h + 2 < 8:
                        dots_mm(ch, h + 2)
                    kb.op("pe", lambda e: e.matmul(PB[ab][:], lhsT=DG[:, h, :], rhs=y[:], start=(h == 0), stop=(h == 7)),
                          reads=[rDG, ry], writes=[rPB[ab]])
                if ch == nch - 1:
                    kb.op("dve", lambda e: e.tensor_reduce(out=BS[:, 2:3], in_=PB[ab][:], axis=AX.X, op=ALU.min), reads=[rPB[ab]], writes=[rBS])
                    kb.op("dve", lambda e: e.tensor_tensor(out=SC[:, ch * 512:(ch + 1) * 512], in0=PB[ab][:], in1=PEN[:], op=ALU.add),
                          reads=[rPB[ab], rTAB], writes=[rSC])
                else:
                    kb.op("act", lambda e: e.copy(out=SC[:, ch * 512:(ch + 1) * 512], in_=PB[ab][:]), reads=[rPB[ab]], writes=[rSC])
            if stop == "2a":
                return finish([(SC[:, 0:512], rSC, 128, 512), (BS[:], rBS, 128, 16)])
            segs = [(a_, min(a_ + 4096, Sc)) for a_ in range(0, Sc, 4096)]
            for si, (a_, b_) in enumerate(segs):
                kb.op("dve", lambda e: e.tensor_reduce(out=BS[:, 0:1] if si == 0 else BS[:, 8:9], in_=SC[:, a_:b_], axis=AX.X, op=ALU.max), reads=[rSC], writes=[rBS])
                if si > 0:
                    kb.op("dve", lambda e: e.tensor_tensor(out=BS[:, 0:1], in0=BS[:, 0:1], in1=BS[:, 8:9], op=ALU.max), reads=[rBS], writes=[rBS])
            if nch > 1:
                fsegs = [(a_, min(a_ + 4096, Sc - 512)) for a_ in range(0, Sc - 512, 4096)]
                for (a_, b_) in fsegs:
                    kb.op("dve", lambda e: e.tensor_reduce(out=BS[:, 7:8], in_=SC[:, a_:b_], axis=AX.X, op=ALU.min), reads=[rSC], writes=[rBS])
                    kb.op("dve", lambda e: e.tensor_tensor(out=BS[:, 2:3], in0=BS[:, 2:3], in1=BS[:, 7:8], op=ALU.min), reads=[rBS], writes=[rBS])
            kb.op("dve", lambda e: e.tensor_tensor(out=BS[:, 3:4], in0=BS[:, 0:1], in1=BS[:, 2:3], op=ALU.subtract), reads=[rBS], writes=[rBS])
            kb.op("dve", lambda e: e.tensor_scalar(out=BS[:, 3:4], in0=BS[:, 3:4], scalar1=1.001, scalar2=1e-6, op0=ALU.mult, op1=ALU.add),
                  reads=[rBS], writes=[rBS])
            kb.op("dve", lambda e: e.tensor_scalar(out=RN[:], in0=P2[:], scalar1=BS[:, 3:4], scalar2=None, op0=ALU.mult), reads=[rBS, rTH], writes=[rBS])
            kb.op("dve", lambda e: e.scalar_tensor_tensor(out=THR[:], in0=BS[:, 3:4], scalar=-0.5, in1=BS[:, 0:1], op0=ALU.mult, op1=ALU.add),
                  reads=[rBS], writes=[rTHR])
            use_act = Sc >= 1536
            Sd = (int(round(0.64 * Sc / 128)) * 128) if use_act else Sc
            dsegs = [(a_, min(a_ + 4096, Sd)) for a_ in range(0, Sd, 4096)]
            asegs = [(a_, min(a_ + 512, Sc)) for a_ in range(Sd, Sc, 512)] if use_act else []
            n_act = Sc - Sd
            for it in range(NIT):
                for k_, (a_, b_) in enumerate(asegs):
                    kb.op("act", lambda e: e.activation(out=PB[k_ % 4][:, 0:b_ - a_], in_=SC[:, a_:b_], func=AF.Sign, scale=-1.0, bias=THR[:, 0:1],
                                                        accum_out=BA[:, k_:k_ + 1]), reads=[rSC, rTHR], writes=[rBA, rPB[k_ % 4]])
                for si, (a_, b_) in enumerate(dsegs):
                    kb.op("dve", lambda e: e.tensor_scalar(out=CJ[:, 0:b_ - a_], in0=SC[:, a_:b_], scalar1=THR[:, 0:1], scalar2=0.0,
                                                           op0=ALU.is_ge, op1=ALU.add, accum_out=BS[:, 5:6] if si == 0 else BS[:, 10:11]),
                          reads=[rSC, rTHR], writes=[rBS, rEB[0], rEB[1]])
                    if si > 0:
                        kb.op("dve", lambda e: e.tensor_tensor(out=BS[:, 5:6], in0=BS[:, 5:6], in1=BS[:, 10:11], op=ALU.add), reads=[rBS], writes=[rBS])
                if use_act:
                    if len(asegs) > 1:
                        kb.op("dve", lambda e: e.tensor_reduce(out=BS[:, 9:10], in_=BA[:, 0:len(asegs)], axis=AX.X, op=ALU.add), reads=[rBA], writes=[rBS])
                        sa = BS[:, 9:10]
                    else:
                        sa = BA[:, 0:1]
                    kb.op("dve", lambda e: e.scalar_tensor_tensor(out=BS[:, 5:6], in0=sa, scalar=-0.5, in1=BS[:, 5:6], op0=ALU.mult, op1=ALU.add),
                          reads=[rBS, rBA], writes=[rBS])
                kb.op("dve", lambda e: e.tensor_scalar(out=BS[:, 6:7], in0=BS[:, 5:6], scalar1=255.5 - 0.5 * n_act, scalar2=-0.5, op0=ALU.is_ge, op1=ALU.add),
                      reads=[rBS], writes=[rBS])
                kb.op("dve", lambda e: e.scalar_tensor_tensor(out=THR[:], in0=BS[:, 6:7], scalar=RN[:, it + 1:it + 2], in1=THR[:], op0=ALU.mult, op1=ALU.add),
                      reads=[rBS, rTHR], writes=[rTHR])
            kb.op("dve", lambda e: e.tensor_tensor(out=BS[:, 1:2], in0=THR[:], in1=RN[:, NIT + 1:NIT + 2], op=ALU.subtract), reads=[rBS, rTHR], writes=[rBS])
            if stop == "2b":
                return finish([(SC[:, 0:512], rSC, 128, 512), (BS[:], rBS, 128, 16)])
            kb.op("dve", lambda e: e.tensor_scalar(out=THD[:], in0=ID32[:], scalar1=BS[:, 1:2], scalar2=None, op0=ALU.mult),
                  reads=[rBS, rC], writes=[rTH])
            kb.op("pe", lambda e: e.matmul(PB[7][:, 0:128], lhsT=ONES[:], rhs=THD[:], start=True, stop=True), reads=[rTH], writes=[rPB[7]])
            kb.op("act", lambda e: e.copy(out=THB[:], in_=PB[7][:, 0:128]), reads=[rPB[7]], writes=[rTH])
            for hh in range(2):
                kb.op("pe", lambda e: e.matmul(PB[4 + hh][:, 0:260], lhsT=ID[:], rhs=ZR[:], start=True, stop=False, skip_group_check=True),
                      reads=[rC, rTH], writes=[rPB[4 + hh]])
            st_sel = {}

            def stage_a(kt):
                sq = (kt % 4) * 128
                kb.op("pe", lambda e: e.transpose(out=PB[6][:, sq:sq + 128], in_=SC[:, kt * 128:(kt + 1) * 128], identity=ID32[:]),
                      reads=[rSC, rC], writes=[rPB[6]])
                sel, rsel, _ = selr.next()
                kb.op("dve", lambda e: e.tensor_tensor(out=sel[:], in0=PB[6][:, sq:sq + 128], in1=THB[:], op=ALU.is_ge),
                      reads=[rPB[6], rTH], writes=[rsel])
                st_sel[kt] = (sel, rsel)
                lb = (kt % 2) * 2
                for h in range(8):
                    hp = (h % 2) * 64
                    kb.op("pe", lambda e: e.matmul(PB[lb + h % 2][:, (h // 2) * 128:(h // 2 + 1) * 128], lhsT=KT[hp:hp + 64, h // 2, kt * 128:(kt + 1) * 128],
                                                   rhs=qT[hp:hp + 64, h // 2, :], start=True, stop=True), reads=[rKT, rqT], writes=[rPB[lb + h % 2]])

            def stage_b(kt):
                sel, rsel = st_sel.pop(kt)
                lb = (kt % 2) * 2
                E, rE, _ = Er.next()
                for hh in range(2):
                    kb.op("act", lambda e: e.activation(out=E[:, hh * 4:(hh + 1) * 4, :], in_=PB[lb + hh][:].rearrange("p (h q) -> p h q", h=4), func=AF.Exp),
                          reads=[rPB[lb + hh]], writes=[rE])
                zi = kt - (4 * i - 1)
                if zi >= 0:
                    kb.op("pool", lambda e: e.tensor_tensor(out=E[:], in0=E[:], in1=TAB[:, zi, :, :], op=ALU.mult), reads=[rE, rTAB], writes=[rE])
                kb.op("dve", lambda e: e.tensor_tensor(out=E[:], in0=E[:], in1=sel[:].unsqueeze(1).to_broadcast([128, 8, 128]), op=ALU.mult),
                      reads=[rE, rsel], writes=[rE])
                for h in range(8):
                    kb.op("pe", lambda e: e.matmul(PB[4 + h // 4][:, (h % 4) * 65:(h % 4 + 1) * 65], lhsT=E[:, (h % 2) * 4 + h // 2, :], rhs=V[:, kt, h, :],
                                                   start=False, stop=(kt == ntile - 1), skip_group_check=True), reads=[rE, rV], writes=[rPB[4 + h // 4]])

            stage_a(0)
            for kt in range(ntile):
                if kt + 1 < ntile:
                    stage_a(kt + 1)
                stage_b(kt)
            for hh in range(2):
                ov = PB[4 + hh][:, 0:260].rearrange("p (h d) -> p h d", h=4)
                kb.op("dve", lambda e: e.reciprocal(out=RS[:, hh * 4:(hh + 1) * 4], in_=ov[:, :, 64]), reads=[rPB[4 + hh]], writes=[rOS])
                kb.op("dve", lambda e: e.tensor_tensor(out=OS[:, hh * 4:(hh + 1) * 4, :], in0=ov[:, :, 0:64],
                                                       in1=RS[:, hh * 4:(hh + 1) * 4].unsqueeze(2).to_broadcast([128, 4, 64]), op=ALU.mult),
                      reads=[rPB[4 + hh], rOS], writes=[rOS])
            kb.dma("sp", scr_o.ap()[i], OS[:].rearrange("p h d -> p (h d)"), ss, reads=[rOS], writes=[r_scr_o])
            if stop == "2_%d" % i:
                return finish([(SC[:, 0:1024], rSC, 128, 1024), (BS[:], rBS, 128, 16), (OS[:].rearrange("p h d -> p (h d)"), rOS, 128, 512),
                               (wi_all[:, i, :], rC, 128, 8), (sg_all[:, i, :], rC, 128, 8), (qT[:, 0, :], rqT, 128, 128), (RS[:], rOS, 128, 8)])
    es_att.close()
    kb.barrier()

    kb.barrier()
    es_h2t = ExitStack()
    H2T = SB(es_h2t, "H2T", [128, 8, NSLOT * 128], BF16)
    rH2T = Res("H2T")
    kb.barrier()
    with Scope() as es:
        Wc = SB(es, "Wc", [128, 8, 1536], BF16)
        Wgl = SB(es, "Wgl", [128, 8, 2048], BF16)
        Wab = SB(es, "Wab", [128, 4, D], BF16)
        Wcb = SB(es, "Wcb", [128, 4, D], BF16)
        cw = SB(es, "cw", [128, 4, 3], F32)
        rW = Res("W3")
        ws = kb.slot("dq_w3")
        kb.dma("pool", Wc[:], win_v[:, :, C_CB:C_CB + 1536], ws, writes=[rW])
        kb.dma("pool", Wgl[:], win_v[:, :, C_GL:C_GL + 2048], ws, writes=[rW])
        kb.dma("pool", Wab[:], wab_d.ap().rearrange("(k p) n -> p k n", p=128), ws, writes=[rW])
        kb.dma("pool", Wcb[:], wcbr_d.ap().rearrange("(k p) n -> p k n", p=128), ws, writes=[rW])
        kb.dma("sp", cw[:], convw_d.ap(), ws, writes=[rW])
        XH = SB(es, "XH", [2, D], F32)
        XNH = SB(es, "XNH", [2, D], BF16)
        STH = SB(es, "STH", [2, 4], F32)
        rXH = Res("XH")
        xr = Ring(kb, "x3", 2, lambda n: SB(es, n, [128, D], F32))
        xn3r = Ring(kb, "XN3", 2, lambda n: SB(es, n, [128, D], BF16))
        st3r = Ring(kb, "ST3", 2, lambda n: SB(es, n, [128, 4], F32))
        HT = SB(es, "HT3", [128, 8, 130], BF16)
        rHT = Res("HT3")
        CCs = SB(es, "CCs", [128, 4, 130], F32)
        U = SB(es, "U", [128, 4, 130], F32)
        CV = SB(es, "CV", [128, 4, 128], F32)
        VT = SB(es, "VT", [128, 4, 128], BF16)
        rU = Res("U")
        rUf = [Res("U%d" % k) for k in range(4)]
        G = SB(es, "G", [128, 2048], F32)
        rG = Res("G")
        M1 = SB(es, "M1", [128, D], F32)
        MB = SB(es, "MB", [128, D], BF16)
        rM = Res("M")
        OSb = SB(es, "OSb", [128, 512], BF16)
        AT = SB(es, "AT", [128, 4, 128], BF16)
        rAT = Res("AT")
        osl = kb.slot("dq_o3")
        pre3 = {}

        def stats3(i):
            X, rX, sl = xr.next()
            kb.dma("sp", X[:], xo_d.ap()[i, 2:130, :], sl, writes=[rX])
            xn, rxn, _ = xn3r.next()
            st, rst, _ = st3r.next()
            norm_stats(X[:], rX, 128, xn, rxn, st, rst)
            pre3[i] = (xn, rxn)

        stats3(0)
        for i in range(NSLOT):
            if i + 1 < NSLOT:
                stats3(i + 1)
            kb.dma("sp", XH[:], xo_d.ap()[i, 0:2, :], None, writes=[rXH])
            kb.dma("sp", OSb[:], scr_o.ap()[i], osl, reads=[r_scr_o], writes=[rAT])
            xn, rxn = pre3.pop(i)
            norm_tr(xn, rxn, 128, lambda c: HT[:, c, 2:130], rHT, s1, sh1, 6)
            kb.op("act", lambda e: e.activation(out=XNH[:], in_=XH[:], func=AF.Square, accum_out=STH[:, 0:1]), reads=[rXH], writes=[rXH])
            kb.op("act", lambda e: e.activation(out=STH[:, 1:2], in_=STH[:, 0:1], func=AF.Ln, scale=1.0 / D, bias=epsb[0:2, 0:1]), reads=[rXH, rC], writes=[rXH])
            kb.op("act", lambda e: e.activation(out=STH[:, 2:3], in_=STH[:, 1:2], func=AF.Exp, scale=-0.5), reads=[rXH], writes=[rXH])
            kb.op("dve", lambda e: e.tensor_scalar(out=XNH[:], in0=XH[:], scalar1=STH[:, 2:3], scalar2=None, op0=ALU.mult), reads=[rXH], writes=[rXH])
            for c in range(8):
                kb.op("pe", lambda e: e.matmul(PB[7][:, c * 2:c * 2 + 2], lhsT=XNH[:, c * 128:(c + 1) * 128], rhs=ID[0:2, 0:2], start=True, stop=True),
                      reads=[rXH, rC], writes=[rPB[7]])
            for c in range(8):
                kb.op("dve", lambda e: e.tensor_scalar(out=HT[:, c, 0:2], in0=PB[7][:, c * 2:c * 2 + 2], scalar1=s1[:, c:c + 1], scalar2=sh1[:, c:c + 1],
                                                       op0=ALU.mult, op1=ALU.add), reads=[rPB[7], rMOD], writes=[rHT])
            for fc in range(4):
                cb0 = (fc % 2) * 3
                for kc in range(8):
                    kb.op("pe", lambda e: e.matmul(PB[cb0][:, 0:130], lhsT=Wc[:, kc, 512 + fc * 128:512 + (fc + 1) * 128], rhs=HT[:, kc, :],
                                                   start=(kc == 0), stop=(kc == 7)), reads=[rW, rHT], writes=[rPB[cb0]])
                for kc in range(8):
                    kb.op("pe", lambda e: e.matmul(PB[cb0 + 1][:, 0:130], lhsT=Wc[:, kc, 1024 + fc * 128:1024 + (fc + 1) * 128], rhs=HT[:, kc, :],
                                                   start=(kc == 0), stop=(kc == 7)), reads=[rW, rHT], writes=[rPB[cb0 + 1]])
                for kc in range(8):
                    kb.op("pe", lambda e: e.matmul(PB[cb0 + 2][:, 0:128], lhsT=Wc[:, kc, fc * 128:(fc + 1) * 128], rhs=HT[:, kc, 2:130],
                                                   start=(kc == 0), stop=(kc == 7)), reads=[rW, rHT], writes=[rPB[cb0 + 2]])
                kb.op("act", lambda e: e.copy(out=CCs[:, fc, :], in_=PB[cb0][:, 0:130]), reads=[rPB[cb0]], writes=[rUf[fc]])
                kb.op("dve", lambda e: e.tensor_tensor(out=U[:, fc, :], in0=CCs[:, fc, :], in1=PB[cb0 + 1][:, 0:130], op=ALU.mult), reads=[rPB[cb0 + 1], rUf[fc]], writes=[rUf[fc]])
                kb.op("dve", lambda e: e.tensor_scalar(out=U[:, fc, 0:2], in0=U[:, fc, 0:2], scalar1=hmask[:, i:i + 1], scalar2=None, op0=ALU.mult),
                      reads=[rUf[fc], rC], writes=[rUf[fc]])
                kb.op("dve", lambda e: e.tensor_scalar(out=CV[:, fc, :], in0=U[:, fc, 0:128], scalar1=cw[:, fc, 0:1], scalar2=None, op0=ALU.mult),
                      reads=[rUf[fc], rW], writes=[rUf[fc]])
                kb.op("dve", lambda e: e.scalar_tensor_tensor(out=CV[:, fc, :], in0=U[:, fc, 1:129], scalar=cw[:, fc, 1:2], in1=CV[:, fc, :],
                                                               op0=ALU.mult, op1=ALU.add), reads=[rUf[fc], rW], writes=[rUf[fc]])
                kb.op("dve", lambda e: e.scalar_tensor_tensor(out=CV[:, fc, :], in0=U[:, fc, 2:130], scalar=cw[:, fc, 2:3], in1=CV[:, fc, :],
                                                               op0=ALU.mult, op1=ALU.add), reads=[rUf[fc], rW], writes=[rUf[fc]])
                kb.op("dve", lambda e: e.tensor_tensor(out=VT[:, fc, :], in0=CV[:, fc, :], in1=PB[cb0 + 2][:, 0:128], op=ALU.mult), reads=[rPB[cb0 + 2], rUf[fc]], writes=[rUf[fc]])
            for n in range(4):
                for kc in range(8):
                    kb.op("pe", lambda e: e.matmul(PB[6 + n % 2][:], lhsT=HT[:, kc, 2:130], rhs=Wgl[:, kc, n * 512:(n + 1) * 512],
                                                   start=(kc == 0), stop=(kc == 7)), reads=[rW, rHT], writes=[rPB[6 + n % 2]])
                kb.op("act", lambda e: e.activation(out=G[:, n * 512:(n + 1) * 512], in_=PB[6 + n % 2][:], func=AF.Sigmoid), reads=[rPB[6 + n % 2]], writes=[rG])
            for c in range(4):
                kb.op("pe", lambda e: e.transpose(out=pbf(5)[:, c * 128:(c + 1) * 128], in_=OSb[:, c * 128:(c + 1) * 128], identity=ID[:]),
                      reads=[rAT, rC], writes=[rPB[5]])
            kb.op("dve", lambda e: e.tensor_copy(out=AT[:].rearrange("p c q -> p (c q)"), in_=pbf(5)[:, 0:512]), reads=[rPB[5]], writes=[rAT])
            for n in range(2):
                for kc in range(4):
                    kb.op("pe", lambda e: e.matmul(PB[0 + n][:], lhsT=AT[:, kc, :], rhs=Wab[:, kc, n * 512:(n + 1) * 512], start=(kc == 0), stop=(kc == 3)),
                          reads=[rW, rAT], writes=[rPB[0 + n]])
                kb.op("dve", lambda e: e.tensor_tensor(out=M1[:, n * 512:(n + 1) * 512], in0=G[:, n * 512:(n + 1) * 512], in1=PB[0 + n][:], op=ALU.mult),
                      reads=[rPB[0 + n], rG], writes=[rM])
            for n in range(2):
                for kc in range(4):
                    kb.op("pe", lambda e: e.matmul(PB[0 + n][:], lhsT=VT[:, kc, :], rhs=Wcb[:, kc, n * 512:(n + 1) * 512], start=(kc == 0), stop=(kc == 3)),
                          reads=[rW] + rUf, writes=[rPB[0 + n]])
                kb.op("dve", lambda e: e.tensor_tensor(out=G[:, D + n * 512:D + (n + 1) * 512], in0=G[:, D + n * 512:D + (n + 1) * 512], in1=PB[0 + n][:], op=ALU.mult),
                      reads=[rPB[0 + n], rG], writes=[rG])
            kb.op("dve", lambda e: e.tensor_tensor(out=MB[:], in0=M1[:], in1=G[:, D:2 * D], op=ALU.add), reads=[rM, rG], writes=[rM])
            for c in range(8):
                kb.op("pe", lambda e: e.transpose(out=pbf(5)[:, c * 128:(c + 1) * 128], in_=MB[:, c * 128:(c + 1) * 128], identity=ID[:]),
                      reads=[rM, rC], writes=[rPB[5]])
            kb.op("act", lambda e: e.copy(out=H2T[:, :, i * 128:(i + 1) * 128], in_=pbf(5).rearrange("p (c q) -> p c q", c=8)),
                  reads=[rPB[5]], writes=[rH2T])

    if stop == "3a":
        return finish([(H2T[:, 0, 0:1024], rH2T, 128, 1024), (H2T[:, 7, 1024:2048], rH2T, 128, 1024)])
    kb.barrier()
    es_moe = ExitStack()
    HR = SB(es_moe, "HR", [128, NSLOT, D], F32)
    GD = SB(es_moe, "GD", [128, NSLOT, 32], F32)
    rHR = [Res("HR%d" % i) for i in range(NSLOT)]
    rGD = Res("GD")
    kb.barrier()
    with Scope() as es:
        Wo = SB(es, "Wo", [128, 8, D], BF16)
        Wr = SB(es, "Wr", [128, 8, 36], F32)
        brb = SB(es, "brb", [128, 36], F32)
        g1bc = SB(es, "g1bc", [128, D], F32)
        rW = Res("W3b")
        rg1 = Res("g1bc")
        ws = kb.slot("dq_w3b")
        kb.dma("pool", Wo[:], wout_d.ap().rearrange("(k p) n -> p k n", p=128), ws, writes=[rW])
        kb.dma("sp", Wr[:], wr_d.ap().rearrange("(k p) n -> p k n", p=128), ws, writes=[rW])
        kb.dma("sp", brb[:], br_d.ap().partition_broadcast(128), ws, writes=[rW])
        gate_bc(g1bc, rg1, 2)
        for kc in range(8):
            kb.op("dve", lambda e: e.tensor_tensor(out=Wo[:, kc, :], in0=Wo[:, kc, :], in1=g1bc[:], op=ALU.mult), reads=[rW, rg1], writes=[rW])
        xr = Ring(kb, "x3b", 2, lambda n: SB(es, n, [128, D], F32))
        ST = SB(es, "ST3b", [128, 4], F32)
        rST = Res()
        H2 = SB(es, "H2", [128, D], F32)
        H2T32 = SB(es, "H2T32", [128, 8, 128], F32)
        rH2 = Res("H2")
        RT = SB(es, "RT", [128, 96], F32)
        rRT = Res("RT")
        for i in range(NSLOT):
            X, rX, sl = xr.next()
            kb.dma("sp", X[:], xo_d.ap()[i, 2:130, :], sl, writes=[rX])
            for n in range(2):
                for kc in range(8):
                    kb.op("pe", lambda e: e.matmul(PB[0 + n][:], lhsT=H2T[:, kc, i * 128:(i + 1) * 128], rhs=Wo[:, kc, n * 512:(n + 1) * 512],
                                                   start=(kc == 0), stop=(kc == 7)), reads=[rW, rH2T], writes=[rPB[0 + n]])
                kb.op("dve", lambda e: e.tensor_tensor(out=HR[:, i, n * 512:(n + 1) * 512], in0=X[:, n * 512:(n + 1) * 512], in1=PB[0 + n][:], op=ALU.add),
                      reads=[rPB[0 + n], rX], writes=[rHR[i]])
            if stop == "3b1":
                return finish([(HR[:, 0, :], rHR[0], 128, 1024)])
            kb.op("act", lambda e: e.activation(out=H2[:], in_=HR[:, i, :], func=AF.Square, accum_out=ST[:, 0:1]), reads=[rHR[i]], writes=[rST, rH2])
            kb.op("act", lambda e: e.activation(out=ST[:, 1:2], in_=ST[:, 0:1], func=AF.Ln, scale=1.0 / D, bias=epsb[:, 0:1]), reads=[rST, rC], writes=[rST])
            kb.op("act", lambda e: e.activation(out=ST[:, 2:3], in_=ST[:, 1:2], func=AF.Exp, scale=-0.5), reads=[rST], writes=[rST])
            kb.op("dve", lambda e: e.tensor_scalar(out=H2[:], in0=HR[:, i, :], scalar1=ST[:, 2:3], scalar2=None, op0=ALU.mult), reads=[rHR[i], rST], writes=[rH2])
            if stop == "3b1a":
                return finish([(H2[:], rH2, 128, 1024), (ST[:], rST, 128, 4)])
            for c in range(8):
                pb = 6 + c // 4
                kb.op("pe", lambda e: e.transpose(out=PB[pb][:, (c % 4) * 128:(c % 4 + 1) * 128], in_=H2[:, c * 128:(c + 1) * 128], identity=ID32[:]),
                      reads=[rH2, rC], writes=[rPB[pb]])
            if stop == "3b1b":
                kb.op("dve", lambda e: e.tensor_copy(out=H2[:, 0:512], in_=PB[6][:]), reads=[rPB[6]], writes=[rH2])
                kb.op("dve", lambda e: e.tensor_copy(out=H2[:, 512:1024], in_=PB[7][:]), reads=[rPB[7]], writes=[rH2])
                return finish([(H2[:], rH2, 128, 1024)])
            for c in range(8):
                pb = 6 + c // 4
                src = PB[pb][:, (c % 4) * 128:(c % 4 + 1) * 128]
                kb.op("act", lambda e: e.activation(out=H2T32[:, c, :], in_=src, func=AF.Identity, scale=s2[:, c:c + 1], bias=sh2[:, c:c + 1]),
                      reads=[rPB[pb], rMOD], writes=[rH2])
                kb.op("dve", lambda e: e.tensor_copy(out=H2T[:, c, i * 128:(i + 1) * 128], in_=H2T32[:, c, :]), reads=[rH2], writes=[rH2T])
            if stop == "3b2":
                return finish([(HR[:, 0, :], rHR[0], 128, 1024), (H2T32[:].rearrange("p c q -> p (c q)"), rH2, 128, 1024)])
            for kc in range(8):
                kb.op("pe", lambda e: e.matmul(PB[2][:, 0:36], lhsT=H2T32[:, kc, :], rhs=Wr[:, kc, :], start=(kc == 0), stop=(kc == 7)),
                      reads=[rW, rH2], writes=[rPB[2]])
            L = RT[:, 0:36]
            if stop == "3b3":
                kb.op("dve", lambda e: e.tensor_copy(out=RT[:, 0:36], in_=PB[2][:, 0:36]), reads=[rPB[2]], writes=[rRT])
                return finish([(RT[:, 0:36], rRT, 128, 36)])

            def rt(fn, extra_r=(), e_="dve"):
                kb.op(e_, fn, reads=[rRT] + list(extra_r), writes=[rRT])
            kb.op("dve", lambda e: e.tensor_tensor(out=L, in0=PB[2][:, 0:36], in1=brb[:], op=ALU.add), reads=[rPB[2], rW], writes=[rRT])
            rt(lambda e: e.tensor_reduce(out=RT[:, 36:37], in_=RT[:, 0:4], axis=AX.X, op=ALU.max))
            rt(lambda e: e.tensor_scalar(out=RT[:, 80:84], in0=RT[:, 0:4], scalar1=RT[:, 36:37], scalar2=None, op0=ALU.is_ge))
            rt(lambda e: e.tensor_scalar(out=RT[:, 37:41], in0=RT[:, 0:4], scalar1=RT[:, 36:37], scalar2=None, op0=ALU.subtract))
            rt(lambda e: e.activation(out=RT[:, 37:41], in_=RT[:, 37:41], func=AF.Exp, accum_out=RT[:, 41:42]), e_="act")
            rt(lambda e: e.reciprocal(out=RT[:, 42:43], in_=RT[:, 41:42]))
            rt(lambda e: e.tensor_scalar(out=RT[:, 84:88], in0=RT[:, 80:84], scalar1=-1.0, scalar2=1.0e30, op0=ALU.add, op1=ALU.mult))
            rt(lambda e: e.tensor_tensor(out=RT[:, 44:76].rearrange("p (g x) -> p g x", g=4), in0=RT[:, 4:36].rearrange("p (g x) -> p g x", g=4),
                                         in1=RT[:, 84:88].unsqueeze(2).to_broadcast([128, 4, 8]), op=ALU.add))
            rt(lambda e: e.tensor_reduce(out=RT[:, 76:77], in_=RT[:, 44:76], axis=AX.X, op=ALU.max))
            rt(lambda e: e.tensor_scalar(out=GD[:, i, :], in0=RT[:, 44:76], scalar1=RT[:, 76:77], scalar2=None, op0=ALU.is_ge), extra_r=[rGD])
            rt(lambda e: e.scalar_tensor_tensor(out=RT[:, 44:76], in0=GD[:, i, :], scalar=-1.0e30, in1=RT[:, 44:76], op0=ALU.mult, op1=ALU.add), extra_r=[rGD])
            rt(lambda e: e.tensor_reduce(out=RT[:, 77:78], in_=RT[:, 44:76], axis=AX.X, op=ALU.max))
            rt(lambda e: e.tensor_scalar(out=RT[:, 44:76], in0=RT[:, 44:76], scalar1=RT[:, 77:78], scalar2=None, op0=ALU.is_ge))
            rt(lambda e: e.tensor_tensor(out=RT[:, 78:79], in0=RT[:, 77:78], in1=RT[:, 76:77], op=ALU.subtract))
            rt(lambda e: e.activation(out=RT[:, 78:79], in_=RT[:, 78:79], func=AF.Exp), e_="act")
            rt(lambda e: e.tensor_scalar(out=RT[:, 78:79], in0=RT[:, 78:79], scalar1=1.0, scalar2=None, op0=ALU.add))
            rt(lambda e: e.reciprocal(out=RT[:, 78:79], in_=RT[:, 78:79]))
            rt(lambda e: e.tensor_scalar(out=RT[:, 79:80], in0=RT[:, 78:79], scalar1=-1.0, scalar2=1.0, op0=ALU.mult, op1=ALU.add))
            rt(lambda e: e.tensor_tensor(out=RT[:, 78:80], in0=RT[:, 78:80], in1=RT[:, 42:43].to_broadcast([128, 2]), op=ALU.mult))
            rt(lambda e: e.tensor_scalar(out=GD[:, i, :], in0=GD[:, i, :], scalar1=RT[:, 78:79], scalar2=None, op0=ALU.mult), extra_r=[rGD])
            kb.op("dve", lambda e: e.scalar_tensor_tensor(out=GD[:, i, :], in0=RT[:, 44:76], scalar=RT[:, 79:80], in1=GD[:, i, :], op0=ALU.mult, op1=ALU.add),
                  reads=[rRT], writes=[rGD, rRT])

    if stop == "3b":
        return finish([(HR[:, 0, :], rHR[0], 128, 1024), (HR[:, 15, :], rHR[15], 128, 1024), (GD[:, 0, :], rGD, 128, 32), (GD[:, 15, :], rGD, 128, 32),
                       (H2T[:, 0, 0:1024], rH2T, 128, 1024)])
    kb.barrier()
    with Scope() as es:
        wgr = Ring(kb, "wg", 2, lambda n: SB(es, n, [128, 8, 512], BF16))
        wur = Ring(kb, "wu", 2, lambda n: SB(es, n, [128, 8, 512], BF16))
        wdr = Ring(kb, "wd", 2, lambda n: SB(es, n, [128, 4, D], BF16))
        sir = Ring(kb, "si", 2, lambda n: SB(es, n, [128, 512], F32))
        acr = Ring(kb, "ac", 2, lambda n: SB(es, n, [128, 4, 512], BF16))
        g2bc = SB(es, "g2bc", [128, D], F32)
        rg2 = Res("g2bc")
        gate_bc(g2bc, rg2, 5)
        wge_v = wge_d.ap().rearrange("e (k p) n -> e p k n", p=128)
        wue_v = wue_d.ap().rearrange("e (k p) n -> e p k n", p=128)
        wde_v = wde_d.ap().rearrange("e (k p) n -> e p k n", p=128)
        wts = {}
        acs = {}

        def load_expert(ex):
            Wg, rWg, sg_ = wgr.next()
            Wu, rWu, su_ = wur.next()
            Wd, rWd, sd_ = wdr.next()
            kb.dma("pool", Wg[:], wge_v[ex], sg_, writes=[rWg])
            kb.dma("pool", Wu[:], wue_v[ex], su_, writes=[rWu])
            kb.dma("pool", Wd[:], wde_v[ex], sd_, writes=[rWd])
            for kc in range(4):
                kb.op("dve", lambda e: e.tensor_tensor(out=Wd[:, kc, :], in0=Wd[:, kc, :], in1=g2bc[:], op=ALU.mult), reads=[rWd, rg2], writes=[rWd])
            wts[ex] = (Wg, rWg, Wu, rWu, Wd, rWd)

        def stage_gu(ex, g):
            if ex not in wts:
                load_expert(ex)
            Wg, rWg, Wu, rWu, Wd, rWd = wts[ex]
            ac, rac, _ = acr.next()
            acs[(ex, g)] = (ac, rac)
            for fc in range(4):
                bg = (fc % 2) * 2
                for kc in range(8):
                    kb.op("pe", lambda e: e.matmul(PB[bg][:], lhsT=Wg[:, kc, fc * 128:(fc + 1) * 128], rhs=H2T[:, kc, g * 512:(g + 1) * 512],
                                                   start=(kc == 0), stop=(kc == 7)), reads=[rWg, rH2T], writes=[rPB[bg]])
                for kc in range(8):
                    kb.op("pe", lambda e: e.matmul(PB[bg + 1][:], lhsT=Wu[:, kc, fc * 128:(fc + 1) * 128], rhs=H2T[:, kc, g * 512:(g + 1) * 512],
                                                   start=(kc == 0), stop=(kc == 7)), reads=[rWu, rH2T], writes=[rPB[bg + 1]])
                si, rsi, _ = sir.next()
                kb.op("act", lambda e: e.activation(out=si[:], in_=PB[bg][:], func=AF.Silu), reads=[rPB[bg]], writes=[rsi])
                kb.op("dve", lambda e: e.tensor_tensor(out=ac[:, fc, :], in0=si[:], in1=PB[bg + 1][:], op=ALU.mult), reads=[rPB[bg + 1], rsi], writes=[rac])

        def stage_down(ex, g):
            Wg, rWg, Wu, rWu, Wd, rWd = wts[ex]
            ac, rac = acs.pop((ex, g))
            for tt in range(4):
                sl_i = g * 4 + tt
                for n in range(2):
                    bd = 4 + (tt % 2) * 2 + n
                    for kc in range(4):
                        kb.op("pe", lambda e: e.matmul(PB[bd][:], lhsT=ac[:, kc, tt * 128:(tt + 1) * 128], rhs=Wd[:, kc, n * 512:(n + 1) * 512],
                                                       start=(kc == 0), stop=(kc == 3)), reads=[rWd, rac], writes=[rPB[bd]])
                    kb.op("dve", lambda e: e.scalar_tensor_tensor(out=HR[:, sl_i, n * 512:(n + 1) * 512], in0=PB[bd][:], scalar=GD[:, sl_i, ex:ex + 1],
                                                                  in1=HR[:, sl_i, n * 512:(n + 1) * 512], op0=ALU.mult, op1=ALU.add),
                          reads=[rPB[bd], rGD], writes=[rHR[sl_i]])
            if g == 3:
                wts.pop(ex)

        items = [(ex, g) for ex in range(32) for g in range(4)]
        stage_gu(*items[0])
        for k_ in range(len(items)):
            if k_ + 1 < len(items):
                stage_gu(*items[k_ + 1])
            stage_down(*items[k_])

        gfb = SB(es, "gfb", [128, D], F32)
        rgf = Res("gf")
        kb.dma("sp", gfb[:], gf_d.ap().partition_broadcast(128), cs, writes=[rgf])
        outr = Ring(kb, "yo", 2, lambda n: SB(es, n, [128, D], F32))
        STF = SB(es, "STF", [128, 4], F32)
        rSTF = Res("STF")
        osl = kb.slot("dq_out")
        outs = []
        for i in range(NSLOT):
            yo, ryo, _ = outr.next()
            kb.op("act", lambda e: e.activation(out=yo[:], in_=HR[:, i, :], func=AF.Square, accum_out=STF[:, 0:1]), reads=[rHR[i]], writes=[rSTF, ryo])
            kb.op("act", lambda e: e.activation(out=STF[:, 1:2], in_=STF[:, 0:1], func=AF.Ln, scale=1.0 / D, bias=epsb[:, 0:1]), reads=[rSTF, rC], writes=[rSTF])
            kb.op("act", lambda e: e.activation(out=STF[:, 2:3], in_=STF[:, 1:2], func=AF.Exp, scale=-0.5), reads=[rSTF], writes=[rSTF])
            kb.op("dve", lambda e: e.scalar_tensor_tensor(out=yo[:], in0=HR[:, i, :], scalar=STF[:, 2:3], in1=gfb[:], op0=ALU.mult, op1=ALU.mult),
                  reads=[rHR[i], rSTF, rgf], writes=[ryo])
            kb.dma("sp", y_d.ap()[i * 128:(i + 1) * 128, :], yo[:], osl, reads=[ryo])
            outs.append(ryo)
        kb.wait_all("sp", outs)
    es_moe.close()
    es_h2t.close()
    es_all.close()
    return nc


def _t5_bucket(n):
    n = np.maximum(n, 0)
    nf = np.maximum(n, 1).astype(np.float32)
    large = 16 + (np.log(nf / np.float32(16)) / np.float32(np.log(128 / 16)) * np.float32(16)).astype(np.int32)
    large = np.minimum(large, 31)
    return np.where(n < 16, n, large)


def _structural_tables(j):
    E = np.zeros((32, TABW), np.float32)
    for zi in range(5):
        z = zi - 1
        m = np.arange(255)
        n = (j - z) * 128 - 127 + m
        ok = n >= 0
        b = _t5_bucket(n)
        cols = zi * 256 + m
        E[b[ok], cols[ok]] += 1.0
        E[31, cols[ok]] -= 1.0
    return E


_NC_CACHE = {}


def kernel(x, c, w_ada, b_ada, norm1_g, w_in, rel_bias, conv_w, w_attn_branch, w_conv_branch, w_out, norm2_g,
           w_router_group, b_router_group, w_router_expert, b_router_expert, w_gate_e, w_up_e, w_down_e, norm_f_g):
    f = lambda a: np.ascontiguousarray(np.asarray(a, dtype=np.float32))
    x = f(x)
    c = f(c)
    shared = {
        "w_ada": f(w_ada)[0],
        "b_adaT": f(np.asarray(b_ada)[0].reshape(48, 128).T),
        "b_ada": f(b_ada)[0:1],
        "g1T": f(np.asarray(norm1_g)[0].reshape(8, 128).T),
        "g2T": f(np.asarray(norm2_g)[0].reshape(8, 128).T),
        "gf": f(norm_f_g).reshape(1, D),
        "w_in": f(w_in)[0],
        "relb": f(np.asarray(rel_bias)[:, [0, 2, 4, 6, 1, 3, 5, 7]]),
        "convw": f(np.asarray(conv_w)[0].reshape(3, 4, 128).transpose(2, 1, 0)),
        "w_ab": f(w_attn_branch)[0],
        "w_cbr": f(w_conv_branch)[0],
        "w_out": f(w_out)[0],
        "wr": f(np.concatenate([np.asarray(w_router_group)[0], np.asarray(w_router_expert)[0]], axis=1)),
        "br": f(np.concatenate([np.asarray(b_router_group)[0], np.asarray(b_router_expert)[0]])).reshape(1, 36),
        "wge": f(w_gate_e)[0],
        "wue": f(w_up_e)[0],
        "wde": f(w_down_e)[0],
    }
    in_maps = []
    for core in range(8):
        b, j = core // 4, core % 4
        xo = np.zeros((NSLOT, 130, D), np.float32)
        hm = np.ones((128, NSLOT), np.float32)
        for i in range(NSLOT):
            st = (4 * i + j) * 128
            if st == 0:
                xo[i, 2:] = x[b, 0:128]
                hm[:, i] = 0.0
            else:
                xo[i] = x[b, st - 2:st + 128]
        m = dict(shared)
        m["xb"] = x[b]
        m["xo"] = xo
        m["c8"] = f(c[b].reshape(8, 128).T)
        m["ej"] = _structural_tables(j)
        m["tq"] = (j * 128 + np.arange(128, dtype=np.float32)).reshape(128, 1)
        m["hmask"] = hm
        in_maps.append(m)
    if "nc" not in _NC_CACHE:
        _NC_CACHE["nc"] = build_nc()
    res = run_bass_kernel_spmd(_NC_CACHE["nc"], in_maps, core_ids=list(range(8)))
    out = np.empty((2, S, D), np.float32)
    for core in range(8):
        b, j = core // 4, core % 4
        y = res.results[core]["y"]
        for i in range(NSLOT):
            st = (4 * i + j) * 128
            out[b, st:st + 128] = y[i * 128:(i + 1) * 128]
    return out
```

```python
from contextlib import ExitStack
import numpy as np
import concourse.bass as bass
import concourse.mybir as mybir
from concourse.bass_utils import run_bass_kernel_spmd

F32 = mybir.dt.float32
BF16 = mybir.dt.bfloat16
ALU = mybir.AluOpType
AF = mybir.ActivationFunctionType
AX = mybir.AxisListType

D = 1024
S = 8192
NT = S // 128
NSLOT = 16
IN_COLS = 5704
C_Q, C_K, C_V, C_QI, C_KI, C_WI, C_CB, C_CC, C_CX, C_GL = 0, 512, 1024, 1536, 2048, 2112, 2120, 2632, 3144, 3656
EPS = 1e-6
NEG = -1.0e30
NIT = 18
TABW = 5 * 256
SEM_ROT = 30000


class Res:
    __slots__ = ("w", "r", "name")

    def __init__(self, name=""):
        self.w = None
        self.r = {}
        self.name = name


class Slot:
    def __init__(self, kb, name):
        self.kb = kb
        self.name = name
        self.sem = kb.nc.alloc_semaphore(name)
        self.val = 0
        kb.slots.append(self)

    def bump(self):
        if self.val + 16 > SEM_ROT:
            self.sem = self.kb.nc.alloc_semaphore(self.name + "_r%d" % self.kb.uid())
            self.val = 0
        self.val += 16
        return self.sem, self.val


class KB:
    def __init__(self, nc):
        self.nc = nc
        self.eng = {"pe": nc.tensor, "act": nc.scalar, "dve": nc.vector, "pool": nc.gpsimd, "sp": nc.sync}
        self._uid = 0
        self.sem = {k: nc.alloc_semaphore("s_" + k) for k in self.eng}
        self.cnt = {k: 0 for k in self.eng}
        self.seen = {k: {} for k in self.eng}
        self.nins = 0
        self.slots = []
        self.pool = {}
        self.pool_i = {}

    def barrier(self):
        evs = [(self.sem[k], self.cnt[k], k) for k in self.eng if self.cnt[k] > 0]
        evs += [(sl.sem, sl.val, "dma") for sl in self.slots if sl.val > 0]
        for e in self.eng:
            for sem, val, src in evs:
                if self.seen[e].get(sem.num, 0) >= val:
                    continue
                self.eng[e].wait_ge(sem, val)
                self.seen[e][sem.num] = val

    def uid(self):
        self._uid += 1
        return self._uid

    def _wait(self, e, ev):
        sem, val, src = ev
        if src == "pe" and e == "pe":
            return
        if self.seen[e].get(sem.num, 0) >= val:
            return
        self.eng[e].wait_ge(sem, val)
        self.seen[e][sem.num] = val

    def deps(self, e, reads, writes):
        for r in reads:
            if r.w is not None:
                self._wait(e, r.w)
        for w in writes:
            if w.w is not None:
                self._wait(e, w.w)
            for ev in w.r.values():
                self._wait(e, ev)

    def _record(self, ev, reads, writes):
        for r in reads:
            r.r[ev[0].num] = ev
        for w in writes:
            w.w = ev
            w.r = {}

    def op(self, e, fn, reads=(), writes=()):
        self.deps(e, reads, writes)
        ins = fn(self.eng[e])
        if self.cnt[e] + 1 > SEM_ROT:
            self.sem[e] = self.nc.alloc_semaphore("s_%s_r%d" % (e, self.uid()))
            self.cnt[e] = 0
        self.cnt[e] += 1
        ins.then_inc(self.sem[e], 1)
        self.nins += 1
        ev = (self.sem[e], self.cnt[e], e)
        self._record(ev, reads, writes)
        return ev

    def dma(self, q, out, in_, slot, reads=(), writes=(), **kw):
        self.deps(q, reads, writes)
        if slot is None:
            if q not in self.pool:
                self.pool[q] = [Slot(self, "dq_%s%d" % (q, k)) for k in range(16)]
                self.pool_i[q] = 0
            slot = self.pool[q][self.pool_i[q] % 16]
            self.pool_i[q] += 1
            if slot.val > 0:
                self._wait(q, (slot.sem, slot.val, "dma"))
        ins = self.eng[q].dma_start(out=out, in_=in_, **kw)
        sem, val = slot.bump()
        ins.then_inc(sem, 16)
        self.nins += 1
        ev = (sem, val, "dma")
        self._record(ev, reads, writes)
        return ev

    def slot(self, name):
        return None

    def wait_all(self, e, resources):
        for r in resources:
            if r.w is not None:
                self._wait(e, r.w)
            for ev in r.r.values():
                self._wait(e, ev)


class Ring:
    def __init__(self, kb, name, n, make):
        self.items = []
        for i in range(n):
            self.items.append((make("%s%d" % (name, i)), Res("%s%d" % (name, i)), LazySlot(kb, "dq_%s%d" % (name, i))))
        self.i = 0

    def next(self):
        it = self.items[self.i % len(self.items)]
        self.i += 1
        return it


class LazySlot:
    def __init__(self, kb, name):
        self.kb = kb
        self.name = name
        self.s = None

    def bump(self):
        if self.s is None:
            self.s = Slot(self.kb, self.name)
        return self.s.bump()


def build_nc(stop=None):
    nc = bass.Bass("TRN2", target_bir_lowering=False)
    kb = KB(nc)
    dbg_row = [0]

    class Scope(ExitStack):
        def __exit__(self, *a):
            r = super().__exit__(*a)
            if a[0] is None:
                kb.barrier()
            return r

    def finish(items):
        sl = kb.slot("dq_dbg")
        rs = []
        for ap, res, p, n in items:
            r0 = dbg_row[0]
            kb.dma("pool", y_d.ap()[r0:r0 + p, 0:n], ap, sl, reads=[res])
            dbg_row[0] += 128
            rs.append(res)
        kb.wait_all("pool", rs)
        return nc

    def din(name, shape):
        return nc.dram_tensor(name, list(shape), F32, kind="ExternalInput")

    xb_d = din("xb", [S, D])
    xo_d = din("xo", [NSLOT, 130, D])
    c8_d = din("c8", [128, 8])
    wada_d = din("w_ada", [D, 6 * D])
    badaT_d = din("b_adaT", [128, 48])
    bada_d = din("b_ada", [1, 6 * D])
    g1T_d = din("g1T", [128, 8])
    g2T_d = din("g2T", [128, 8])
    gf_d = din("gf", [1, D])
    win_d = din("w_in", [D, IN_COLS])
    relb_d = din("relb", [32, 8])
    ej_d = din("ej", [32, TABW])
    convw_d = din("convw", [128, 4, 3])
    wab_d = din("w_ab", [512, D])
    wcbr_d = din("w_cbr", [512, D])
    wout_d = din("w_out", [D, D])
    wr_d = din("wr", [D, 36])
    br_d = din("br", [1, 36])
    wge_d = din("wge", [32, D, 512])
    wue_d = din("wue", [32, D, 512])
    wde_d = din("wde", [32, 512, D])
    tq_d = din("tq", [128, 1])
    hmask_d = din("hmask", [128, NSLOT])
    y_d = nc.dram_tensor("y", [NSLOT * 128, D], F32, kind="ExternalOutput")
    scr_tab = nc.dram_tensor("scr_tab", [8, TABW], F32, kind="Internal")
    scr_q = nc.dram_tensor("scr_q", [NSLOT, 128, 4 * 128], BF16, kind="Internal")
    scr_qi = nc.dram_tensor("scr_qi", [NSLOT, 128, 4 * 128], BF16, kind="Internal")
    scr_o = nc.dram_tensor("scr_o", [NSLOT, 128, 512], BF16, kind="Internal")
    r_scr_tab, r_scr_q, r_scr_qi, r_scr_o = Res(), Res(), Res(), Res()

    win_v = win_d.ap().rearrange("(k p) n -> p k n", p=128)

    PB = [nc.alloc_psum_tensor("pb%d" % i, [128, 512], F32) for i in range(8)]
    rPB = [Res("pb%d" % i) for i in range(8)]

    def pbf(i):
        return PB[i][:].bitcast(BF16)

    es_all = ExitStack()

    def SB(es, name, shape, dt):
        return es.enter_context(nc.sbuf_tensor("sb_" + name, list(shape), dt))

    ID32 = SB(es_all, "ID32", [128, 128], F32)
    ID = SB(es_all, "ID", [128, 128], BF16)
    JJ = SB(es_all, "JJ", [128, 128], F32)
    JK = SB(es_all, "JK", [128, 8], F32)
    junk_ap = JK[:].ap
    rJK = Res("junk")

    def junk(p, n, col=0):
        return bass.AP(JK, col, [[junk_ap[0][0], p], [0, n]])

    modT = SB(es_all, "modT", [128, 48], F32)
    s1 = SB(es_all, "s1", [128, 8], F32)
    s2 = SB(es_all, "s2", [128, 8], F32)
    cact = SB(es_all, "cact", [128, 8], F32)
    epsb = SB(es_all, "epsb", [128, 1], F32)
    tq = SB(es_all, "tq", [128, 1], F32)
    hmask = SB(es_all, "hmask", [128, NSLOT], F32)
    wi_all = SB(es_all, "wi_all", [128, NSLOT, 8], F32)
    sg_all = SB(es_all, "sg_all", [128, NSLOT, 8], F32)
    rC = Res("consts")
    rMOD = Res("mod")
    cs = kb.slot("dq_const")

    kb.op("pool", lambda e: e.memset(ID32[:], 0.0), writes=[rC])
    kb.op("pool", lambda e: e.affine_select(out=ID32[:], in_=ID32[:], compare_op=ALU.not_equal, fill=1.0, base=0,
                                            pattern=[[-1, 128]], channel_multiplier=1), reads=[rC], writes=[rC])
    kb.op("pool", lambda e: e.memset(JJ[:], 0.0), reads=[rC], writes=[rC])
    kb.op("pool", lambda e: e.affine_select(out=JJ[:], in_=JJ[:], compare_op=ALU.not_equal, fill=1.0, base=-127,
                                            pattern=[[1, 128]], channel_multiplier=1), reads=[rC], writes=[rC])
    kb.op("dve", lambda e: e.tensor_copy(out=ID[:], in_=ID32[:]), reads=[rC], writes=[rC])
    kb.op("pool", lambda e: e.memset(epsb[:], EPS), reads=[rC], writes=[rC])
    kb.dma("sp", tq[:], tq_d.ap(), cs, writes=[rC])
    kb.dma("sp", hmask[:], hmask_d.ap(), cs, writes=[rC])
    kb.dma("sp", cact[:], c8_d.ap(), cs, writes=[rC])
    kb.dma("sp", modT[:], badaT_d.ap(), cs, writes=[rMOD])
    kb.dma("sp", s1[:], g1T_d.ap(), cs, writes=[rMOD])
    kb.dma("sp", s2[:], g2T_d.ap(), cs, writes=[rMOD])
    kb.op("act", lambda e: e.activation(out=cact[:], in_=cact[:], func=AF.Silu), reads=[rC], writes=[rC])

    if stop == "c0":
        return finish([(cact[:], rC, 128, 8), (ID32[:], rC, 128, 128), (JJ[:], rC, 128, 128), (modT[:], rMOD, 128, 48)])
    wada_v = wada_d.ap().rearrange("(k p) n -> k p n", p=128)

    def gate_bc(gb, rgb, part):
        kb.barrier()
        with Scope() as es:
            wa = Ring(kb, "wg%d" % part, 2, lambda n: SB(es, n, [128, D], F32))
            cbr = Ring(kb, "cb%d" % part, 2, lambda n: SB(es, n, [128, 128], F32))
            kb.dma("sp", gb[:], bada_d.ap()[0:1, part * D:(part + 1) * D].partition_broadcast(128), cs, writes=[rgb])
            for k in range(8):
                wt, rw, sl = wa.next()
                kb.dma("sp", wt[:], wada_v[k][:, part * D:(part + 1) * D], sl, writes=[rw])
                cbk, rcbk, _ = cbr.next()
                kb.op("dve", lambda e: e.tensor_copy(out=cbk[:], in_=cact[:, k:k + 1].to_broadcast([128, 128])), reads=[rC], writes=[rcbk])
                for n in range(2):
                    kb.op("pe", lambda e: e.matmul(PB[n][:], lhsT=cbk[:], rhs=wt[:, n * 512:(n + 1) * 512], start=(k == 0), stop=(k == 7)),
                          reads=[rw, rcbk], writes=[rPB[n]])
            for n in range(2):
                kb.op("dve", lambda e: e.tensor_tensor(out=gb[:, n * 512:(n + 1) * 512], in0=gb[:, n * 512:(n + 1) * 512], in1=PB[n][:], op=ALU.add),
                      reads=[rPB[n], rgb], writes=[rgb])

    kb.barrier()
    with Scope() as es:
        gtmp = SB(es, "gtmp", [128, D], F32)
        dtmp = SB(es, "dtmp", [128, 8, 128], F32)
        rgt = Res("gtmp")
        for part in (0, 1, 3, 4):
            gate_bc(gtmp, rgt, part)
            kb.op("dve", lambda e: e.tensor_tensor(out=dtmp[:], in0=gtmp[:].rearrange("p (c q) -> p c q", c=8),
                                                   in1=ID32[:].unsqueeze(1).to_broadcast([128, 8, 128]), op=ALU.mult), reads=[rgt, rC], writes=[rgt])
            kb.op("dve", lambda e: e.tensor_reduce(out=modT[:, part * 8:(part + 1) * 8], in_=dtmp[:], axis=AX.X, op=ALU.add), reads=[rgt], writes=[rMOD])
        kb.op("dve", lambda e: e.scalar_tensor_tensor(out=s1[:], in0=modT[:, 8:16], scalar=1.0, in1=s1[:], op0=ALU.add, op1=ALU.mult),
              reads=[rMOD], writes=[rMOD])
        kb.op("dve", lambda e: e.scalar_tensor_tensor(out=s2[:], in0=modT[:, 32:40], scalar=1.0, in1=s2[:], op0=ALU.add, op1=ALU.mult),
              reads=[rMOD], writes=[rMOD])
    if stop == "p0":
        return finish([(modT[:], rMOD, 128, 48), (s1[:], rMOD, 128, 8), (s2[:], rMOD, 128, 8), (cact[:], rC, 128, 8)])
    sh1 = modT[:, 0:8]
    sh2 = modT[:, 24:32]

    def norm_stats(X, rX, p, xn, rxn, st, rst, on_pool=False):
        kb.op("act", lambda e: e.activation(out=xn[0:p, :], in_=X, func=AF.Square, accum_out=st[0:p, 0:1]),
              reads=[rX], writes=[rst, rxn])
        kb.op("act", lambda e: e.activation(out=st[0:p, 1:2], in_=st[0:p, 0:1], func=AF.Ln, scale=1.0 / D, bias=epsb[0:p, 0:1]), reads=[rst, rC], writes=[rst])
        kb.op("act", lambda e: e.activation(out=st[0:p, 2:3], in_=st[0:p, 1:2], func=AF.Exp, scale=-0.5), reads=[rst], writes=[rst])
        kb.op("dve", lambda e: e.tensor_scalar(out=xn[0:p, :], in0=X, scalar1=st[0:p, 2:3], scalar2=None, op0=ALU.mult),
              reads=[rX, rst], writes=[rxn])

    def norm_T(X, rX, p, xn, rxn, st, rst, hT_out, rhT, sc, sh, pbank, on_pool=False):
        norm_stats(X, rX, p, xn, rxn, st, rst, on_pool)
        norm_tr(xn, rxn, p, hT_out, rhT, sc, sh, pbank)

    def norm_tr(xn, rxn, p, hT_out, rhT, sc, sh, pbank):
        for c in range(8):
            if p == 128:
                kb.op("pe", lambda e: e.transpose(out=pbf(pbank)[:, c * 128:(c + 1) * 128], in_=xn[:, c * 128:(c + 1) * 128], identity=ID[:]),
                      reads=[rxn, rC], writes=[rPB[pbank]])
        for c in range(8):
            src = pbf(pbank)[:, c * 128:(c + 1) * 128]
            if c % 2 == 0:
                kb.op("act", lambda e: e.activation(out=hT_out(c), in_=src, func=AF.Identity, scale=sc[:, c:c + 1], bias=sh[:, c:c + 1]),
                      reads=[rPB[pbank], rMOD], writes=[rhT])
            else:
                kb.op("dve", lambda e: e.tensor_scalar(out=hT_out(c), in0=src, scalar1=sc[:, c:c + 1], scalar2=sh[:, c:c + 1], op0=ALU.mult, op1=ALU.add),
                      reads=[rPB[pbank], rMOD], writes=[rhT])

    TAB = SB(es_all, "TAB", [128, 5, 8, 128], BF16)
    rTAB = Res("TAB")
    PEN = SB(es_all, "PEN", [128, 512], BF16)
    kb.barrier()
    with Scope() as es2:
        relb = SB(es2, "relb", [32, 8], F32)
        ej = SB(es2, "ej", [32, TABW], F32)
        rtab = SB(es2, "rtab", [8, TABW], F32)
        XZ = SB(es2, "XZ", [128, 8, 128], F32)
        rT0, rXZ = Res(), Res()
        ts_ = kb.slot("dq_tab")
        kb.dma("sp", relb[:], relb_d.ap(), ts_, writes=[rT0])
        kb.dma("sp", ej[:], ej_d.ap(), ts_, writes=[rT0])
        for c0 in range(0, TABW, 512):
            w = min(512, TABW - c0)
            kb.op("pe", lambda e: e.matmul(PB[0][0:8, 0:w], lhsT=relb[:], rhs=ej[:, c0:c0 + w], start=True, stop=True),
                  reads=[rT0], writes=[rPB[0]])
            kb.op("act", lambda e: e.activation(out=rtab[:, c0:c0 + w], in_=PB[0][0:8, 0:w], func=AF.Exp), reads=[rPB[0]], writes=[rT0])
        kb.dma("sp", scr_tab.ap(), rtab[:], ts_, reads=[rT0], writes=[r_scr_tab])
        for zi in range(5):
            toe = bass.AP(scr_tab, zi * 256, [[1, 128], [TABW, 8], [1, 128]])
            kb.dma("sp", XZ[:], toe, ts_, reads=[r_scr_tab], writes=[rXZ])
            for n in range(2):
                kb.op("pe", lambda e: e.matmul(PB[1 + n][:], lhsT=JJ[:], rhs=XZ[:].rearrange("p h q -> p (h q)")[:, n * 512:(n + 1) * 512], start=True, stop=True),
                      reads=[rXZ, rC], writes=[rPB[1 + n]])
                kb.op("dve", lambda e: e.tensor_copy(out=TAB[:, zi, n * 4:(n + 1) * 4, :], in_=PB[1 + n][:].rearrange("p (h q) -> p h q", h=4)),
                      reads=[rPB[1 + n]], writes=[rTAB])
        IOT = SB(es2, "IOT", [128, 512], F32)
        kb.op("pool", lambda e: e.iota(IOT[:], pattern=[[1, 512]], base=0, channel_multiplier=0, allow_small_or_imprecise_dtypes=True), writes=[rTAB])
        kb.op("dve", lambda e: e.tensor_scalar(out=PEN[:], in0=IOT[:], scalar1=tq[:, 0:1], scalar2=NEG, op0=ALU.is_gt, op1=ALU.mult),
              reads=[rTAB, rC], writes=[rTAB])


    kb.barrier()
    es_att = ExitStack()
    KT = SB(es_att, "KT", [128, 4, S], BF16)
    V = SB(es_att, "V", [128, NT, 8, 65], BF16)
    KI = SB(es_att, "KI", [128, S], BF16)
    rKT, rV, rKI = Res("KT"), Res("V"), Res("KI")
    kb.op("pool", lambda e: e.memset(V[:, :, :, 64:65], 1.0), writes=[rV])

    kb.barrier()
    with Scope() as es:
        Wk = SB(es, "Wk", [128, 8, 512], BF16)
        Wv = SB(es, "Wv", [128, 8, 512], BF16)
        Wki = SB(es, "Wki", [128, 8, 128], BF16)
        rW = Res("W1a")
        ws = kb.slot("dq_w1a")
        kb.dma("pool", Wk[:], win_v[:, :, C_K:C_K + 512], ws, writes=[rW])
        kb.dma("pool", Wv[:], win_v[:, :, C_V:C_V + 512], ws, writes=[rW])
        kb.dma("pool", Wki[:, :, 0:64], win_v[:, :, C_KI:C_KI + 64], ws, writes=[rW])
        kb.dma("pool", Wki[:, :, 64:128], win_v[:, :, C_KI:C_KI + 64], ws, writes=[rW])
        xr = Ring(kb, "x", 2, lambda n: SB(es, n, [128, D], F32))
        xnr = Ring(kb, "xn", 2, lambda n: SB(es, n, [128, D], BF16))
        str_ = Ring(kb, "st", 2, lambda n: SB(es, n, [128, 4], F32))
        h4r = Ring(kb, "h4", 2, lambda n: SB(es, n, [128, 8, 512], BF16))
        xb_v = xb_d.ap().rearrange("(t p) n -> t p n", p=128)
        ev_i = 0
        pre = {}

        def do_stats(t):
            X, rX, sl = xr.next()
            kb.dma("sp", X[:], xb_v[t], sl, writes=[rX])
            xn, rxn, _ = xnr.next()
            st, rst, _ = str_.next()
            norm_stats(X[:], rX, 128, xn, rxn, st, rst, on_pool=(t % 2 == 1))
            pre[t] = (xn, rxn)

        do_stats(0)
        for g in range(NT // 4):
            h4, rh4, _ = h4r.next()
            for tt in range(4):
                t = g * 4 + tt
                if t + 1 < NT:
                    do_stats(t + 1)
                xn, rxn = pre.pop(t)
                norm_tr(xn, rxn, 128, lambda c: h4[:, c, tt * 128:(tt + 1) * 128], rh4, s1, sh1, 6 + (t % 2))
            for fc in range(5):
                bank = fc % 4
                for kc in range(8):
                    lw = Wk[:, kc, fc * 128:(fc + 1) * 128] if fc < 4 else Wki[:, kc, :]
                    kb.op("pe", lambda e: e.matmul(PB[bank][:], lhsT=lw, rhs=h4[:, kc, :], start=(kc == 0), stop=(kc == 7)),
                          reads=[rW, rh4], writes=[rPB[bank]])
                dst = KT[:, fc, g * 512:(g + 1) * 512] if fc < 4 else KI[:, g * 512:(g + 1) * 512]
                rd = rKT if fc < 4 else rKI
                if ev_i % 2 == 0:
                    kb.op("act", lambda e: e.copy(out=dst, in_=PB[bank][:]), reads=[rPB[bank]], writes=[rd])
                else:
                    kb.op("dve", lambda e: e.tensor_copy(out=dst, in_=PB[bank][:]), reads=[rPB[bank]], writes=[rd])
                ev_i += 1
            for tt in range(4):
                t = g * 4 + tt
                bank = 4 + (tt % 2)
                for kc in range(8):
                    kb.op("pe", lambda e: e.matmul(PB[bank][:], lhsT=h4[:, kc, tt * 128:(tt + 1) * 128], rhs=Wv[:, kc, :],
                                                   start=(kc == 0), stop=(kc == 7)), reads=[rW, rh4], writes=[rPB[bank]])
                src = PB[bank][:].rearrange("p (h d) -> p h d", h=8)
                if ev_i % 2 == 0:
                    kb.op("act", lambda e: e.copy(out=V[:, t, :, 0:64], in_=src), reads=[rPB[bank]], writes=[rV])
                else:
                    kb.op("dve", lambda e: e.tensor_copy(out=V[:, t, :, 0:64], in_=src), reads=[rPB[bank]], writes=[rV])
                ev_i += 1

    if stop == "1a":
        return finish([(TAB[:, 1, 0, :], rTAB, 128, 128), (TAB[:, 0, 3, :], rTAB, 128, 128), (PEN[:], rTAB, 128, 512),
                       (KT[:, 0, 0:1024], rKT, 128, 1024), (KT[:, 3, 7168:8192], rKT, 128, 1024),
                       (V[:, 5, :, :].rearrange("p h d -> p (h d)"), rV, 128, 520), (KI[:, 0:1024], rKI, 128, 1024)])
    kb.barrier()
    with Scope() as es:
        Wq = SB(es, "Wq", [128, 8, 512], BF16)
        Wqi = SB(es, "Wqi", [128, 8, 512], BF16)
        Wwi = SB(es, "Wwi", [128, 8, 8], BF16)
        rW = Res("W1b")
        ws = kb.slot("dq_w1b")
        kb.dma("pool", Wq[:], win_v[:, :, C_Q:C_Q + 512], ws, writes=[rW])
        kb.dma("pool", Wqi[:], win_v[:, :, C_QI:C_QI + 512], ws, writes=[rW])
        kb.dma("pool", Wwi[:], win_v[:, :, C_WI:C_WI + 8], ws, writes=[rW])
        xr = Ring(kb, "xo", 2, lambda n: SB(es, n, [128, D], F32))
        xnr = Ring(kb, "xno", 2, lambda n: SB(es, n, [128, D], BF16))
        str_ = Ring(kb, "sto", 2, lambda n: SB(es, n, [128, 4], F32))
        htr = Ring(kb, "hto", 2, lambda n: SB(es, n, [128, 8, 128], BF16))
        qsr = Ring(kb, "qs", 2, lambda n: SB(es, n, [128, 2, 512], BF16))
        ss = kb.slot("dq_spill")
        pre1b = {}

        def stats1b(i):
            X, rX, sl = xr.next()
            kb.dma("sp", X[:], xo_d.ap()[i, 2:130, :], sl, writes=[rX])
            xn, rxn, _ = xnr.next()
            st, rst, _ = str_.next()
            norm_stats(X[:], rX, 128, xn, rxn, st, rst)
            pre1b[i] = (xn, rxn)

        stats1b(0)
        for i in range(NSLOT):
            if i + 1 < NSLOT:
                stats1b(i + 1)
            xn, rxn = pre1b.pop(i)
            hT, rhT, _ = htr.next()
            norm_tr(xn, rxn, 128, lambda c: hT[:, c, :], rhT, s1, sh1, 6 + (i % 2))
            qs, rqs, _ = qsr.next()
            for wi_, (Wt, scl) in enumerate(((Wq, 0.125), (Wqi, 1.0))):
                bank = wi_
                for fc in range(4):
                    for kc in range(8):
                        kb.op("pe", lambda e: e.matmul(PB[bank][:, fc * 128:(fc + 1) * 128], lhsT=Wt[:, kc, fc * 128:(fc + 1) * 128],
                                                       rhs=hT[:, kc, :], start=(kc == 0), stop=(kc == 7)), reads=[rW, rhT], writes=[rPB[bank]])
                kb.op("act", lambda e: e.activation(out=qs[:, wi_, :], in_=PB[bank][:], func=AF.Copy, scale=scl),
                      reads=[rPB[bank]], writes=[rqs])
            kb.dma("sp", scr_q.ap()[i], qs[:, 0, :], ss, reads=[rqs], writes=[r_scr_q])
            kb.dma("sp", scr_qi.ap()[i], qs[:, 1, :], ss, reads=[rqs], writes=[r_scr_qi])
            for kc in range(8):
                kb.op("pe", lambda e: e.matmul(PB[2][:, 0:8], lhsT=hT[:, kc, :], rhs=Wwi[:, kc, :], start=(kc == 0), stop=(kc == 7)),
                      reads=[rW, rhT], writes=[rPB[2]])
            kb.op("act", lambda e: e.activation(out=wi_all[:, i, :], in_=PB[2][:, 0:8], func=AF.Abs, scale=0.125 * (8 ** -0.5)),
                  reads=[rPB[2]], writes=[rC])
            kb.op("dve", lambda e: e.tensor_scalar(out=sg_all[:, i, :], in0=PB[2][:, 0:8], scalar1=0.0, scalar2=2.0,
                                                   op0=ALU.is_ge, op1=ALU.mult), reads=[rPB[2]], writes=[rC])
            kb.op("dve", lambda e: e.tensor_scalar(out=sg_all[:, i, :], in0=sg_all[:, i, :], scalar1=-1.0, scalar2=None, op0=ALU.add),
                  reads=[rC], writes=[rC])

    if stop == "1b":
        return finish([(wi_all[:, 1, :], rC, 128, 8), (sg_all[:, 1, :], rC, 128, 8)])
    kb.barrier()
    with Scope() as es:
        SC = SB(es, "SC", [128, S], F32)
        rSC = Res("SC")
        qTr = Ring(kb, "qT", 1, lambda n: SB(es, n, [128, 4, 128], BF16))
        qiTr = Ring(kb, "qiT", 1, lambda n: SB(es, n, [128, 4, 128], BF16))
        DG = SB(es, "DG", [128, 8, 128], BF16)
        rDG = Res("DG")
        yr = Ring(kb, "yh", 2, lambda n: SB(es, n, [128, 512], BF16))
        BS = SB(es, "BS", [128, 16], F32)
        rBS = Res("BS")
        THD = SB(es, "THD", [128, 128], F32)
        THB = SB(es, "THB", [128, 128], F32)
        ONES = SB(es, "ONES", [128, 128], F32)
        rTH = Res("TH")
        kb.op("pool", lambda e: e.memset(ONES[:], 1.0), writes=[rTH])
        THR = SB(es, "THR", [128, 1], F32)
        BA = SB(es, "BA", [128, 16], F32)
        rTHR, rBA = Res("THR"), Res("BA")
        P2 = SB(es, "P2", [128, NIT + 2], F32)
        RN = SB(es, "RN", [128, NIT + 2], F32)
        for m_ in range(NIT + 2):
            kb.op("pool", lambda e: e.memset(P2[:, m_:m_ + 1], 2.0 ** -m_), writes=[rTH])
        ZR = SB(es, "ZR", [128, 260], BF16)
        kb.op("pool", lambda e: e.memset(ZR[:], 0.0), writes=[rTH])
        selr = Ring(kb, "sel", 3, lambda n: SB(es, n, [128, 128], BF16))
        EB = SB(es, "EB", [128, 2, 8, 128], BF16)
        rEB = [Res("E0"), Res("E1")]
        CJ = EB[:].rearrange("p a h q -> p (a h q)").bitcast(mybir.dt.uint8)

        class _ER:
            n = 0

            def next(self):
                k = self.n % 2
                self.n += 1
                return EB[:, k], rEB[k], None
        Er = _ER()
        OS = SB(es, "OS", [128, 8, 64], BF16)
        RS = SB(es, "RS", [128, 8], F32)
        rOS = Res("OS")

        for i in range(NSLOT):
            nch = i + 1
            Sc = nch * 512
            ntile = 4 * nch
            qT, rqT, sl1 = qTr.next()
            qiT, rqiT, sl2 = qiTr.next()
            kb.dma("sp", qT[:], scr_q.ap()[i].rearrange("p (c q) -> p c q", c=4), sl1, reads=[r_scr_q], writes=[rqT])
            kb.dma("sp", qiT[:], scr_qi.ap()[i].rearrange("p (c q) -> p c q", c=4), sl2, reads=[r_scr_qi], writes=[rqiT])
            for h in range(8):
                kb.op("pool", lambda e: e.tensor_scalar(out=DG[:, h, :], in0=ID[:], scalar1=sg_all[:, i, h:h + 1], scalar2=None, op0=ALU.mult),
                      reads=[rC], writes=[rDG])
            def dots_mm(ch, h):
                bank = h % 4
                hp = (h % 2) * 64
                kb.op("pe", lambda e: e.matmul(PB[bank][:], lhsT=qiT[hp:hp + 64, h // 2, :], rhs=KI[hp:hp + 64, ch * 512:(ch + 1) * 512],
                                               start=True, stop=True), reads=[rqiT, rKI], writes=[rPB[bank]])

            for ch in range(nch):
                ab = 6 + ch % 2
                dots_mm(ch, 0)
                dots_mm(ch, 1)
                for h in range(8):
                    bank = h % 4
                    y, ry, _ = yr.next()
                    if h % 2 == 0:
                        kb.op("act", lambda e: e.activation(out=y[:], in_=PB[bank][:], func=AF.Relu, scale=wi_all[:, i, h:h + 1]),
                              reads=[rPB[bank], rC], writes=[ry])
                    else:
                        kb.op("dve", lambda e: e.tensor_scalar(out=y[:], in0=PB[bank][:], scalar1=0.0, scalar2=wi_all[:, i, h:h + 1],
                                                               op0=ALU.max, op1=ALU.mult), reads=[rPB[bank], rC], writes=[ry])
                    if h + 2 < 8:
                        dots_mm(ch, h + 2)
                    kb.op("pe", lambda e: e.matmul(PB[ab][:], lhsT=DG[:, h, :], rhs=y[:], start=(h == 0), stop=(h == 7)),
                          reads=[rDG, ry], writes=[rPB[ab]])
                if ch == nch - 1:
                    kb.op("dve", lambda e: e.tensor_reduce(out=BS[:, 2:3], in_=PB[ab][:], axis=AX.X, op=ALU.min), reads=[rPB[ab]], writes=[rBS])
                    kb.op("dve", lambda e: e.tensor_tensor(out=SC[:, ch * 512:(ch + 1) * 512], in0=PB[ab][:], in1=PEN[:], op=ALU.add),
                          reads=[rPB[ab], rTAB], writes=[rSC])
                else:
                    kb.op("act", lambda e: e.copy(out=SC[:, ch * 512:(ch + 1) * 512], in_=PB[ab][:]), reads=[rPB[ab]], writes=[rSC])
            if stop == "2a":
                return finish([(SC[:, 0:512], rSC, 128, 512), (BS[:], rBS, 128, 16)])
            segs = [(a_, min(a_ + 4096, Sc)) for a_ in range(0, Sc, 4096)]
            for si, (a_, b_) in enumerate(segs):
                kb.op("dve", lambda e: e.tensor_reduce(out=BS[:, 0:1] if si == 0 else BS[:, 8:9], in_=SC[:, a_:b_], axis=AX.X, op=ALU.max), reads=[rSC], writes=[rBS])
                if si > 0:
                    kb.op("dve", lambda e: e.tensor_tensor(out=BS[:, 0:1], in0=BS[:, 0:1], in1=BS[:, 8:9], op=ALU.max), reads=[rBS], writes=[rBS])
            if nch > 1:
                fsegs = [(a_, min(a_ + 4096, Sc - 512)) for a_ in range(0, Sc - 512, 4096)]
                for (a_, b_) in fsegs:
                    kb.op("dve", lambda e: e.tensor_reduce(out=BS[:, 7:8], in_=SC[:, a_:b_], axis=AX.X, op=ALU.min), reads=[rSC], writes=[rBS])
                    kb.op("dve", lambda e: e.tensor_tensor(out=BS[:, 2:3], in0=BS[:, 2:3], in1=BS[:, 7:8], op=ALU.min), reads=[rBS], writes=[rBS])
            kb.op("dve", lambda e: e.tensor_tensor(out=BS[:, 3:4], in0=BS[:, 0:1], in1=BS[:, 2:3], op=ALU.subtract), reads=[rBS], writes=[rBS])
            kb.op("dve", lambda e: e.tensor_scalar(out=BS[:, 3:4], in0=BS[:, 3:4], scalar1=1.001, scalar2=1e-6, op0=ALU.mult, op1=ALU.add),
                  reads=[rBS], writes=[rBS])
            kb.op("dve", lambda e: e.tensor_scalar(out=RN[:], in0=P2[:], scalar1=BS[:, 3:4], scalar2=None, op0=ALU.mult), reads=[rBS, rTH], writes=[rBS])
            kb.op("dve", lambda e: e.scalar_tensor_tensor(out=THR[:], in0=BS[:, 3:4], scalar=-0.5, in1=BS[:, 0:1], op0=ALU.mult, op1=ALU.add),
                  reads=[rBS], writes=[rTHR])
            use_act = Sc >= 1536
            Sd = (int(round(0.64 * Sc / 128)) * 128) if use_act else Sc
            dsegs = [(a_, min(a_ + 4096, Sd)) for a_ in range(0, Sd, 4096)]
            asegs = [(a_, min(a_ + 512, Sc)) for a_ in range(Sd, Sc, 512)] if use_act else []
            n_act = Sc - Sd
            for it in range(NIT):
                for k_, (a_, b_) in enumerate(asegs):
                    kb.op("act", lambda e: e.activation(out=PB[k_ % 4][:, 0:b_ - a_], in_=SC[:, a_:b_], func=AF.Sign, scale=-1.0, bias=THR[:, 0:1],
                                                        accum_out=BA[:, k_:k_ + 1]), reads=[rSC, rTHR], writes=[rBA, rPB[k_ % 4]])
                for si, (a_, b_) in enumerate(dsegs):
                    kb.op("dve", lambda e: e.tensor_scalar(out=CJ[:, 0:b_ - a_], in0=SC[:, a_:b_], scalar1=THR[:, 0:1], scalar2=0.0,
                                                           op0=ALU.is_ge, op1=ALU.add, accum_out=BS[:, 5:6] if si == 0 else BS[:, 10:11]),
                          reads=[rSC, rTHR], writes=[rBS, rEB[0], rEB[1]])
                    if si > 0:
                        kb.op("dve", lambda e: e.tensor_tensor(out=BS[:, 5:6], in0=BS[:, 5:6], in1=BS[:, 10:11], op=ALU.add), reads=[rBS], writes=[rBS])
                if use_act:
                    if len(asegs) > 1:
                        kb.op("dve", lambda e: e.tensor_reduce(out=BS[:, 9:10], in_=BA[:, 0:len(asegs)], axis=AX.X, op=ALU.add), reads=[rBA], writes=[rBS])
                        sa = BS[:, 9:10]
                    else:
                        sa = BA[:, 0:1]
                    kb.op("dve", lambda e: e.scalar_tensor_tensor(out=BS[:, 5:6], in0=sa, scalar=-0.5, in1=BS[:, 5:6], op0=ALU.mult, op1=ALU.add),
                          reads=[rBS, rBA], writes=[rBS])
                kb.op("dve", lambda e: e.tensor_scalar(out=BS[:, 6:7], in0=BS[:, 5:6], scalar1=255.5 - 0.5 * n_act, scalar2=-0.5, op0=ALU.is_ge, op1=ALU.add),
                      reads=[rBS], writes=[rBS])
                kb.op("dve", lambda e: e.scalar_tensor_tensor(out=THR[:], in0=BS[:, 6:7], scalar=RN[:, it + 1:it + 2], in1=THR[:], op0=ALU.mult, op1=ALU.add),
                      reads=[rBS, rTHR], writes=[rTHR])
            kb.op("dve", lambda e: e.tensor_tensor(out=BS[:, 1:2], in0=THR[:], in1=RN[:, NIT + 1:NIT + 2], op=ALU.subtract), reads=[rBS, rTHR], writes=[rBS])
            if stop == "2b":
                return finish([(SC[:, 0:512], rSC, 128, 512), (BS[:], rBS, 128, 16)])
            kb.op("dve", lambda e: e.tensor_scalar(out=THD[:], in0=ID32[:], scalar1=BS[:, 1:2], scalar2=None, op0=ALU.mult),
                  reads=[rBS, rC], writes=[rTH])
            kb.op("pe", lambda e: e.matmul(PB[7][:, 0:128], lhsT=ONES[:], rhs=THD[:], start=True, stop=True), reads=[rTH], writes=[rPB[7]])
            kb.op("act", lambda e: e.copy(out=THB[:], in_=PB[7][:, 0:128]), reads=[rPB[7]], writes=[rTH])
            for hh in range(2):
                kb.op("pe", lambda e: e.matmul(PB[4 + hh][:, 0:260], lhsT=ID[:], rhs=ZR[:], start=True, stop=False, skip_group_check=True),
                      reads=[rC, rTH], writes=[rPB[4 + hh]])
            st_sel = {}

            def stage_a(kt):
                sq = (kt % 4) * 128
                kb.op("pe", lambda e: e.transpose(out=PB[6][:, sq:sq + 128], in_=SC[:, kt * 128:(kt + 1) * 128], identity=ID32[:]),
                      reads=[rSC, rC], writes=[rPB[6]])
                sel, rsel, _ = selr.next()
                kb.op("dve", lambda e: e.tensor_tensor(out=sel[:], in0=PB[6][:, sq:sq + 128], in1=THB[:], op=ALU.is_ge),
                      reads=[rPB[6], rTH], writes=[rsel])
                st_sel[kt] = (sel, rsel)
                lb = (kt % 2) * 2
                for h in range(8):
                    hp = (h % 2) * 64
                    kb.op("pe", lambda e: e.matmul(PB[lb + h % 2][:, (h // 2) * 128:(h // 2 + 1) * 128], lhsT=KT[hp:hp + 64, h // 2, kt * 128:(kt + 1) * 128],
                                                   rhs=qT[hp:hp + 64, h // 2, :], start=True, stop=True), reads=[rKT, rqT], writes=[rPB[lb + h % 2]])

            def stage_b(kt):
                sel, rsel = st_sel.pop(kt)
                lb = (kt % 2) * 2
                E, rE, _ = Er.next()
                for hh in range(2):
                    kb.op("act", lambda e: e.activation(out=E[:, hh * 4:(hh + 1) * 4, :], in_=PB[lb + hh][:].rearrange("p (h q) -> p h q", h=4), func=AF.Exp),
                          reads=[rPB[lb + hh]], writes=[rE])
                zi = kt - (4 * i - 1)
                if zi >= 0:
                    kb.op("dve", lambda e: e.tensor_tensor(out=E[:], in0=E[:], in1=TAB[:, zi, :, :], op=ALU.mult), reads=[rE, rTAB], writes=[rE])
                kb.op("dve", lambda e: e.tensor_tensor(out=E[:], in0=E[:], in1=sel[:].unsqueeze(1).to_broadcast([128, 8, 128]), op=ALU.mult),
                      reads=[rE, rsel], writes=[rE])
                for h in range(8):
                    kb.op("pe", lambda e: e.matmul(PB[4 + h // 4][:, (h % 4) * 65:(h % 4 + 1) * 65], lhsT=E[:, (h % 2) * 4 + h // 2, :], rhs=V[:, kt, h, :],
                                                   start=False, stop=(kt == ntile - 1), skip_group_check=True), reads=[rE, rV], writes=[rPB[4 + h // 4]])

            stage_a(0)
            for kt in range(ntile):
                if kt + 1 < ntile:
                    stage_a(kt + 1)
                stage_b(kt)
            for hh in range(2):
                ov = PB[4 + hh][:, 0:260].rearrange("p (h d) -> p h d", h=4)
                kb.op("dve", lambda e: e.reciprocal(out=RS[:, hh * 4:(hh + 1) * 4], in_=ov[:, :, 64]), reads=[rPB[4 + hh]], writes=[rOS])
                kb.op("dve", lambda e: e.tensor_tensor(out=OS[:, hh * 4:(hh + 1) * 4, :], in0=ov[:, :, 0:64],
                                                       in1=RS[:, hh * 4:(hh + 1) * 4].unsqueeze(2).to_broadcast([128, 4, 64]), op=ALU.mult),
                      reads=[rPB[4 + hh], rOS], writes=[rOS])
            kb.dma("sp", scr_o.ap()[i], OS[:].rearrange("p h d -> p (h d)"), ss, reads=[rOS], writes=[r_scr_o])
            if stop == "2_%d" % i:
                return finish([(SC[:, 0:1024], rSC, 128, 1024), (BS[:], rBS, 128, 16), (OS[:].rearrange("p h d -> p (h d)"), rOS, 128, 512),
                               (wi_all[:, i, :], rC, 128, 8), (sg_all[:, i, :], rC, 128, 8), (qT[:, 0, :], rqT, 128, 128), (RS[:], rOS, 128, 8)])
    es_att.close()
    kb.barrier()

    kb.barrier()
    es_h2t = ExitStack()
    H2T = SB(es_h2t, "H2T", [128, 8, NSLOT * 128], BF16)
    rH2T = Res("H2T")
    kb.barrier()
    with Scope() as es:
        Wc = SB(es, "Wc", [128, 8, 1536], BF16)
        Wgl = SB(es, "Wgl", [128, 8, 2048], BF16)
        Wab = SB(es, "Wab", [128, 4, D], BF16)
        Wcb = SB(es, "Wcb", [128, 4, D], BF16)
        cw = SB(es, "cw", [128, 4, 3], F32)
        rW = Res("W3")
        ws = kb.slot("dq_w3")
        kb.dma("pool", Wc[:], win_v[:, :, C_CB:C_CB + 1536], ws, writes=[rW])
        kb.dma("pool", Wgl[:], win_v[:, :, C_GL:C_GL + 2048], ws, writes=[rW])
        kb.dma("pool", Wab[:], wab_d.ap().rearrange("(k p) n -> p k n", p=128), ws, writes=[rW])
        kb.dma("pool", Wcb[:], wcbr_d.ap().rearrange("(k p) n -> p k n", p=128), ws, writes=[rW])
        kb.dma("sp", cw[:], convw_d.ap(), ws, writes=[rW])
        XH = SB(es, "XH", [2, D], F32)
        XNH = SB(es, "XNH", [2, D], BF16)
        STH = SB(es, "STH", [2, 4], F32)
        rXH = Res("XH")
        xr = Ring(kb, "x3", 2, lambda n: SB(es, n, [128, D], F32))
        xn3r = Ring(kb, "XN3", 2, lambda n: SB(es, n, [128, D], BF16))
        st3r = Ring(kb, "ST3", 2, lambda n: SB(es, n, [128, 4], F32))
        HT = SB(es, "HT3", [128, 8, 130], BF16)
        rHT = Res("HT3")
        CCs = SB(es, "CCs", [128, 4, 130], F32)
        U = SB(es, "U", [128, 4, 130], F32)
        CV = SB(es, "CV", [128, 4, 128], F32)
        VT = SB(es, "VT", [128, 4, 128], BF16)
        rU = Res("U")
        rUf = [Res("U%d" % k) for k in range(4)]
        G = SB(es, "G", [128, 2048], F32)
        rG = Res("G")
        M1 = SB(es, "M1", [128, D], F32)
        MB = SB(es, "MB", [128, D], BF16)
        rM = Res("M")
        OSb = SB(es, "OSb", [128, 512], BF16)
        AT = SB(es, "AT", [128, 4, 128], BF16)
        rAT = Res("AT")
        osl = kb.slot("dq_o3")
        pre3 = {}

        def stats3(i):
            X, rX, sl = xr.next()
            kb.dma("sp", X[:], xo_d.ap()[i, 2:130, :], sl, writes=[rX])
            xn, rxn, _ = xn3r.next()
            st, rst, _ = st3r.next()
            norm_stats(X[:], rX, 128, xn, rxn, st, rst)
            pre3[i] = (xn, rxn)

        stats3(0)
        for i in range(NSLOT):
            if i + 1 < NSLOT:
                stats3(i + 1)
            kb.dma("sp", XH[:], xo_d.ap()[i, 0:2, :], None, writes=[rXH])
            kb.dma("sp", OSb[:], scr_o.ap()[i], osl, reads=[r_scr_o], writes=[rAT])
            xn, rxn = pre3.pop(i)
            norm_tr(xn, rxn, 128, lambda c: HT[:, c, 2:130], rHT, s1, sh1, 6)
            kb.op("act", lambda e: e.activation(out=XNH[:], in_=XH[:], func=AF.Square, accum_out=STH[:, 0:1]), reads=[rXH], writes=[rXH])
            kb.op("act", lambda e: e.activation(out=STH[:, 1:2], in_=STH[:, 0:1], func=AF.Ln, scale=1.0 / D, bias=epsb[0:2, 0:1]), reads=[rXH, rC], writes=[rXH])
            kb.op("act", lambda e: e.activation(out=STH[:, 2:3], in_=STH[:, 1:2], func=AF.Exp, scale=-0.5), reads=[rXH], writes=[rXH])
            kb.op("dve", lambda e: e.tensor_scalar(out=XNH[:], in0=XH[:], scalar1=STH[:, 2:3], scalar2=None, op0=ALU.mult), reads=[rXH], writes=[rXH])
            for c in range(8):
                kb.op("pe", lambda e: e.matmul(PB[7][:, c * 2:c * 2 + 2], lhsT=XNH[:, c * 128:(c + 1) * 128], rhs=ID[0:2, 0:2], start=True, stop=True),
                      reads=[rXH, rC], writes=[rPB[7]])
            for c in range(8):
                kb.op("dve", lambda e: e.tensor_scalar(out=HT[:, c, 0:2], in0=PB[7][:, c * 2:c * 2 + 2], scalar1=s1[:, c:c + 1], scalar2=sh1[:, c:c + 1],
                                                       op0=ALU.mult, op1=ALU.add), reads=[rPB[7], rMOD], writes=[rHT])
            for fc in range(4):
                cb0 = (fc % 2) * 3
                for kc in range(8):
                    kb.op("pe", lambda e: e.matmul(PB[cb0][:, 0:130], lhsT=Wc[:, kc, 512 + fc * 128:512 + (fc + 1) * 128], rhs=HT[:, kc, :],
                                                   start=(kc == 0), stop=(kc == 7)), reads=[rW, rHT], writes=[rPB[cb0]])
                for kc in range(8):
                    kb.op("pe", lambda e: e.matmul(PB[cb0 + 1][:, 0:130], lhsT=Wc[:, kc, 1024 + fc * 128:1024 + (fc + 1) * 128], rhs=HT[:, kc, :],
                                                   start=(kc == 0), stop=(kc == 7)), reads=[rW, rHT], writes=[rPB[cb0 + 1]])
                for kc in range(8):
                    kb.op("pe", lambda e: e.matmul(PB[cb0 + 2][:, 0:128], lhsT=Wc[:, kc, fc * 128:(fc + 1) * 128], rhs=HT[:, kc, 2:130],
                                                   start=(kc == 0), stop=(kc == 7)), reads=[rW, rHT], writes=[rPB[cb0 + 2]])
                kb.op("act", lambda e: e.copy(out=CCs[:, fc, :], in_=PB[cb0][:, 0:130]), reads=[rPB[cb0]], writes=[rUf[fc]])
                kb.op("dve", lambda e: e.tensor_tensor(out=U[:, fc, :], in0=CCs[:, fc, :], in1=PB[cb0 + 1][:, 0:130], op=ALU.mult), reads=[rPB[cb0 + 1], rUf[fc]], writes=[rUf[fc]])
                kb.op("dve", lambda e: e.tensor_scalar(out=U[:, fc, 0:2], in0=U[:, fc, 0:2], scalar1=hmask[:, i:i + 1], scalar2=None, op0=ALU.mult),
                      reads=[rUf[fc], rC], writes=[rUf[fc]])
                kb.op("dve", lambda e: e.tensor_scalar(out=CV[:, fc, :], in0=U[:, fc, 0:128], scalar1=cw[:, fc, 0:1], scalar2=None, op0=ALU.mult),
                      reads=[rUf[fc], rW], writes=[rUf[fc]])
                kb.op("dve", lambda e: e.scalar_tensor_tensor(out=CV[:, fc, :], in0=U[:, fc, 1:129], scalar=cw[:, fc, 1:2], in1=CV[:, fc, :],
                                                               op0=ALU.mult, op1=ALU.add), reads=[rUf[fc], rW], writes=[rUf[fc]])
                kb.op("dve", lambda e: e.scalar_tensor_tensor(out=CV[:, fc, :], in0=U[:, fc, 2:130], scalar=cw[:, fc, 2:3], in1=CV[:, fc, :],
                                                               op0=ALU.mult, op1=ALU.add), reads=[rUf[fc], rW], writes=[rUf[fc]])
                kb.op("dve", lambda e: e.tensor_tensor(out=VT[:, fc, :], in0=CV[:, fc, :], in1=PB[cb0 + 2][:, 0:128], op=ALU.mult), reads=[rPB[cb0 + 2], rUf[fc]], writes=[rUf[fc]])
            for n in range(4):
                for kc in range(8):
                    kb.op("pe", lambda e: e.matmul(PB[6 + n % 2][:], lhsT=HT[:, kc, 2:130], rhs=Wgl[:, kc, n * 512:(n + 1) * 512],
                                                   start=(kc == 0), stop=(kc == 7)), reads=[rW, rHT], writes=[rPB[6 + n % 2]])
                kb.op("act", lambda e: e.activation(out=G[:, n * 512:(n + 1) * 512], in_=PB[6 + n % 2][:], func=AF.Sigmoid), reads=[rPB[6 + n % 2]], writes=[rG])
            for c in range(4):
                kb.op("pe", lambda e: e.transpose(out=pbf(5)[:, c * 128:(c + 1) * 128], in_=OSb[:, c * 128:(c + 1) * 128], identity=ID[:]),
                      reads=[rAT, rC], writes=[rPB[5]])
            kb.op("dve", lambda e: e.tensor_copy(out=AT[:].rearrange("p c q -> p (c q)"), in_=pbf(5)[:, 0:512]), reads=[rPB[5]], writes=[rAT])
            for n in range(2):
                for kc in range(4):
                    kb.op("pe", lambda e: e.matmul(PB[0 + n][:], lhsT=AT[:, kc, :], rhs=Wab[:, kc, n * 512:(n + 1) * 512], start=(kc == 0), stop=(kc == 3)),
                          reads=[rW, rAT], writes=[rPB[0 + n]])
                kb.op("dve", lambda e: e.tensor_tensor(out=M1[:, n * 512:(n + 1) * 512], in0=G[:, n * 512:(n + 1) * 512], in1=PB[0 + n][:], op=ALU.mult),
                      reads=[rPB[0 + n], rG], writes=[rM])
            for n in range(2):
                for kc in range(4):
                    kb.op("pe", lambda e: e.matmul(PB[0 + n][:], lhsT=VT[:, kc, :], rhs=Wcb[:, kc, n * 512:(n + 1) * 512], start=(kc == 0), stop=(kc == 3)),
                          reads=[rW] + rUf, writes=[rPB[0 + n]])
                kb.op("dve", lambda e: e.tensor_tensor(out=G[:, D + n * 512:D + (n + 1) * 512], in0=G[:, D + n * 512:D + (n + 1) * 512], in1=PB[0 + n][:], op=ALU.mult),
                      reads=[rPB[0 + n], rG], writes=[rG])
            kb.op("dve", lambda e: e.tensor_tensor(out=MB[:], in0=M1[:], in1=G[:, D:2 * D], op=ALU.add), reads=[rM, rG], writes=[rM])
            for c in range(8):
                kb.op("pe", lambda e: e.transpose(out=pbf(5)[:, c * 128:(c + 1) * 128], in_=MB[:, c * 128:(c + 1) * 128], identity=ID[:]),
                      reads=[rM, rC], writes=[rPB[5]])
            kb.op("act", lambda e: e.copy(out=H2T[:, :, i * 128:(i + 1) * 128], in_=pbf(5).rearrange("p (c q) -> p c q", c=8)),
                  reads=[rPB[5]], writes=[rH2T])

    if stop == "3a":
        return finish([(H2T[:, 0, 0:1024], rH2T, 128, 1024), (H2T[:, 7, 1024:2048], rH2T, 128, 1024)])
    kb.barrier()
    es_moe = ExitStack()
    HR = SB(es_moe, "HR", [128, NSLOT, D], F32)
    GD = SB(es_moe, "GD", [128, NSLOT, 32], F32)
    rHR = [Res("HR%d" % i) for i in range(NSLOT)]
    rGD = Res("GD")
    kb.barrier()
    with Scope() as es:
        Wo = SB(es, "Wo", [128, 8, D], BF16)
        Wr = SB(es, "Wr", [128, 8, 36], F32)
        brb = SB(es, "brb", [128, 36], F32)
        g1bc = SB(es, "g1bc", [128, D], F32)
        rW = Res("W3b")
        rg1 = Res("g1bc")
        ws = kb.slot("dq_w3b")
        kb.dma("pool", Wo[:], wout_d.ap().rearrange("(k p) n -> p k n", p=128), ws, writes=[rW])
        kb.dma("sp", Wr[:], wr_d.ap().rearrange("(k p) n -> p k n", p=128), ws, writes=[rW])
        kb.dma("sp", brb[:], br_d.ap().partition_broadcast(128), ws, writes=[rW])
        gate_bc(g1bc, rg1, 2)
        for kc in range(8):
            kb.op("dve", lambda e: e.tensor_tensor(out=Wo[:, kc, :], in0=Wo[:, kc, :], in1=g1bc[:], op=ALU.mult), reads=[rW, rg1], writes=[rW])
        xr = Ring(kb, "x3b", 2, lambda n: SB(es, n, [128, D], F32))
        ST = SB(es, "ST3b", [128, 4], F32)
        rST = Res()
        H2 = SB(es, "H2", [128, D], F32)
        H2T32 = SB(es, "H2T32", [128, 8, 128], F32)
        rH2 = Res("H2")
        RT = SB(es, "RT", [128, 96], F32)
        rRT = Res("RT")
        for i in range(NSLOT):
            X, rX, sl = xr.next()
            kb.dma("sp", X[:], xo_d.ap()[i, 2:130, :], sl, writes=[rX])
            for n in range(2):
                for kc in range(8):
                    kb.op("pe", lambda e: e.matmul(PB[0 + n][:], lhsT=H2T[:, kc, i * 128:(i + 1) * 128], rhs=Wo[:, kc, n * 512:(n + 1) * 512],
                                                   start=(kc == 0), stop=(kc == 7)), reads=[rW, rH2T], writes=[rPB[0 + n]])
                kb.op("dve", lambda e: e.tensor_tensor(out=HR[:, i, n * 512:(n + 1) * 512], in0=X[:, n * 512:(n + 1) * 512], in1=PB[0 + n][:], op=ALU.add),
                      reads=[rPB[0 + n], rX], writes=[rHR[i]])
            if stop == "3b1":
                return finish([(HR[:, 0, :], rHR[0], 128, 1024)])
            kb.op("act", lambda e: e.activation(out=H2[:], in_=HR[:, i, :], func=AF.Square, accum_out=ST[:, 0:1]), reads=[rHR[i]], writes=[rST, rH2])
            kb.op("act", lambda e: e.activation(out=ST[:, 1:2], in_=ST[:, 0:1], func=AF.Ln, scale=1.0 / D, bias=epsb[:, 0:1]), reads=[rST, rC], writes=[rST])
            kb.op("act", lambda e: e.activation(out=ST[:, 2:3], in_=ST[:, 1:2], func=AF.Exp, scale=-0.5), reads=[rST], writes=[rST])
            kb.op("dve", lambda e: e.tensor_scalar(out=H2[:], in0=HR[:, i, :], scalar1=ST[:, 2:3], scalar2=None, op0=ALU.mult), reads=[rHR[i], rST], writes=[rH2])
            if stop == "3b1a":
                return finish([(H2[:], rH2, 128, 1024), (ST[:], rST, 128, 4)])
            for c in range(8):
                pb = 6 + c // 4
                kb.op("pe", lambda e: e.transpose(out=PB[pb][:, (c % 4) * 128:(c % 4 + 1) * 128], in_=H2[:, c * 128:(c + 1) * 128], identity=ID32[:]),
                      reads=[rH2, rC], writes=[rPB[pb]])
            if stop == "3b1b":
                kb.op("dve", lambda e: e.tensor_copy(out=H2[:, 0:512], in_=PB[6][:]), reads=[rPB[6]], writes=[rH2])
                kb.op("dve", lambda e: e.tensor_copy(out=H2[:, 512:1024], in_=PB[7][:]), reads=[rPB[7]], writes=[rH2])
                return finish([(H2[:], rH2, 128, 1024)])
            for c in range(8):
                pb = 6 + c // 4
                src = PB[pb][:, (c % 4) * 128:(c % 4 + 1) * 128]
                kb.op("act", lambda e: e.activation(out=H2T32[:, c, :], in_=src, func=AF.Identity, scale=s2[:, c:c + 1], bias=sh2[:, c:c + 1]),
                      reads=[rPB[pb], rMOD], writes=[rH2])
                kb.op("dve", lambda e: e.tensor_copy(out=H2T[:, c, i * 128:(i + 1) * 128], in_=H2T32[:, c, :]), reads=[rH2], writes=[rH2T])
            if stop == "3b2":
                return finish([(HR[:, 0, :], rHR[0], 128, 1024), (H2T32[:].rearrange("p c q -> p (c q)"), rH2, 128, 1024)])
            for kc in range(8):
                kb.op("pe", lambda e: e.matmul(PB[2][:, 0:36], lhsT=H2T32[:, kc, :], rhs=Wr[:, kc, :], start=(kc == 0), stop=(kc == 7)),
                      reads=[rW, rH2], writes=[rPB[2]])
            L = RT[:, 0:36]
            if stop == "3b3":
                kb.op("dve", lambda e: e.tensor_copy(out=RT[:, 0:36], in_=PB[2][:, 0:36]), reads=[rPB[2]], writes=[rRT])
                return finish([(RT[:, 0:36], rRT, 128, 36)])

            def rt(fn, extra_r=(), e_="dve"):
                kb.op(e_, fn, reads=[rRT] + list(extra_r), writes=[rRT])
            kb.op("dve", lambda e: e.tensor_tensor(out=L, in0=PB[2][:, 0:36], in1=brb[:], op=ALU.add), reads=[rPB[2], rW], writes=[rRT])
            rt(lambda e: e.tensor_reduce(out=RT[:, 36:37], in_=RT[:, 0:4], axis=AX.X, op=ALU.max))
            rt(lambda e: e.tensor_scalar(out=RT[:, 80:84], in0=RT[:, 0:4], scalar1=RT[:, 36:37], scalar2=None, op0=ALU.is_ge))
            rt(lambda e: e.tensor_scalar(out=RT[:, 37:41], in0=RT[:, 0:4], scalar1=RT[:, 36:37], scalar2=None, op0=ALU.subtract))
            rt(lambda e: e.activation(out=RT[:, 37:41], in_=RT[:, 37:41], func=AF.Exp, accum_out=RT[:, 41:42]), e_="act")
            rt(lambda e: e.reciprocal(out=RT[:, 42:43], in_=RT[:, 41:42]))
            rt(lambda e: e.tensor_scalar(out=RT[:, 84:88], in0=RT[:, 80:84], scalar1=-1.0, scalar2=1.0e30, op0=ALU.add, op1=ALU.mult))
            rt(lambda e: e.tensor_tensor(out=RT[:, 44:76].rearrange("p (g x) -> p g x", g=4), in0=RT[:, 4:36].rearrange("p (g x) -> p g x", g=4),
                                         in1=RT[:, 84:88].unsqueeze(2).to_broadcast([128, 4, 8]), op=ALU.add))
            rt(lambda e: e.tensor_reduce(out=RT[:, 76:77], in_=RT[:, 44:76], axis=AX.X, op=ALU.max))
            rt(lambda e: e.tensor_scalar(out=GD[:, i, :], in0=RT[:, 44:76], scalar1=RT[:, 76:77], scalar2=None, op0=ALU.is_ge), extra_r=[rGD])
            rt(lambda e: e.scalar_tensor_tensor(out=RT[:, 44:76], in0=GD[:, i, :], scalar=-1.0e30, in1=RT[:, 44:76], op0=ALU.mult, op1=ALU.add), extra_r=[rGD])
            rt(lambda e: e.tensor_reduce(out=RT[:, 77:78], in_=RT[:, 44:76], axis=AX.X, op=ALU.max))
            rt(lambda e: e.tensor_scalar(out=RT[:, 44:76], in0=RT[:, 44:76], scalar1=RT[:, 77:78], scalar2=None, op0=ALU.is_ge))
            rt(lambda e: e.tensor_tensor(out=RT[:, 78:79], in0=RT[:, 77:78], in1=RT[:, 76:77], op=ALU.subtract))
            rt(lambda e: e.activation(out=RT[:, 78:79], in_=RT[:, 78:79], func=AF.Exp), e_="act")
            rt(lambda e: e.tensor_scalar(out=RT[:, 78:79], in0=RT[:, 78:79], scalar1=1.0, scalar2=None, op0=ALU.add))
            rt(lambda e: e.reciprocal(out=RT[:, 78:79], in_=RT[:, 78:79]))
            rt(lambda e: e.tensor_scalar(out=RT[:, 79:80], in0=RT[:, 78:79], scalar1=-1.0, scalar2=1.0, op0=ALU.mult, op1=ALU.add))
            rt(lambda e: e.tensor_tensor(out=RT[:, 78:80], in0=RT[:, 78:80], in1=RT[:, 42:43].to_broadcast([128, 2]), op=ALU.mult))
            rt(lambda e: e.tensor_scalar(out=GD[:, i, :], in0=GD[:, i, :], scalar1=RT[:, 78:79], scalar2=None, op0=ALU.mult), extra_r=[rGD])
            kb.op("dve", lambda e: e.scalar_tensor_tensor(out=GD[:, i, :], in0=RT[:, 44:76], scalar=RT[:, 79:80], in1=GD[:, i, :], op0=ALU.mult, op1=ALU.add),
                  reads=[rRT], writes=[rGD, rRT])

    if stop == "3b":
        return finish([(HR[:, 0, :], rHR[0], 128, 1024), (HR[:, 15, :], rHR[15], 128, 1024), (GD[:, 0, :], rGD, 128, 32), (GD[:, 15, :], rGD, 128, 32),
                       (H2T[:, 0, 0:1024], rH2T, 128, 1024)])
    kb.barrier()
    with Scope() as es:
        wgr = Ring(kb, "wg", 2, lambda n: SB(es, n, [128, 8, 512], BF16))
        wur = Ring(kb, "wu", 2, lambda n: SB(es, n, [128, 8, 512], BF16))
        wdr = Ring(kb, "wd", 2, lambda n: SB(es, n, [128, 4, D], BF16))
        sir = Ring(kb, "si", 2, lambda n: SB(es, n, [128, 512], F32))
        acr = Ring(kb, "ac", 2, lambda n: SB(es, n, [128, 4, 512], BF16))
        g2bc = SB(es, "g2bc", [128, D], F32)
        rg2 = Res("g2bc")
        gate_bc(g2bc, rg2, 5)
        wge_v = wge_d.ap().rearrange("e (k p) n -> e p k n", p=128)
        wue_v = wue_d.ap().rearrange("e (k p) n -> e p k n", p=128)
        wde_v = wde_d.ap().rearrange("e (k p) n -> e p k n", p=128)
        wts = {}
        acs = {}

        def load_expert(ex):
            Wg, rWg, sg_ = wgr.next()
            Wu, rWu, su_ = wur.next()
            Wd, rWd, sd_ = wdr.next()
            kb.dma("pool", Wg[:], wge_v[ex], sg_, writes=[rWg])
            kb.dma("pool", Wu[:], wue_v[ex], su_, writes=[rWu])
            kb.dma("pool", Wd[:], wde_v[ex], sd_, writes=[rWd])
            for kc in range(4):
                kb.op("dve", lambda e: e.tensor_tensor(out=Wd[:, kc, :], in0=Wd[:, kc, :], in1=g2bc[:], op=ALU.mult), reads=[rWd, rg2], writes=[rWd])
            wts[ex] = (Wg, rWg, Wu, rWu, Wd, rWd)

        def stage_gu(ex, g):
            if ex not in wts:
                load_expert(ex)
            Wg, rWg, Wu, rWu, Wd, rWd = wts[ex]
            ac, rac, _ = acr.next()
            acs[(ex, g)] = (ac, rac)
            for fc in range(4):
                bg = (fc % 2) * 2
                for kc in range(8):
                    kb.op("pe", lambda e: e.matmul(PB[bg][:], lhsT=Wg[:, kc, fc * 128:(fc + 1) * 128], rhs=H2T[:, kc, g * 512:(g + 1) * 512],
                                                   start=(kc == 0), stop=(kc == 7)), reads=[rWg, rH2T], writes=[rPB[bg]])
                for kc in range(8):
                    kb.op("pe", lambda e: e.matmul(PB[bg + 1][:], lhsT=Wu[:, kc, fc * 128:(fc + 1) * 128], rhs=H2T[:, kc, g * 512:(g + 1) * 512],
                                                   start=(kc == 0), stop=(kc == 7)), reads=[rWu, rH2T], writes=[rPB[bg + 1]])
                si, rsi, _ = sir.next()
                kb.op("act", lambda e: e.activation(out=si[:], in_=PB[bg][:], func=AF.Silu), reads=[rPB[bg]], writes=[rsi])
                kb.op("dve", lambda e: e.tensor_tensor(out=ac[:, fc, :], in0=si[:], in1=PB[bg + 1][:], op=ALU.mult), reads=[rPB[bg + 1], rsi], writes=[rac])

        def stage_down(ex, g):
            Wg, rWg, Wu, rWu, Wd, rWd = wts[ex]
            ac, rac = acs.pop((ex, g))
            for tt in range(4):
                sl_i = g * 4 + tt
                for n in range(2):
                    bd = 4 + (tt % 2) * 2 + n
                    for kc in range(4):
                        kb.op("pe", lambda e: e.matmul(PB[bd][:], lhsT=ac[:, kc, tt * 128:(tt + 1) * 128], rhs=Wd[:, kc, n * 512:(n + 1) * 512],
                                                       start=(kc == 0), stop=(kc == 3)), reads=[rWd, rac], writes=[rPB[bd]])
                    kb.op("dve", lambda e: e.scalar_tensor_tensor(out=HR[:, sl_i, n * 512:(n + 1) * 512], in0=PB[bd][:], scalar=GD[:, sl_i, ex:ex + 1],
                                                                  in1=HR[:, sl_i, n * 512:(n + 1) * 512], op0=ALU.mult, op1=ALU.add),
                          reads=[rPB[bd], rGD], writes=[rHR[sl_i]])
            if g == 3:
                wts.pop(ex)

        items = [(ex, g) for ex in range(32) for g in range(4)]
        stage_gu(*items[0])
        for k_ in range(len(items)):
            if k_ + 1 < len(items):
                stage_gu(*items[k_ + 1])
            stage_down(*items[k_])

        gfb = SB(es, "gfb", [128, D], F32)
        rgf = Res("gf")
        kb.dma("sp", gfb[:], gf_d.ap().partition_broadcast(128), cs, writes=[rgf])
        outr = Ring(kb, "yo", 2, lambda n: SB(es, n, [128, D], F32))
        STF = SB(es, "STF", [128, 4], F32)
        rSTF = Res("STF")
        osl = kb.slot("dq_out")
        outs = []
        for i in range(NSLOT):
            yo, ryo, _ = outr.next()
            kb.op("act", lambda e: e.activation(out=yo[:], in_=HR[:, i, :], func=AF.Square, accum_out=STF[:, 0:1]), reads=[rHR[i]], writes=[rSTF, ryo])
            kb.op("act", lambda e: e.activation(out=STF[:, 1:2], in_=STF[:, 0:1], func=AF.Ln, scale=1.0 / D, bias=epsb[:, 0:1]), reads=[rSTF, rC], writes=[rSTF])
            kb.op("act", lambda e: e.activation(out=STF[:, 2:3], in_=STF[:, 1:2], func=AF.Exp, scale=-0.5), reads=[rSTF], writes=[rSTF])
            kb.op("dve", lambda e: e.scalar_tensor_tensor(out=yo[:], in0=HR[:, i, :], scalar=STF[:, 2:3], in1=gfb[:], op0=ALU.mult, op1=ALU.mult),
                  reads=[rHR[i], rSTF, rgf], writes=[ryo])
            kb.dma("sp", y_d.ap()[i * 128:(i + 1) * 128, :], yo[:], osl, reads=[ryo])
            outs.append(ryo)
        kb.wait_all("sp", outs)
    es_moe.close()
    es_h2t.close()
    es_all.close()
    return nc


def _t5_bucket(n):
    n = np.maximum(n, 0)
    nf = np.maximum(n, 1).astype(np.float32)
    large = 16 + (np.log(nf / np.float32(16)) / np.float32(np.log(128 / 16)) * np.float32(16)).astype(np.int32)
    large = np.minimum(large, 31)
    return np.where(n < 16, n, large)


def _structural_tables(j):
    E = np.zeros((32, TABW), np.float32)
    for zi in range(5):
        z = zi - 1
        m = np.arange(255)
        n = (j - z) * 128 - 127 + m
        ok = n >= 0
        b = _t5_bucket(n)
        cols = zi * 256 + m
        E[b[ok], cols[ok]] += 1.0
        E[31, cols[ok]] -= 1.0
    return E


_NC_CACHE = {}


def kernel(x, c, w_ada, b_ada, norm1_g, w_in, rel_bias, conv_w, w_attn_branch, w_conv_branch, w_out, norm2_g,
           w_router_group, b_router_group, w_router_expert, b_router_expert, w_gate_e, w_up_e, w_down_e, norm_f_g):
    f = lambda a: np.ascontiguousarray(np.asarray(a, dtype=np.float32))
    x = f(x)
    c = f(c)
    shared = {
        "w_ada": f(w_ada)[0],
        "b_adaT": f(np.asarray(b_ada)[0].reshape(48, 128).T),
        "b_ada": f(b_ada)[0:1],
        "g1T": f(np.asarray(norm1_g)[0].reshape(8, 128).T),
        "g2T": f(np.asarray(norm2_g)[0].reshape(8, 128).T),
        "gf": f(norm_f_g).reshape(1, D),
        "w_in": f(w_in)[0],
        "relb": f(np.asarray(rel_bias)[:, [0, 2, 4, 6, 1, 3, 5, 7]]),
        "convw": f(np.asarray(conv_w)[0].reshape(3, 4, 128).transpose(2, 1, 0)),
        "w_ab": f(w_attn_branch)[0],
        "w_cbr": f(w_conv_branch)[0],
        "w_out": f(w_out)[0],
        "wr": f(np.concatenate([np.asarray(w_router_group)[0], np.asarray(w_router_expert)[0]], axis=1)),
        "br": f(np.concatenate([np.asarray(b_router_group)[0], np.asarray(b_router_expert)[0]])).reshape(1, 36),
        "wge": f(w_gate_e)[0],
        "wue": f(w_up_e)[0],
        "wde": f(w_down_e)[0],
    }
    in_maps = []
    for core in range(8):
        b, j = core // 4, core % 4
        xo = np.zeros((NSLOT, 130, D), np.float32)
        hm = np.ones((128, NSLOT), np.float32)
        for i in range(NSLOT):
            st = (4 * i + j) * 128
            if st == 0:
                xo[i, 2:] = x[b, 0:128]
                hm[:, i] = 0.0
            else:
                xo[i] = x[b, st - 2:st + 128]
        m = dict(shared)
        m["xb"] = x[b]
        m["xo"] = xo
        m["c8"] = f(c[b].reshape(8, 128).T)
        m["ej"] = _structural_tables(j)
        m["tq"] = (j * 128 + np.arange(128, dtype=np.float32)).reshape(128, 1)
        m["hmask"] = hm
        in_maps.append(m)
    if "nc" not in _NC_CACHE:
        _NC_CACHE["nc"] = build_nc()
    res = run_bass_kernel_spmd(_NC_CACHE["nc"], in_maps, core_ids=list(range(8)))
    out = np.empty((2, S, D), np.float32)
    for core in range(8):
        b, j = core // 4, core % 4
        y = res.results[core]["y"]
        for i in range(NSLOT):
            st = (4 * i + j) * 128
            out[b, st:st + 128] = y[i * 128:(i + 1) * 128]
    return out
```

```python
from contextlib import ExitStack
import numpy as np
import concourse.bass as bass
import concourse.mybir as mybir
from concourse.bass_utils import run_bass_kernel_spmd

F32 = mybir.dt.float32
BF16 = mybir.dt.bfloat16
ALU = mybir.AluOpType
AF = mybir.ActivationFunctionType
AX = mybir.AxisListType

D = 1024
S = 8192
NT = S // 128
NSLOT = 16
IN_COLS = 5704
C_Q, C_K, C_V, C_QI, C_KI, C_WI, C_CB, C_CC, C_CX, C_GL = 0, 512, 1024, 1536, 2048, 2112, 2120, 2632, 3144, 3656
EPS = 1e-6
NEG = -1.0e30
NIT = 18
TABW = 5 * 256
SEM_ROT = 30000


class Res:
    __slots__ = ("w", "r", "name")

    def __init__(self, name=""):
        self.w = None
        self.r = {}
        self.name = name


class Slot:
    def __init__(self, kb, name):
        self.kb = kb
        self.name = name
        self.sem = kb.nc.alloc_semaphore(name)
        self.val = 0
        kb.slots.append(self)

    def bump(self):
        if self.val + 16 > SEM_ROT:
            self.sem = self.kb.nc.alloc_semaphore(self.name + "_r%d" % self.kb.uid())
            self.val = 0
        self.val += 16
        return self.sem, self.val


class KB:
    def __init__(self, nc):
        self.nc = nc
        self.eng = {"pe": nc.tensor, "act": nc.scalar, "dve": nc.vector, "pool": nc.gpsimd, "sp": nc.sync}
        self._uid = 0
        self.sem = {k: nc.alloc_semaphore("s_" + k) for k in self.eng}
        self.cnt = {k: 0 for k in self.eng}
        self.seen = {k: {} for k in self.eng}
        self.nins = 0
        self.slots = []
        self.pool = {}
        self.pool_i = {}

    def barrier(self):
        evs = [(self.sem[k], self.cnt[k], k) for k in self.eng if self.cnt[k] > 0]
        evs += [(sl.sem, sl.val, "dma") for sl in self.slots if sl.val > 0]
        for e in self.eng:
            for sem, val, src in evs:
                if self.seen[e].get(sem.num, 0) >= val:
                    continue
                self.eng[e].wait_ge(sem, val)
                self.seen[e][sem.num] = val

    def uid(self):
        self._uid += 1
        return self._uid

    def _wait(self, e, ev):
        sem, val, src = ev
        if src == "pe" and e == "pe":
            return
        if self.seen[e].get(sem.num, 0) >= val:
            return
        self.eng[e].wait_ge(sem, val)
        self.seen[e][sem.num] = val

    def deps(self, e, reads, writes):
        for r in reads:
            if r.w is not None:
                self._wait(e, r.w)
        for w in writes:
            if w.w is not None:
                self._wait(e, w.w)
            for ev in w.r.values():
                self._wait(e, ev)

    def _record(self, ev, reads, writes):
        for r in reads:
            r.r[ev[0].num] = ev
        for w in writes:
            w.w = ev
            w.r = {}

    def op(self, e, fn, reads=(), writes=()):
        self.deps(e, reads, writes)
        ins = fn(self.eng[e])
        if self.cnt[e] + 1 > SEM_ROT:
            self.sem[e] = self.nc.alloc_semaphore("s_%s_r%d" % (e, self.uid()))
            self.cnt[e] = 0
        self.cnt[e] += 1
        ins.then_inc(self.sem[e], 1)
        self.nins += 1
        ev = (self.sem[e], self.cnt[e], e)
        self._record(ev, reads, writes)
        return ev

    def dma(self, q, out, in_, slot, reads=(), writes=(), **kw):
        self.deps(q, reads, writes)
        if slot is None:
            if q not in self.pool:
                self.pool[q] = [Slot(self, "dq_%s%d" % (q, k)) for k in range(16)]
                self.pool_i[q] = 0
            slot = self.pool[q][self.pool_i[q] % 16]
            self.pool_i[q] += 1
            if slot.val > 0:
                self._wait(q, (slot.sem, slot.val, "dma"))
        ins = self.eng[q].dma_start(out=out, in_=in_, **kw)
        sem, val = slot.bump()
        ins.then_inc(sem, 16)
        self.nins += 1
        ev = (sem, val, "dma")
        self._record(ev, reads, writes)
        return ev

    def slot(self, name):
        return None

    def wait_all(self, e, resources):
        for r in resources:
            if r.w is not None:
                self._wait(e, r.w)
            for ev in r.r.values():
                self._wait(e, ev)


class Ring:
    def __init__(self, kb, name, n, make):
        self.items = []
        for i in range(n):
            self.items.append((make("%s%d" % (name, i)), Res("%s%d" % (name, i)), LazySlot(kb, "dq_%s%d" % (name, i))))
        self.i = 0

    def next(self):
        it = self.items[self.i % len(self.items)]
        self.i += 1
        return it


class LazySlot:
    def __init__(self, kb, name):
        self.kb = kb
        self.name = name
        self.s = None

    def bump(self):
        if self.s is None:
            self.s = Slot(self.kb, self.name)
        return self.s.bump()


def build_nc(stop=None):
    nc = bass.Bass("TRN2", target_bir_lowering=False)
    kb = KB(nc)
    dbg_row = [0]

    class Scope(ExitStack):
        def __exit__(self, *a):
            r = super().__exit__(*a)
            if a[0] is None:
                kb.barrier()
            return r

    def finish(items):
        sl = kb.slot("dq_dbg")
        rs = []
        for ap, res, p, n in items:
            r0 = dbg_row[0]
            kb.dma("pool", y_d.ap()[r0:r0 + p, 0:n], ap, sl, reads=[res])
            dbg_row[0] += 128
            rs.append(res)
        kb.wait_all("pool", rs)
        return nc

    def din(name, shape):
        return nc.dram_tensor(name, list(shape), F32, kind="ExternalInput")

    xb_d = din("xb", [S, D])
    xo_d = din("xo", [NSLOT, 130, D])
    c8_d = din("c8", [128, 8])
    wada_d = din("w_ada", [D, 6 * D])
    badaT_d = din("b_adaT", [128, 48])
    bada_d = din("b_ada", [1, 6 * D])
    g1T_d = din("g1T", [128, 8])
    g2T_d = din("g2T", [128, 8])
    gf_d = din("gf", [1, D])
    win_d = din("w_in", [D, IN_COLS])
    relb_d = din("relb", [32, 8])
    ej_d = din("ej", [32, TABW])
    convw_d = din("convw", [128, 4, 3])
    wab_d = din("w_ab", [512, D])
    wcbr_d = din("w_cbr", [512, D])
    wout_d = din("w_out", [D, D])
    wr_d = din("wr", [D, 36])
    br_d = din("br", [1, 36])
    wge_d = din("wge", [32, D, 512])
    wue_d = din("wue", [32, D, 512])
    wde_d = din("wde", [32, 512, D])
    tq_d = din("tq", [128, 1])
    hmask_d = din("hmask", [128, NSLOT])
    y_d = nc.dram_tensor("y", [NSLOT * 128, D], F32, kind="ExternalOutput")
    scr_tab = nc.dram_tensor("scr_tab", [8, TABW], F32, kind="Internal")
    scr_q = nc.dram_tensor("scr_q", [NSLOT, 128, 4 * 128], BF16, kind="Internal")
    scr_qi = nc.dram_tensor("scr_qi", [NSLOT, 128, 4 * 128], BF16, kind="Internal")
    scr_o = nc.dram_tensor("scr_o", [NSLOT, 128, 512], BF16, kind="Internal")
    r_scr_tab, r_scr_q, r_scr_qi, r_scr_o = Res(), Res(), Res(), Res()

    win_v = win_d.ap().rearrange("(k p) n -> p k n", p=128)

    PB = [nc.alloc_psum_tensor("pb%d" % i, [128, 512], F32) for i in range(8)]
    rPB = [Res("pb%d" % i) for i in range(8)]

    def pbf(i):
        return PB[i][:].bitcast(BF16)

    es_all = ExitStack()

    def SB(es, name, shape, dt):
        return es.enter_context(nc.sbuf_tensor("sb_" + name, list(shape), dt))

    ID32 = SB(es_all, "ID32", [128, 128], F32)
    ID = SB(es_all, "ID", [128, 128], BF16)
    JJ = SB(es_all, "JJ", [128, 128], F32)
    JK = SB(es_all, "JK", [128, 8], F32)
    junk_ap = JK[:].ap
    rJK = Res("junk")

    def junk(p, n, col=0):
        return bass.AP(JK, col, [[junk_ap[0][0], p], [0, n]])

    modT = SB(es_all, "modT", [128, 48], F32)
    s1 = SB(es_all, "s1", [128, 8], F32)
    s2 = SB(es_all, "s2", [128, 8], F32)
    cact = SB(es_all, "cact", [128, 8], F32)
    epsb = SB(es_all, "epsb", [128, 1], F32)
    tq = SB(es_all, "tq", [128, 1], F32)
    hmask = SB(es_all, "hmask", [128, NSLOT], F32)
    wi_all = SB(es_all, "wi_all", [128, NSLOT, 8], F32)
    sg_all = SB(es_all, "sg_all", [128, NSLOT, 8], F32)
    rC = Res("consts")
    rMOD = Res("mod")
    cs = kb.slot("dq_const")

    kb.op("pool", lambda e: e.memset(ID32[:], 0.0), writes=[rC])
    kb.op("pool", lambda e: e.affine_select(out=ID32[:], in_=ID32[:], compare_op=ALU.not_equal, fill=1.0, base=0,
                                            pattern=[[-1, 128]], channel_multiplier=1), reads=[rC], writes=[rC])
    kb.op("pool", lambda e: e.memset(JJ[:], 0.0), reads=[rC], writes=[rC])
    kb.op("pool", lambda e: e.affine_select(out=JJ[:], in_=JJ[:], compare_op=ALU.not_equal, fill=1.0, base=-127,
                                            pattern=[[1, 128]], channel_multiplier=1), reads=[rC], writes=[rC])
    kb.op("dve", lambda e: e.tensor_copy(out=ID[:], in_=ID32[:]), reads=[rC], writes=[rC])
    kb.op("pool", lambda e: e.memset(epsb[:], EPS), reads=[rC], writes=[rC])
    kb.dma("sp", tq[:], tq_d.ap(), cs, writes=[rC])
    kb.dma("sp", hmask[:], hmask_d.ap(), cs, writes=[rC])
    kb.dma("sp", cact[:], c8_d.ap(), cs, writes=[rC])
    kb.dma("sp", modT[:], badaT_d.ap(), cs, writes=[rMOD])
    kb.dma("sp", s1[:], g1T_d.ap(), cs, writes=[rMOD])
    kb.dma("sp", s2[:], g2T_d.ap(), cs, writes=[rMOD])
    kb.op("act", lambda e: e.activation(out=cact[:], in_=cact[:], func=AF.Silu), reads=[rC], writes=[rC])

    if stop == "c0":
        return finish([(cact[:], rC, 128, 8), (ID32[:], rC, 128, 128), (JJ[:], rC, 128, 128), (modT[:], rMOD, 128, 48)])
    wada_v = wada_d.ap().rearrange("(k p) n -> k p n", p=128)

    def gate_bc(gb, rgb, part):
        kb.barrier()
        with Scope() as es:
            wa = Ring(kb, "wg%d" % part, 2, lambda n: SB(es, n, [128, D], F32))
            cbr = Ring(kb, "cb%d" % part, 2, lambda n: SB(es, n, [128, 128], F32))
            kb.dma("sp", gb[:], bada_d.ap()[0:1, part * D:(part + 1) * D].partition_broadcast(128), cs, writes=[rgb])
            for k in range(8):
                wt, rw, sl = wa.next()
                kb.dma("sp", wt[:], wada_v[k][:, part * D:(part + 1) * D], sl, writes=[rw])
                cbk, rcbk, _ = cbr.next()
                kb.op("dve", lambda e: e.tensor_copy(out=cbk[:], in_=cact[:, k:k + 1].to_broadcast([128, 128])), reads=[rC], writes=[rcbk])
                for n in range(2):
                    kb.op("pe", lambda e: e.matmul(PB[n][:], lhsT=cbk[:], rhs=wt[:, n * 512:(n + 1) * 512], start=(k == 0), stop=(k == 7)),
                          reads=[rw, rcbk], writes=[rPB[n]])
            for n in range(2):
                kb.op("dve", lambda e: e.tensor_tensor(out=gb[:, n * 512:(n + 1) * 512], in0=gb[:, n * 512:(n + 1) * 512], in1=PB[n][:], op=ALU.add),
                      reads=[rPB[n], rgb], writes=[rgb])

    kb.barrier()
    with Scope() as es:
        gtmp = SB(es, "gtmp", [128, D], F32)
        dtmp = SB(es, "dtmp", [128, 8, 128], F32)
        rgt = Res("gtmp")
        for part in (0, 1, 3, 4):
            gate_bc(gtmp, rgt, part)
            kb.op("dve", lambda e: e.tensor_tensor(out=dtmp[:], in0=gtmp[:].rearrange("p (c q) -> p c q", c=8),
                                                   in1=ID32[:].unsqueeze(1).to_broadcast([128, 8, 128]), op=ALU.mult), reads=[rgt, rC], writes=[rgt])
            kb.op("dve", lambda e: e.tensor_reduce(out=modT[:, part * 8:(part + 1) * 8], in_=dtmp[:], axis=AX.X, op=ALU.add), reads=[rgt], writes=[rMOD])
        kb.op("dve", lambda e: e.scalar_tensor_tensor(out=s1[:], in0=modT[:, 8:16], scalar=1.0, in1=s1[:], op0=ALU.add, op1=ALU.mult),
              reads=[rMOD], writes=[rMOD])
        kb.op("dve", lambda e: e.scalar_tensor_tensor(out=s2[:], in0=modT[:, 32:40], scalar=1.0, in1=s2[:], op0=ALU.add, op1=ALU.mult),
              reads=[rMOD], writes=[rMOD])
    if stop == "p0":
        return finish([(modT[:], rMOD, 128, 48), (s1[:], rMOD, 128, 8), (s2[:], rMOD, 128, 8), (cact[:], rC, 128, 8)])
    sh1 = modT[:, 0:8]
    sh2 = modT[:, 24:32]

    def norm_stats(X, rX, p, xn, rxn, st, rst, on_pool=False):
        kb.op("act", lambda e: e.activation(out=xn[0:p, :], in_=X, func=AF.Square, accum_out=st[0:p, 0:1]),
              reads=[rX], writes=[rst, rxn])
        kb.op("act", lambda e: e.activation(out=st[0:p, 1:2], in_=st[0:p, 0:1], func=AF.Ln, scale=1.0 / D, bias=epsb[0:p, 0:1]), reads=[rst, rC], writes=[rst])
        kb.op("act", lambda e: e.activation(out=st[0:p, 2:3], in_=st[0:p, 1:2], func=AF.Exp, scale=-0.5), reads=[rst], writes=[rst])
        kb.op("dve", lambda e: e.tensor_scalar(out=xn[0:p, :], in0=X, scalar1=st[0:p, 2:3], scalar2=None, op0=ALU.mult),
              reads=[rX, rst], writes=[rxn])

    def norm_T(X, rX, p, xn, rxn, st, rst, hT_out, rhT, sc, sh, pbank, on_pool=False):
        norm_stats(X, rX, p, xn, rxn, st, rst, on_pool)
        norm_tr(xn, rxn, p, hT_out, rhT, sc, sh, pbank)

    def norm_tr(xn, rxn, p, hT_out, rhT, sc, sh, pbank):
        for c in range(8):
            if p == 128:
                kb.op("pe", lambda e: e.transpose(out=pbf(pbank)[:, c * 128:(c + 1) * 128], in_=xn[:, c * 128:(c + 1) * 128], identity=ID[:]),
                      reads=[rxn, rC], writes=[rPB[pbank]])
        for c in range(8):
            src = pbf(pbank)[:, c * 128:(c + 1) * 128]
            if c % 2 == 0:
                kb.op("act", lambda e: e.activation(out=hT_out(c), in_=src, func=AF.Identity, scale=sc[:, c:c + 1], bias=sh[:, c:c + 1]),
                      reads=[rPB[pbank], rMOD], writes=[rhT])
            else:
                kb.op("dve", lambda e: e.tensor_scalar(out=hT_out(c), in0=src, scalar1=sc[:, c:c + 1], scalar2=sh[:, c:c + 1], op0=ALU.mult, op1=ALU.add),
                      reads=[rPB[pbank], rMOD], writes=[rhT])

    TAB = SB(es_all, "TAB", [128, 5, 8, 128], BF16)
    rTAB = Res("TAB")
    PEN = SB(es_all, "PEN", [128, 512], BF16)
    kb.barrier()
    with Scope() as es2:
        relb = SB(es2, "relb", [32, 8], F32)
        ej = SB(es2, "ej", [32, TABW], F32)
        rtab = SB(es2, "rtab", [8, TABW], F32)
        XZ = SB(es2, "XZ", [128, 8, 128], F32)
        rT0, rXZ = Res(), Res()
        ts_ = kb.slot("dq_tab")
        kb.dma("sp", relb[:], relb_d.ap(), ts_, writes=[rT0])
        kb.dma("sp", ej[:], ej_d.ap(), ts_, writes=[rT0])
        for c0 in range(0, TABW, 512):
            w = min(512, TABW - c0)
            kb.op("pe", lambda e: e.matmul(PB[0][0:8, 0:w], lhsT=relb[:], rhs=ej[:, c0:c0 + w], start=True, stop=True),
                  reads=[rT0], writes=[rPB[0]])
            kb.op("act", lambda e: e.activation(out=rtab[:, c0:c0 + w], in_=PB[0][0:8, 0:w], func=AF.Exp), reads=[rPB[0]], writes=[rT0])
        kb.dma("sp", scr_tab.ap(), rtab[:], ts_, reads=[rT0], writes=[r_scr_tab])
        for zi in range(5):
            toe = bass.AP(scr_tab, zi * 256, [[1, 128], [TABW, 8], [1, 128]])
            kb.dma("sp", XZ[:], toe, ts_, reads=[r_scr_tab], writes=[rXZ])
            for n in range(2):
                kb.op("pe", lambda e: e.matmul(PB[1 + n][:], lhsT=JJ[:], rhs=XZ[:].rearrange("p h q -> p (h q)")[:, n * 512:(n + 1) * 512], start=True, stop=True),
                      reads=[rXZ, rC], writes=[rPB[1 + n]])
                kb.op("dve", lambda e: e.tensor_copy(out=TAB[:, zi, n * 4:(n + 1) * 4, :], in_=PB[1 + n][:].rearrange("p (h q) -> p h q", h=4)),
                      reads=[rPB[1 + n]], writes=[rTAB])
        IOT = SB(es2, "IOT", [128, 512], F32)
        kb.op("pool", lambda e: e.iota(IOT[:], pattern=[[1, 512]], base=0, channel_multiplier=0, allow_small_or_imprecise_dtypes=True), writes=[rTAB])
        kb.op("dve", lambda e: e.tensor_scalar(out=PEN[:], in0=IOT[:], scalar1=tq[:, 0:1], scalar2=NEG, op0=ALU.is_gt, op1=ALU.mult),
              reads=[rTAB, rC], writes=[rTAB])


    kb.barrier()
    es_att = ExitStack()
    KT = SB(es_att, "KT", [128, 4, S], BF16)
    V = SB(es_att, "V", [128, NT, 8, 65], BF16)
    KI = SB(es_att, "KI", [128, S], BF16)
    rKT, rV, rKI = Res("KT"), Res("V"), Res("KI")
    kb.op("pool", lambda e: e.memset(V[:, :, :, 64:65], 1.0), writes=[rV])

    kb.barrier()
    with Scope() as es:
        Wk = SB(es, "Wk", [128, 8, 512], BF16)
        Wv = SB(es, "Wv", [128, 8, 512], BF16)
        Wki = SB(es, "Wki", [128, 8, 128], BF16)
        rW = Res("W1a")
        ws = kb.slot("dq_w1a")
        kb.dma("pool", Wk[:], win_v[:, :, C_K:C_K + 512], ws, writes=[rW])
        kb.dma("pool", Wv[:], win_v[:, :, C_V:C_V + 512], ws, writes=[rW])
        kb.dma("pool", Wki[:, :, 0:64], win_v[:, :, C_KI:C_KI + 64], ws, writes=[rW])
        kb.dma("pool", Wki[:, :, 64:128], win_v[:, :, C_KI:C_KI + 64], ws, writes=[rW])
        xr = Ring(kb, "x", 2, lambda n: SB(es, n, [128, D], F32))
        xnr = Ring(kb, "xn", 2, lambda n: SB(es, n, [128, D], BF16))
        str_ = Ring(kb, "st", 2, lambda n: SB(es, n, [128, 4], F32))
        h4r = Ring(kb, "h4", 2, lambda n: SB(es, n, [128, 8, 512], BF16))
        xb_v = xb_d.ap().rearrange("(t p) n -> t p n", p=128)
        ev_i = 0
        pre = {}

        def do_stats(t):
            X, rX, sl = xr.next()
            kb.dma("sp", X[:], xb_v[t], sl, writes=[rX])
            xn, rxn, _ = xnr.next()
            st, rst, _ = str_.next()
            norm_stats(X[:], rX, 128, xn, rxn, st, rst, on_pool=(t % 2 == 1))
            pre[t] = (xn, rxn)

        do_stats(0)
        for g in range(NT // 4):
            h4, rh4, _ = h4r.next()
            for tt in range(4):
                t = g * 4 + tt
                if t + 1 < NT:
                    do_stats(t + 1)
                xn, rxn = pre.pop(t)
                norm_tr(xn, rxn, 128, lambda c: h4[:, c, tt * 128:(tt + 1) * 128], rh4, s1, sh1, 6 + (t % 2))
            for fc in range(5):
                bank = fc % 4
                for kc in range(8):
                    lw = Wk[:, kc, fc * 128:(fc + 1) * 128] if fc < 4 else Wki[:, kc, :]
                    kb.op("pe", lambda e: e.matmul(PB[bank][:], lhsT=lw, rhs=h4[:, kc, :], start=(kc == 0), stop=(kc == 7)),
                          reads=[rW, rh4], writes=[rPB[bank]])
                dst = KT[:, fc, g * 512:(g + 1) * 512] if fc < 4 else KI[:, g * 512:(g + 1) * 512]
                rd = rKT if fc < 4 else rKI
                if ev_i % 2 == 0:
                    kb.op("act", lambda e: e.copy(out=dst, in_=PB[bank][:]), reads=[rPB[bank]], writes=[rd])
                else:
                    kb.op("dve", lambda e: e.tensor_copy(out=dst, in_=PB[bank][:]), reads=[rPB[bank]], writes=[rd])
                ev_i += 1
            for tt in range(4):
                t = g * 4 + tt
                bank = 4 + (tt % 2)
                for kc in range(8):
                    kb.op("pe", lambda e: e.matmul(PB[bank][:], lhsT=h4[:, kc, tt * 128:(tt + 1) * 128], rhs=Wv[:, kc, :],
                                                   start=(kc == 0), stop=(kc == 7)), reads=[rW, rh4], writes=[rPB[bank]])
                src = PB[bank][:].rearrange("p (h d) -> p h d", h=8)
                if ev_i % 2 == 0:
                    kb.op("act", lambda e: e.copy(out=V[:, t, :, 0:64], in_=src), reads=[rPB[bank]], writes=[rV])
                else:
                    kb.op("dve", lambda e: e.tensor_copy(out=V[:, t, :, 0:64], in_=src), reads=[rPB[bank]], writes=[rV])
                ev_i += 1

    if stop == "1a":
        return finish([(TAB[:, 1, 0, :], rTAB, 128, 128), (TAB[:, 0, 3, :], rTAB, 128, 128), (PEN[:], rTAB, 128, 512),
                       (KT[:, 0, 0:1024], rKT, 128, 1024), (KT[:, 3, 7168:8192], rKT, 128, 1024),
                       (V[:, 5, :, :].rearrange("p h d -> p (h d)"), rV, 128, 520), (KI[:, 0:1024], rKI, 128, 1024)])
    kb.barrier()
    with Scope() as es:
        Wq = SB(es, "Wq", [128, 8, 512], BF16)
        Wqi = SB(es, "Wqi", [128, 8, 512], BF16)
        Wwi = SB(es, "Wwi", [128, 8, 8], BF16)
        rW = Res("W1b")
        ws = kb.slot("dq_w1b")
        kb.dma("pool", Wq[:], win_v[:, :, C_Q:C_Q + 512], ws, writes=[rW])
        kb.dma("pool", Wqi[:], win_v[:, :, C_QI:C_QI + 512], ws, writes=[rW])
        kb.dma("pool", Wwi[:], win_v[:, :, C_WI:C_WI + 8], ws, writes=[rW])
        xr = Ring(kb, "xo", 2, lambda n: SB(es, n, [128, D], F32))
        xnr = Ring(kb, "xno", 2, lambda n: SB(es, n, [128, D], BF16))
        str_ = Ring(kb, "sto", 2, lambda n: SB(es, n, [128, 4], F32))
        htr = Ring(kb, "hto", 2, lambda n: SB(es, n, [128, 8, 128], BF16))
        qsr = Ring(kb, "qs", 2, lambda n: SB(es, n, [128, 2, 512], BF16))
        ss = kb.slot("dq_spill")
        pre1b = {}

        def stats1b(i):
            X, rX, sl = xr.next()
            kb.dma("sp", X[:], xo_d.ap()[i, 2:130, :], sl, writes=[rX])
            xn, rxn, _ = xnr.next()
            st, rst, _ = str_.next()
            norm_stats(X[:], rX, 128, xn, rxn, st, rst)
            pre1b[i] = (xn, rxn)

        stats1b(0)
        for i in range(NSLOT):
            if i + 1 < NSLOT:
                stats1b(i + 1)
            xn, rxn = pre1b.pop(i)
            hT, rhT, _ = htr.next()
            norm_tr(xn, rxn, 128, lambda c: hT[:, c, :], rhT, s1, sh1, 6 + (i % 2))
            qs, rqs, _ = qsr.next()
            for wi_, (Wt, scl) in enumerate(((Wq, 0.125), (Wqi, 1.0))):
                bank = wi_
                for fc in range(4):
                    for kc in range(8):
                        kb.op("pe", lambda e: e.matmul(PB[bank][:, fc * 128:(fc + 1) * 128], lhsT=Wt[:, kc, fc * 128:(fc + 1) * 128],
                                                       rhs=hT[:, kc, :], start=(kc == 0), stop=(kc == 7)), reads=[rW, rhT], writes=[rPB[bank]])
                kb.op("act", lambda e: e.activation(out=qs[:, wi_, :], in_=PB[bank][:], func=AF.Copy, scale=scl),
                      reads=[rPB[bank]], writes=[rqs])
            kb.dma("sp", scr_q.ap()[i], qs[:, 0, :], ss, reads=[rqs], writes=[r_scr_q])
            kb.dma("sp", scr_qi.ap()[i], qs[:, 1, :], ss, reads=[rqs], writes=[r_scr_qi])
            for kc in range(8):
                kb.op("pe", lambda e: e.matmul(PB[2][:, 0:8], lhsT=hT[:, kc, :], rhs=Wwi[:, kc, :], start=(kc == 0), stop=(kc == 7)),
                      reads=[rW, rhT], writes=[rPB[2]])
            kb.op("act", lambda e: e.activation(out=wi_all[:, i, :], in_=PB[2][:, 0:8], func=AF.Abs, scale=0.125 * (8 ** -0.5)),
                  reads=[rPB[2]], writes=[rC])
            kb.op("dve", lambda e: e.tensor_scalar(out=sg_all[:, i, :], in0=PB[2][:, 0:8], scalar1=0.0, scalar2=2.0,
                                                   op0=ALU.is_ge, op1=ALU.mult), reads=[rPB[2]], writes=[rC])
            kb.op("dve", lambda e: e.tensor_scalar(out=sg_all[:, i, :], in0=sg_all[:, i, :], scalar1=-1.0, scalar2=None, op0=ALU.add),
                  reads=[rC], writes=[rC])

    if stop == "1b":
        return finish([(wi_all[:, 1, :], rC, 128, 8), (sg_all[:, 1, :], rC, 128, 8)])
    kb.barrier()
    with Scope() as es:
        SC = SB(es, "SC", [128, S], F32)
        rSC = Res("SC")
        qTr = Ring(kb, "qT", 1, lambda n: SB(es, n, [128, 4, 128], BF16))
        qiTr = Ring(kb, "qiT", 1, lambda n: SB(es, n, [128, 4, 128], BF16))
        DG = SB(es, "DG", [128, 8, 128], BF16)
        rDG = Res("DG")
        yr = Ring(kb, "yh", 3, lambda n: SB(es, n, [128, 512], BF16))
        BS = SB(es, "BS", [128, 16], F32)
        rBS = Res("BS")
        THD = SB(es, "THD", [128, 128], F32)
        THB = SB(es, "THB", [128, 128], F32)
        ONES = SB(es, "ONES", [128, 128], F32)
        rTH = Res("TH")
        kb.op("pool", lambda e: e.memset(ONES[:], 1.0), writes=[rTH])
        THR = SB(es, "THR", [128, 1], F32)
        BA = SB(es, "BA", [128, 16], F32)
        rTHR, rBA = Res("THR"), Res("BA")
        P2 = SB(es, "P2", [128, NIT + 2], F32)
        RN = SB(es, "RN", [128, NIT + 2], F32)
        for m_ in range(NIT + 2):
            kb.op("pool", lambda e: e.memset(P2[:, m_:m_ + 1], 2.0 ** -m_), writes=[rTH])
        ZR = SB(es, "ZR", [128, 260], BF16)
        kb.op("pool", lambda e: e.memset(ZR[:], 0.0), writes=[rTH])
        selr = Ring(kb, "sel", 3, lambda n: SB(es, n, [128, 128], BF16))
        EB = SB(es, "EB", [128, 2, 8, 128], BF16)
        rEB = [Res("E0"), Res("E1")]
        CJ = EB[:].rearrange("p a h q -> p (a h q)").bitcast(mybir.dt.uint8)

        class _ER:
            n = 0

            def next(self):
                k = self.n % 2
                self.n += 1
                return EB[:, k], rEB[k], None
        Er = _ER()
        OS = SB(es, "OS", [128, 8, 64], BF16)
        RS = SB(es, "RS", [128, 8], F32)
        rOS = Res("OS")

        for i in range(NSLOT):
            nch = i + 1
            Sc = nch * 512
            ntile = 4 * nch
            qT, rqT, sl1 = qTr.next()
            qiT, rqiT, sl2 = qiTr.next()
            kb.dma("sp", qT[:], scr_q.ap()[i].rearrange("p (c q) -> p c q", c=4), sl1, reads=[r_scr_q], writes=[rqT])
            kb.dma("sp", qiT[:], scr_qi.ap()[i].rearrange("p (c q) -> p c q", c=4), sl2, reads=[r_scr_qi], writes=[rqiT])
            for h in range(8):
                kb.op("pool", lambda e: e.tensor_scalar(out=DG[:, h, :], in0=ID[:], scalar1=sg_all[:, i, h:h + 1], scalar2=None, op0=ALU.mult),
                      reads=[rC], writes=[rDG])
            def dots_mm(ch, h):
                bank = h % 4
                hp = (h % 2) * 64
                kb.op("pe", lambda e: e.matmul(PB[bank][:], lhsT=qiT[hp:hp + 64, h // 2, :], rhs=KI[hp:hp + 64, ch * 512:(ch + 1) * 512],
                                               start=True, stop=True), reads=[rqiT, rKI], writes=[rPB[bank]])

            for ch in range(nch):
                ab = 6 + ch % 2
                dots_mm(ch, 0)
                dots_mm(ch, 1)
                dots_mm(ch, 2)
                for h in range(8):
                    bank = h % 4
                    y, ry, _ = yr.next()
                    if h % 2 == 0:
                        kb.op("act", lambda e: e.activation(out=y[:], in_=PB[bank][:], func=AF.Relu, scale=wi_all[:, i, h:h + 1]),
                              reads=[rPB[bank], rC], writes=[ry])
                    else:
                        kb.op("dve", lambda e: e.tensor_scalar(out=y[:], in0=PB[bank][:], scalar1=0.0, scalar2=wi_all[:, i, h:h + 1],
                                                               op0=ALU.max, op1=ALU.mult), reads=[rPB[bank], rC], writes=[ry])
                    if h + 3 < 8:
                        dots_mm(ch, h + 3)
                    kb.op("pe", lambda e: e.matmul(PB[ab][:], lhsT=DG[:, h, :], rhs=y[:], start=(h == 0), stop=(h == 7)),
                          reads=[rDG, ry], writes=[rPB[ab]])
                if ch == nch - 1:
                    kb.op("dve", lambda e: e.tensor_reduce(out=BS[:, 2:3], in_=PB[ab][:], axis=AX.X, op=ALU.min), reads=[rPB[ab]], writes=[rBS])
                    kb.op("dve", lambda e: e.tensor_tensor(out=SC[:, ch * 512:(ch + 1) * 512], in0=PB[ab][:], in1=PEN[:], op=ALU.add),
                          reads=[rPB[ab], rTAB], writes=[rSC])
                else:
                    kb.op("act", lambda e: e.copy(out=SC[:, ch * 512:(ch + 1) * 512], in_=PB[ab][:]), reads=[rPB[ab]], writes=[rSC])
            if stop == "2a":
                return finish([(SC[:, 0:512], rSC, 128, 512), (BS[:], rBS, 128, 16)])
            segs = [(a_, min(a_ + 4096, Sc)) for a_ in range(0, Sc, 4096)]
            for si, (a_, b_) in enumerate(segs):
                kb.op("dve", lambda e: e.tensor_reduce(out=BS[:, 0:1] if si == 0 else BS[:, 8:9], in_=SC[:, a_:b_], axis=AX.X, op=ALU.max), reads=[rSC], writes=[rBS])
                if si > 0:
                    kb.op("dve", lambda e: e.tensor_tensor(out=BS[:, 0:1], in0=BS[:, 0:1], in1=BS[:, 8:9], op=ALU.max), reads=[rBS], writes=[rBS])
            if nch > 1:
                fsegs = [(a_, min(a_ + 4096, Sc - 512)) for a_ in range(0, Sc - 512, 4096)]
                for (a_, b_) in fsegs:
                    kb.op("dve", lambda e: e.tensor_reduce(out=BS[:, 7:8], in_=SC[:, a_:b_], axis=AX.X, op=ALU.min), reads=[rSC], writes=[rBS])
                    kb.op("dve", lambda e: e.tensor_tensor(out=BS[:, 2:3], in0=BS[:, 2:3], in1=BS[:, 7:8], op=ALU.min), reads=[rBS], writes=[rBS])
            kb.op("dve", lambda e: e.tensor_tensor(out=BS[:, 3:4], in0=BS[:, 0:1], in1=BS[:, 2:3], op=ALU.subtract), reads=[rBS], writes=[rBS])
            kb.op("dve", lambda e: e.tensor_scalar(out=BS[:, 3:4], in0=BS[:, 3:4], scalar1=1.001, scalar2=1e-6, op0=ALU.mult, op1=ALU.add),
                  reads=[rBS], writes=[rBS])
            kb.op("dve", lambda e: e.tensor_scalar(out=RN[:], in0=P2[:], scalar1=BS[:, 3:4], scalar2=None, op0=ALU.mult), reads=[rBS, rTH], writes=[rBS])
            kb.op("dve", lambda e: e.scalar_tensor_tensor(out=THR[:], in0=BS[:, 3:4], scalar=-0.5, in1=BS[:, 0:1], op0=ALU.mult, op1=ALU.add),
                  reads=[rBS], writes=[rTHR])
            use_act = Sc >= 1536
            Sd = (int(round(0.64 * Sc / 128)) * 128) if use_act else Sc
            dsegs = [(a_, min(a_ + 4096, Sd)) for a_ in range(0, Sd, 4096)]
            asegs = [(a_, min(a_ + 512, Sc)) for a_ in range(Sd, Sc, 512)] if use_act else []
            n_act = Sc - Sd
            for it in range(NIT):
                for k_, (a_, b_) in enumerate(asegs):
                    kb.op("act", lambda e: e.activation(out=PB[k_ % 4][:, 0:b_ - a_], in_=SC[:, a_:b_], func=AF.Sign, scale=-1.0, bias=THR[:, 0:1],
                                                        accum_out=BA[:, k_:k_ + 1]), reads=[rSC, rTHR], writes=[rBA, rPB[k_ % 4]])
                for si, (a_, b_) in enumerate(dsegs):
                    kb.op("dve", lambda e: e.tensor_scalar(out=CJ[:, 0:b_ - a_], in0=SC[:, a_:b_], scalar1=THR[:, 0:1], scalar2=0.0,
                                                           op0=ALU.is_ge, op1=ALU.add, accum_out=BS[:, 5:6] if si == 0 else BS[:, 10:11]),
                          reads=[rSC, rTHR], writes=[rBS, rEB[0], rEB[1]])
                    if si > 0:
                        kb.op("dve", lambda e: e.tensor_tensor(out=BS[:, 5:6], in0=BS[:, 5:6], in1=BS[:, 10:11], op=ALU.add), reads=[rBS], writes=[rBS])
                if use_act:
                    if len(asegs) > 1:
                        kb.op("dve", lambda e: e.tensor_reduce(out=BS[:, 9:10], in_=BA[:, 0:len(asegs)], axis=AX.X, op=ALU.add), reads=[rBA], writes=[rBS])
                        sa = BS[:, 9:10]
                    else:
                        sa = BA[:, 0:1]
                    kb.op("dve", lambda e: e.scalar_tensor_tensor(out=BS[:, 5:6], in0=sa, scalar=-0.5, in1=BS[:, 5:6], op0=ALU.mult, op1=ALU.add),
                          reads=[rBS, rBA], writes=[rBS])
                kb.op("dve", lambda e: e.tensor_scalar(out=BS[:, 6:7], in0=BS[:, 5:6], scalar1=255.5 - 0.5 * n_act, scalar2=-0.5, op0=ALU.is_ge, op1=ALU.add),
                      reads=[rBS], writes=[rBS])
                kb.op("dve", lambda e: e.scalar_tensor_tensor(out=THR[:], in0=BS[:, 6:7], scalar=RN[:, it + 1:it + 2], in1=THR[:], op0=ALU.mult, op1=ALU.add),
                      reads=[rBS, rTHR], writes=[rTHR])
            kb.op("dve", lambda e: e.tensor_tensor(out=BS[:, 1:2], in0=THR[:], in1=RN[:, NIT + 1:NIT + 2], op=ALU.subtract), reads=[rBS, rTHR], writes=[rBS])
            if stop == "2b":
                return finish([(SC[:, 0:512], rSC, 128, 512), (BS[:], rBS, 128, 16)])
            kb.op("dve", lambda e: e.tensor_scalar(out=THD[:], in0=ID32[:], scalar1=BS[:, 1:2], scalar2=None, op0=ALU.mult),
                  reads=[rBS, rC], writes=[rTH])
            kb.op("pe", lambda e: e.matmul(PB[7][:, 0:128], lhsT=ONES[:], rhs=THD[:], start=True, stop=True), reads=[rTH], writes=[rPB[7]])
            kb.op("act", lambda e: e.copy(out=THB[:], in_=PB[7][:, 0:128]), reads=[rPB[7]], writes=[rTH])
            for hh in range(2):
                kb.op("pe", lambda e: e.matmul(PB[4 + hh][:, 0:260], lhsT=ID[:], rhs=ZR[:], start=True, stop=False, skip_group_check=True),
                      reads=[rC, rTH], writes=[rPB[4 + hh]])
            st_sel = {}

            def stage_a(kt):
                sq = (kt % 4) * 128
                kb.op("pe", lambda e: e.transpose(out=PB[6][:, sq:sq + 128], in_=SC[:, kt * 128:(kt + 1) * 128], identity=ID32[:]),
                      reads=[rSC, rC], writes=[rPB[6]])
                sel, rsel, _ = selr.next()
                kb.op("dve", lambda e: e.tensor_tensor(out=sel[:], in0=PB[6][:, sq:sq + 128], in1=THB[:], op=ALU.is_ge),
                      reads=[rPB[6], rTH], writes=[rsel])
                st_sel[kt] = (sel, rsel)
                lb = (kt % 2) * 2
                for h in range(8):
                    hp = (h % 2) * 64
                    kb.op("pe", lambda e: e.matmul(PB[lb + h % 2][:, (h // 2) * 128:(h // 2 + 1) * 128], lhsT=KT[hp:hp + 64, h // 2, kt * 128:(kt + 1) * 128],
                                                   rhs=qT[hp:hp + 64, h // 2, :], start=True, stop=True), reads=[rKT, rqT], writes=[rPB[lb + h % 2]])

            def stage_b(kt):
                sel, rsel = st_sel.pop(kt)
                lb = (kt % 2) * 2
                E, rE, _ = Er.next()
                for hh in range(2):
                    kb.op("act", lambda e: e.activation(out=E[:, hh * 4:(hh + 1) * 4, :], in_=PB[lb + hh][:].rearrange("p (h q) -> p h q", h=4), func=AF.Exp),
                          reads=[rPB[lb + hh]], writes=[rE])
                zi = kt - (4 * i - 1)
                if zi >= 0:
                    kb.op("dve", lambda e: e.tensor_tensor(out=E[:], in0=E[:], in1=TAB[:, zi, :, :], op=ALU.mult), reads=[rE, rTAB], writes=[rE])
                kb.op("dve", lambda e: e.tensor_tensor(out=E[:], in0=E[:], in1=sel[:].unsqueeze(1).to_broadcast([128, 8, 128]), op=ALU.mult),
                      reads=[rE, rsel], writes=[rE])
                for h in range(8):
                    kb.op("pe", lambda e: e.matmul(PB[4 + h // 4][:, (h % 4) * 65:(h % 4 + 1) * 65], lhsT=E[:, (h % 2) * 4 + h // 2, :], rhs=V[:, kt, h, :],
                                                   start=False, stop=(kt == ntile - 1), skip_group_check=True), reads=[rE, rV], writes=[rPB[4 + h // 4]])

            stage_a(0)
            for kt in range(ntile):
                if kt + 1 < ntile:
                    stage_a(kt + 1)
                stage_b(kt)
            for hh in range(2):
                ov = PB[4 + hh][:, 0:260].rearrange("p (h d) -> p h d", h=4)
                kb.op("dve", lambda e: e.reciprocal(out=RS[:, hh * 4:(hh + 1) * 4], in_=ov[:, :, 64]), reads=[rPB[4 + hh]], writes=[rOS])
                kb.op("dve", lambda e: e.tensor_tensor(out=OS[:, hh * 4:(hh + 1) * 4, :], in0=ov[:, :, 0:64],
                                                       in1=RS[:, hh * 4:(hh + 1) * 4].unsqueeze(2).to_broadcast([128, 4, 64]), op=ALU.mult),
                      reads=[rPB[4 + hh], rOS], writes=[rOS])
            kb.dma("sp", scr_o.ap()[i], OS[:].rearrange("p h d -> p (h d)"), ss, reads=[rOS], writes=[r_scr_o])
            if stop == "2_%d" % i:
                return finish([(SC[:, 0:1024], rSC, 128, 1024), (BS[:], rBS, 128, 16), (OS[:].rearrange("p h d -> p (h d)"), rOS, 128, 512),
                               (wi_all[:, i, :], rC, 128, 8), (sg_all[:, i, :], rC, 128, 8), (qT[:, 0, :], rqT, 128, 128), (RS[:], rOS, 128, 8)])
    es_att.close()
    kb.barrier()

    kb.barrier()
    es_h2t = ExitStack()
    H2T = SB(es_h2t, "H2T", [128, 8, NSLOT * 128], BF16)
    rH2T = Res("H2T")
    kb.barrier()
    with Scope() as es:
        Wc = SB(es, "Wc", [128, 8, 1536], BF16)
        Wgl = SB(es, "Wgl", [128, 8, 2048], BF16)
        Wab = SB(es, "Wab", [128, 4, D], BF16)
        Wcb = SB(es, "Wcb", [128, 4, D], BF16)
        cw = SB(es, "cw", [128, 4, 3], F32)
        rW = Res("W3")
        ws = kb.slot("dq_w3")
        kb.dma("pool", Wc[:], win_v[:, :, C_CB:C_CB + 1536], ws, writes=[rW])
        kb.dma("pool", Wgl[:], win_v[:, :, C_GL:C_GL + 2048], ws, writes=[rW])
        kb.dma("pool", Wab[:], wab_d.ap().rearrange("(k p) n -> p k n", p=128), ws, writes=[rW])
        kb.dma("pool", Wcb[:], wcbr_d.ap().rearrange("(k p) n -> p k n", p=128), ws, writes=[rW])
        kb.dma("sp", cw[:], convw_d.ap(), ws, writes=[rW])
        XH = SB(es, "XH", [2, D], F32)
        XNH = SB(es, "XNH", [2, D], BF16)
        STH = SB(es, "STH", [2, 4], F32)
        rXH = Res("XH")
        xr = Ring(kb, "x3", 2, lambda n: SB(es, n, [128, D], F32))
        xn3r = Ring(kb, "XN3", 2, lambda n: SB(es, n, [128, D], BF16))
        st3r = Ring(kb, "ST3", 2, lambda n: SB(es, n, [128, 4], F32))
        HT = SB(es, "HT3", [128, 8, 130], BF16)
        rHT = Res("HT3")
        CCs = SB(es, "CCs", [128, 4, 130], F32)
        U = SB(es, "U", [128, 4, 130], F32)
        CV = SB(es, "CV", [128, 4, 128], F32)
        VT = SB(es, "VT", [128, 4, 128], BF16)
        rU = Res("U")
        rUf = [Res("U%d" % k) for k in range(4)]
        G = SB(es, "G", [128, 2048], F32)
        rG = Res("G")
        M1 = SB(es, "M1", [128, D], F32)
        MB = SB(es, "MB", [128, D], BF16)
        rM = Res("M")
        OSb = SB(es, "OSb", [128, 512], BF16)
        AT = SB(es, "AT", [128, 4, 128], BF16)
        rAT = Res("AT")
        osl = kb.slot("dq_o3")
        pre3 = {}

        def stats3(i):
            X, rX, sl = xr.next()
            kb.dma("sp", X[:], xo_d.ap()[i, 2:130, :], sl, writes=[rX])
            xn, rxn, _ = xn3r.next()
            st, rst, _ = st3r.next()
            norm_stats(X[:], rX, 128, xn, rxn, st, rst)
            pre3[i] = (xn, rxn)

        stats3(0)
        for i in range(NSLOT):
            if i + 1 < NSLOT:
                stats3(i + 1)
            kb.dma("sp", XH[:], xo_d.ap()[i, 0:2, :], None, writes=[rXH])
            kb.dma("sp", OSb[:], scr_o.ap()[i], osl, reads=[r_scr_o], writes=[rAT])
            xn, rxn = pre3.pop(i)
            norm_tr(xn, rxn, 128, lambda c: HT[:, c, 2:130], rHT, s1, sh1, 6)
            kb.op("act", lambda e: e.activation(out=XNH[:], in_=XH[:], func=AF.Square, accum_out=STH[:, 0:1]), reads=[rXH], writes=[rXH])
            kb.op("act", lambda e: e.activation(out=STH[:, 1:2], in_=STH[:, 0:1], func=AF.Ln, scale=1.0 / D, bias=epsb[0:2, 0:1]), reads=[rXH, rC], writes=[rXH])
            kb.op("act", lambda e: e.activation(out=STH[:, 2:3], in_=STH[:, 1:2], func=AF.Exp, scale=-0.5), reads=[rXH], writes=[rXH])
            kb.op("dve", lambda e: e.tensor_scalar(out=XNH[:], in0=XH[:], scalar1=STH[:, 2:3], scalar2=None, op0=ALU.mult), reads=[rXH], writes=[rXH])
            for c in range(8):
                kb.op("pe", lambda e: e.matmul(PB[7][:, c * 2:c * 2 + 2], lhsT=XNH[:, c * 128:(c + 1) * 128], rhs=ID[0:2, 0:2], start=True, stop=True),
                      reads=[rXH, rC], writes=[rPB[7]])
            for c in range(8):
                kb.op("dve", lambda e: e.tensor_scalar(out=HT[:, c, 0:2], in0=PB[7][:, c * 2:c * 2 + 2], scalar1=s1[:, c:c + 1], scalar2=sh1[:, c:c + 1],
                                                       op0=ALU.mult, op1=ALU.add), reads=[rPB[7], rMOD], writes=[rHT])
            for fc in range(4):
                cb0 = (fc % 2) * 3
                for kc in range(8):
                    kb.op("pe", lambda e: e.matmul(PB[cb0][:, 0:130], lhsT=Wc[:, kc, 512 + fc * 128:512 + (fc + 1) * 128], rhs=HT[:, kc, :],
                                                   start=(kc == 0), stop=(kc == 7)), reads=[rW, rHT], writes=[rPB[cb0]])
                for kc in range(8):
                    kb.op("pe", lambda e: e.matmul(PB[cb0 + 1][:, 0:130], lhsT=Wc[:, kc, 1024 + fc * 128:1024 + (fc + 1) * 128], rhs=HT[:, kc, :],
                                                   start=(kc == 0), stop=(kc == 7)), reads=[rW, rHT], writes=[rPB[cb0 + 1]])
                for kc in range(8):
                    kb.op("pe", lambda e: e.matmul(PB[cb0 + 2][:, 0:128], lhsT=Wc[:, kc, fc * 128:(fc + 1) * 128], rhs=HT[:, kc, 2:130],
                                                   start=(kc == 0), stop=(kc == 7)), reads=[rW, rHT], writes=[rPB[cb0 + 2]])
                kb.op("act", lambda e: e.copy(out=CCs[:, fc, :], in_=PB[cb0][:, 0:130]), reads=[rPB[cb0]], writes=[rUf[fc]])
                kb.op("dve", lambda e: e.tensor_tensor(out=U[:, fc, :], in0=CCs[:, fc, :], in1=PB[cb0 + 1][:, 0:130], op=ALU.mult), reads=[rPB[cb0 + 1], rUf[fc]], writes=[rUf[fc]])
                kb.op("dve", lambda e: e.tensor_scalar(out=U[:, fc, 0:2], in0=U[:, fc, 0:2], scalar1=hmask[:, i:i + 1], scalar2=None, op0=ALU.mult),
                      reads=[rUf[fc], rC], writes=[rUf[fc]])
                kb.op("dve", lambda e: e.tensor_scalar(out=CV[:, fc, :], in0=U[:, fc, 0:128], scalar1=cw[:, fc, 0:1], scalar2=None, op0=ALU.mult),
                      reads=[rUf[fc], rW], writes=[rUf[fc]])
                kb.op("dve", lambda e: e.scalar_tensor_tensor(out=CV[:, fc, :], in0=U[:, fc, 1:129], scalar=cw[:, fc, 1:2], in1=CV[:, fc, :],
                                                               op0=ALU.mult, op1=ALU.add), reads=[rUf[fc], rW], writes=[rUf[fc]])
                kb.op("dve", lambda e: e.scalar_tensor_tensor(out=CV[:, fc, :], in0=U[:, fc, 2:130], scalar=cw[:, fc, 2:3], in1=CV[:, fc, :],
                                                               op0=ALU.mult, op1=ALU.add), reads=[rUf[fc], rW], writes=[rUf[fc]])
                kb.op("dve", lambda e: e.tensor_tensor(out=VT[:, fc, :], in0=CV[:, fc, :], in1=PB[cb0 + 2][:, 0:128], op=ALU.mult), reads=[rPB[cb0 + 2], rUf[fc]], writes=[rUf[fc]])
            for n in range(4):
                for kc in range(8):
                    kb.op("pe", lambda e: e.matmul(PB[6 + n % 2][:], lhsT=HT[:, kc, 2:130], rhs=Wgl[:, kc, n * 512:(n + 1) * 512],
                                                   start=(kc == 0), stop=(kc == 7)), reads=[rW, rHT], writes=[rPB[6 + n % 2]])
                kb.op("act", lambda e: e.activation(out=G[:, n * 512:(n + 1) * 512], in_=PB[6 + n % 2][:], func=AF.Sigmoid), reads=[rPB[6 + n % 2]], writes=[rG])
            for c in range(4):
                kb.op("pe", lambda e: e.transpose(out=pbf(5)[:, c * 128:(c + 1) * 128], in_=OSb[:, c * 128:(c + 1) * 128], identity=ID[:]),
                      reads=[rAT, rC], writes=[rPB[5]])
            kb.op("dve", lambda e: e.tensor_copy(out=AT[:].rearrange("p c q -> p (c q)"), in_=pbf(5)[:, 0:512]), reads=[rPB[5]], writes=[rAT])
            for n in range(2):
                for kc in range(4):
                    kb.op("pe", lambda e: e.matmul(PB[0 + n][:], lhsT=AT[:, kc, :], rhs=Wab[:, kc, n * 512:(n + 1) * 512], start=(kc == 0), stop=(kc == 3)),
                          reads=[rW, rAT], writes=[rPB[0 + n]])
                kb.op("dve", lambda e: e.tensor_tensor(out=M1[:, n * 512:(n + 1) * 512], in0=G[:, n * 512:(n + 1) * 512], in1=PB[0 + n][:], op=ALU.mult),
                      reads=[rPB[0 + n], rG], writes=[rM])
            for n in range(2):
                for kc in range(4):
                    kb.op("pe", lambda e: e.matmul(PB[0 + n][:], lhsT=VT[:, kc, :], rhs=Wcb[:, kc, n * 512:(n + 1) * 512], start=(kc == 0), stop=(kc == 3)),
                          reads=[rW] + rUf, writes=[rPB[0 + n]])
                kb.op("dve", lambda e: e.tensor_tensor(out=G[:, D + n * 512:D + (n + 1) * 512], in0=G[:, D + n * 512:D + (n + 1) * 512], in1=PB[0 + n][:], op=ALU.mult),
                      reads=[rPB[0 + n], rG], writes=[rG])
            kb.op("dve", lambda e: e.tensor_tensor(out=MB[:], in0=M1[:], in1=G[:, D:2 * D], op=ALU.add), reads=[rM, rG], writes=[rM])
            for c in range(8):
                kb.op("pe", lambda e: e.transpose(out=pbf(5)[:, c * 128:(c + 1) * 128], in_=MB[:, c * 128:(c + 1) * 128], identity=ID[:]),
                      reads=[rM, rC], writes=[rPB[5]])
            kb.op("act", lambda e: e.copy(out=H2T[:, :, i * 128:(i + 1) * 128], in_=pbf(5).rearrange("p (c q) -> p c q", c=8)),
                  reads=[rPB[5]], writes=[rH2T])

    if stop == "3a":
        return finish([(H2T[:, 0, 0:1024], rH2T, 128, 1024), (H2T[:, 7, 1024:2048], rH2T, 128, 1024)])
    kb.barrier()
    es_moe = ExitStack()
    HR = SB(es_moe, "HR", [128, NSLOT, D], F32)
    GD = SB(es_moe, "GD", [128, NSLOT, 32], F32)
    rHR = [Res("HR%d" % i) for i in range(NSLOT)]
    rGD = Res("GD")
    kb.barrier()
    with Scope() as es:
        Wo = SB(es, "Wo", [128, 8, D], BF16)
        Wr = SB(es, "Wr", [128, 8, 36], F32)
        brb = SB(es, "brb", [128, 36], F32)
        g1bc = SB(es, "g1bc", [128, D], F32)
        rW = Res("W3b")
        rg1 = Res("g1bc")
        ws = kb.slot("dq_w3b")
        kb.dma("pool", Wo[:], wout_d.ap().rearrange("(k p) n -> p k n", p=128), ws, writes=[rW])
        kb.dma("sp", Wr[:], wr_d.ap().rearrange("(k p) n -> p k n", p=128), ws, writes=[rW])
        kb.dma("sp", brb[:], br_d.ap().partition_broadcast(128), ws, writes=[rW])
        gate_bc(g1bc, rg1, 2)
        for kc in range(8):
            kb.op("dve", lambda e: e.tensor_tensor(out=Wo[:, kc, :], in0=Wo[:, kc, :], in1=g1bc[:], op=ALU.mult), reads=[rW, rg1], writes=[rW])
        xr = Ring(kb, "x3b", 2, lambda n: SB(es, n, [128, D], F32))
        ST = SB(es, "ST3b", [128, 4], F32)
        rST = Res()
        H2 = SB(es, "H2", [128, D], F32)
        H2T32 = SB(es, "H2T32", [128, 8, 128], F32)
        rH2 = Res("H2")
        RT = SB(es, "RT", [128, 96], F32)
        rRT = Res("RT")
        def pb_p1(i):
                X, rX, sl = xr.next()
                kb.dma("sp", X[:], xo_d.ap()[i, 2:130, :], sl, writes=[rX])
                for n in range(2):
                    for kc in range(8):
                        kb.op("pe", lambda e: e.matmul(PB[0 + n][:], lhsT=H2T[:, kc, i * 128:(i + 1) * 128], rhs=Wo[:, kc, n * 512:(n + 1) * 512],
                                                       start=(kc == 0), stop=(kc == 7)), reads=[rW, rH2T], writes=[rPB[0 + n]])
                    kb.op("dve", lambda e: e.tensor_tensor(out=HR[:, i, n * 512:(n + 1) * 512], in0=X[:, n * 512:(n + 1) * 512], in1=PB[0 + n][:], op=ALU.add),
                          reads=[rPB[0 + n], rX], writes=[rHR[i]])
                kb.op("act", lambda e: e.activation(out=H2[:], in_=HR[:, i, :], func=AF.Square, accum_out=ST[:, 0:1]), reads=[rHR[i]], writes=[rST, rH2])
                kb.op("act", lambda e: e.activation(out=ST[:, 1:2], in_=ST[:, 0:1], func=AF.Ln, scale=1.0 / D, bias=epsb[:, 0:1]), reads=[rST, rC], writes=[rST])
                kb.op("act", lambda e: e.activation(out=ST[:, 2:3], in_=ST[:, 1:2], func=AF.Exp, scale=-0.5), reads=[rST], writes=[rST])
                kb.op("dve", lambda e: e.tensor_scalar(out=H2[:], in0=HR[:, i, :], scalar1=ST[:, 2:3], scalar2=None, op0=ALU.mult), reads=[rHR[i], rST], writes=[rH2])
                for c in range(8):
                    pb = 6 + c // 4
                    kb.op("pe", lambda e: e.transpose(out=PB[pb][:, (c % 4) * 128:(c % 4 + 1) * 128], in_=H2[:, c * 128:(c + 1) * 128], identity=ID32[:]),
                          reads=[rH2, rC], writes=[rPB[pb]])

        def pb_ev(i):
                for c in range(8):
                    pb = 6 + c // 4
                    src = PB[pb][:, (c % 4) * 128:(c % 4 + 1) * 128]
                    kb.op("act", lambda e: e.activation(out=H2T32[:, c, :], in_=src, func=AF.Identity, scale=s2[:, c:c + 1], bias=sh2[:, c:c + 1]),
                          reads=[rPB[pb], rMOD], writes=[rH2])
                    kb.op("dve", lambda e: e.tensor_copy(out=H2T[:, c, i * 128:(i + 1) * 128], in_=H2T32[:, c, :]), reads=[rH2], writes=[rH2T])

        def pb_rc(i):
                for kc in range(8):
                    kb.op("pe", lambda e: e.matmul(PB[2][:, 0:36], lhsT=H2T32[:, kc, :], rhs=Wr[:, kc, :], start=(kc == 0), stop=(kc == 7)),
                          reads=[rW, rH2], writes=[rPB[2]])
                L = RT[:, 0:36]
                def rt(fn, extra_r=(), e_="dve"):
                    kb.op(e_, fn, reads=[rRT] + list(extra_r), writes=[rRT])
                kb.op("dve", lambda e: e.tensor_tensor(out=L, in0=PB[2][:, 0:36], in1=brb[:], op=ALU.add), reads=[rPB[2], rW], writes=[rRT])
                rt(lambda e: e.tensor_reduce(out=RT[:, 36:37], in_=RT[:, 0:4], axis=AX.X, op=ALU.max))
                rt(lambda e: e.tensor_scalar(out=RT[:, 80:84], in0=RT[:, 0:4], scalar1=RT[:, 36:37], scalar2=None, op0=ALU.is_ge))
                rt(lambda e: e.tensor_scalar(out=RT[:, 37:41], in0=RT[:, 0:4], scalar1=RT[:, 36:37], scalar2=None, op0=ALU.subtract))
                rt(lambda e: e.activation(out=RT[:, 37:41], in_=RT[:, 37:41], func=AF.Exp, accum_out=RT[:, 41:42]), e_="act")
                rt(lambda e: e.reciprocal(out=RT[:, 42:43], in_=RT[:, 41:42]))
                rt(lambda e: e.tensor_scalar(out=RT[:, 84:88], in0=RT[:, 80:84], scalar1=-1.0, scalar2=1.0e30, op0=ALU.add, op1=ALU.mult))
                rt(lambda e: e.tensor_tensor(out=RT[:, 44:76].rearrange("p (g x) -> p g x", g=4), in0=RT[:, 4:36].rearrange("p (g x) -> p g x", g=4),
                                             in1=RT[:, 84:88].unsqueeze(2).to_broadcast([128, 4, 8]), op=ALU.add))
                rt(lambda e: e.tensor_reduce(out=RT[:, 76:77], in_=RT[:, 44:76], axis=AX.X, op=ALU.max))
                rt(lambda e: e.tensor_scalar(out=GD[:, i, :], in0=RT[:, 44:76], scalar1=RT[:, 76:77], scalar2=None, op0=ALU.is_ge), extra_r=[rGD])
                rt(lambda e: e.scalar_tensor_tensor(out=RT[:, 44:76], in0=GD[:, i, :], scalar=-1.0e30, in1=RT[:, 44:76], op0=ALU.mult, op1=ALU.add), extra_r=[rGD])
                rt(lambda e: e.tensor_reduce(out=RT[:, 77:78], in_=RT[:, 44:76], axis=AX.X, op=ALU.max))
                rt(lambda e: e.tensor_scalar(out=RT[:, 44:76], in0=RT[:, 44:76], scalar1=RT[:, 77:78], scalar2=None, op0=ALU.is_ge))
                rt(lambda e: e.tensor_tensor(out=RT[:, 78:79], in0=RT[:, 77:78], in1=RT[:, 76:77], op=ALU.subtract))
                rt(lambda e: e.activation(out=RT[:, 78:79], in_=RT[:, 78:79], func=AF.Exp), e_="act")
                rt(lambda e: e.tensor_scalar(out=RT[:, 78:79], in0=RT[:, 78:79], scalar1=1.0, scalar2=None, op0=ALU.add))
                rt(lambda e: e.reciprocal(out=RT[:, 78:79], in_=RT[:, 78:79]))
                rt(lambda e: e.tensor_scalar(out=RT[:, 79:80], in0=RT[:, 78:79], scalar1=-1.0, scalar2=1.0, op0=ALU.mult, op1=ALU.add))
                rt(lambda e: e.tensor_tensor(out=RT[:, 78:80], in0=RT[:, 78:80], in1=RT[:, 42:43].to_broadcast([128, 2]), op=ALU.mult))
                rt(lambda e: e.tensor_scalar(out=GD[:, i, :], in0=GD[:, i, :], scalar1=RT[:, 78:79], scalar2=None, op0=ALU.mult), extra_r=[rGD])
                kb.op("dve", lambda e: e.scalar_tensor_tensor(out=GD[:, i, :], in0=RT[:, 44:76], scalar=RT[:, 79:80], in1=GD[:, i, :], op0=ALU.mult, op1=ALU.add),
                      reads=[rRT], writes=[rGD, rRT])

        pb_p1(0)
        pb_ev(0)
        for i in range(1, NSLOT):
            pb_p1(i)
            pb_rc(i - 1)
            pb_ev(i)
        pb_rc(NSLOT - 1)

    if stop == "3b":
        return finish([(HR[:, 0, :], rHR[0], 128, 1024), (HR[:, 15, :], rHR[15], 128, 1024), (GD[:, 0, :], rGD, 128, 32), (GD[:, 15, :], rGD, 128, 32),
                       (H2T[:, 0, 0:1024], rH2T, 128, 1024)])
    kb.barrier()
    with Scope() as es:
        wgr = Ring(kb, "wg", 2, lambda n: SB(es, n, [128, 8, 512], BF16))
        wur = Ring(kb, "wu", 2, lambda n: SB(es, n, [128, 8, 512], BF16))
        wdr = Ring(kb, "wd", 2, lambda n: SB(es, n, [128, 4, D], BF16))
        sir = Ring(kb, "si", 2, lambda n: SB(es, n, [128, 512], F32))
        acr = Ring(kb, "ac", 2, lambda n: SB(es, n, [128, 4, 512], BF16))
        g2bc = SB(es, "g2bc", [128, D], F32)
        rg2 = Res("g2bc")
        gate_bc(g2bc, rg2, 5)
        wge_v = wge_d.ap().rearrange("e (k p) n -> e p k n", p=128)
        wue_v = wue_d.ap().rearrange("e (k p) n -> e p k n", p=128)
        wde_v = wde_d.ap().rearrange("e (k p) n -> e p k n", p=128)
        wts = {}
        acs = {}

        def load_expert(ex):
            Wg, rWg, sg_ = wgr.next()
            Wu, rWu, su_ = wur.next()
            Wd, rWd, sd_ = wdr.next()
            kb.dma("pool", Wg[:], wge_v[ex], sg_, writes=[rWg])
            kb.dma("pool", Wu[:], wue_v[ex], su_, writes=[rWu])
            kb.dma("pool", Wd[:], wde_v[ex], sd_, writes=[rWd])
            for kc in range(4):
                kb.op("dve", lambda e: e.tensor_tensor(out=Wd[:, kc, :], in0=Wd[:, kc, :], in1=g2bc[:], op=ALU.mult), reads=[rWd, rg2], writes=[rWd])
            wts[ex] = (Wg, rWg, Wu, rWu, Wd, rWd)

        def stage_gu(ex, g):
            if ex not in wts:
                load_expert(ex)
            Wg, rWg, Wu, rWu, Wd, rWd = wts[ex]
            ac, rac, _ = acr.next()
            acs[(ex, g)] = (ac, rac)
            for fc in range(4):
                bg = (fc % 2) * 2
                for kc in range(8):
                    kb.op("pe", lambda e: e.matmul(PB[bg][:], lhsT=Wg[:, kc, fc * 128:(fc + 1) * 128], rhs=H2T[:, kc, g * 512:(g + 1) * 512],
                                                   start=(kc == 0), stop=(kc == 7)), reads=[rWg, rH2T], writes=[rPB[bg]])
                for kc in range(8):
                    kb.op("pe", lambda e: e.matmul(PB[bg + 1][:], lhsT=Wu[:, kc, fc * 128:(fc + 1) * 128], rhs=H2T[:, kc, g * 512:(g + 1) * 512],
                                                   start=(kc == 0), stop=(kc == 7)), reads=[rWu, rH2T], writes=[rPB[bg + 1]])
                si, rsi, _ = sir.next()
                kb.op("act", lambda e: e.activation(out=si[:], in_=PB[bg][:], func=AF.Silu), reads=[rPB[bg]], writes=[rsi])
                kb.op("dve", lambda e: e.tensor_tensor(out=ac[:, fc, :], in0=si[:], in1=PB[bg + 1][:], op=ALU.mult), reads=[rPB[bg + 1], rsi], writes=[rac])

        def stage_down(ex, g):
            Wg, rWg, Wu, rWu, Wd, rWd = wts[ex]
            ac, rac = acs.pop((ex, g))
            for tt in range(4):
                sl_i = g * 4 + tt
                for n in range(2):
                    bd = 4 + (tt % 2) * 2 + n
                    for kc in range(4):
                        kb.op("pe", lambda e: e.matmul(PB[bd][:], lhsT=ac[:, kc, tt * 128:(tt + 1) * 128], rhs=Wd[:, kc, n * 512:(n + 1) * 512],
                                                       start=(kc == 0), stop=(kc == 3)), reads=[rWd, rac], writes=[rPB[bd]])
                    kb.op("dve", lambda e: e.scalar_tensor_tensor(out=HR[:, sl_i, n * 512:(n + 1) * 512], in0=PB[bd][:], scalar=GD[:, sl_i, ex:ex + 1],
                                                                  in1=HR[:, sl_i, n * 512:(n + 1) * 512], op0=ALU.mult, op1=ALU.add),
                          reads=[rPB[bd], rGD], writes=[rHR[sl_i]])
            if g == 3:
                wts.pop(ex)

        items = [(ex, g) for ex in range(32) for g in range(4)]
        stage_gu(*items[0])
        for k_ in range(len(items)):
            if k_ + 1 < len(items):
                stage_gu(*items[k_ + 1])
            stage_down(*items[k_])

        gfb = SB(es, "gfb", [128, D], F32)
        rgf = Res("gf")
        kb.dma("sp", gfb[:], gf_d.ap().partition_broadcast(128), cs, writes=[rgf])
        outr = Ring(kb, "yo", 2, lambda n: SB(es, n, [128, D], F32))
        STF = SB(es, "STF", [128, 4], F32)
        rSTF = Res("STF")
        osl = kb.slot("dq_out")
        outs = []
        for i in range(NSLOT):
            yo, ryo, _ = outr.next()
            kb.op("act", lambda e: e.activation(out=yo[:], in_=HR[:, i, :], func=AF.Square, accum_out=STF[:, 0:1]), reads=[rHR[i]], writes=[rSTF, ryo])
            kb.op("act", lambda e: e.activation(out=STF[:, 1:2], in_=STF[:, 0:1], func=AF.Ln, scale=1.0 / D, bias=epsb[:, 0:1]), reads=[rSTF, rC], writes=[rSTF])
            kb.op("act", lambda e: e.activation(out=STF[:, 2:3], in_=STF[:, 1:2], func=AF.Exp, scale=-0.5), reads=[rSTF], writes=[rSTF])
            kb.op("dve", lambda e: e.scalar_tensor_tensor(out=yo[:], in0=HR[:, i, :], scalar=STF[:, 2:3], in1=gfb[:], op0=ALU.mult, op1=ALU.mult),
                  reads=[rHR[i], rSTF, rgf], writes=[ryo])
            kb.dma("sp", y_d.ap()[i * 128:(i + 1) * 128, :], yo[:], osl, reads=[ryo])
            outs.append(ryo)
        kb.wait_all("sp", outs)
    es_moe.close()
    es_h2t.close()
    es_all.close()
    return nc


def _t5_bucket(n):
    n = np.maximum(n, 0)
    nf = np.maximum(n, 1).astype(np.float32)
    large = 16 + (np.log(nf / np.float32(16)) / np.float32(np.log(128 / 16)) * np.float32(16)).astype(np.int32)
    large = np.minimum(large, 31)
    return np.where(n < 16, n, large)


def _structural_tables(j):
    E = np.zeros((32, TABW), np.float32)
    for zi in range(5):
        z = zi - 1
        m = np.arange(255)
        n = (j - z) * 128 - 127 + m
        ok = n >= 0
        b = _t5_bucket(n)
        cols = zi * 256 + m
        E[b[ok], cols[ok]] += 1.0
        E[31, cols[ok]] -= 1.0
    return E


_NC_CACHE = {}


def kernel(x, c, w_ada, b_ada, norm1_g, w_in, rel_bias, conv_w, w_attn_branch, w_conv_branch, w_out, norm2_g,
           w_router_group, b_router_group, w_router_expert, b_router_expert, w_gate_e, w_up_e, w_down_e, norm_f_g):
    f = lambda a: np.ascontiguousarray(np.asarray(a, dtype=np.float32))
    x = f(x)
    c = f(c)
    shared = {
        "w_ada": f(w_ada)[0],
        "b_adaT": f(np.asarray(b_ada)[0].reshape(48, 128).T),
        "b_ada": f(b_ada)[0:1],
        "g1T": f(np.asarray(norm1_g)[0].reshape(8, 128).T),
        "g2T": f(np.asarray(norm2_g)[0].reshape(8, 128).T),
        "gf": f(norm_f_g).reshape(1, D),
        "w_in": f(w_in)[0],
        "relb": f(np.asarray(rel_bias)[:, [0, 2, 4, 6, 1, 3, 5, 7]]),
        "convw": f(np.asarray(conv_w)[0].reshape(3, 4, 128).transpose(2, 1, 0)),
        "w_ab": f(w_attn_branch)[0],
        "w_cbr": f(w_conv_branch)[0],
        "w_out": f(w_out)[0],
        "wr": f(np.concatenate([np.asarray(w_router_group)[0], np.asarray(w_router_expert)[0]], axis=1)),
        "br": f(np.concatenate([np.asarray(b_router_group)[0], np.asarray(b_router_expert)[0]])).reshape(1, 36),
        "wge": f(w_gate_e)[0],
        "wue": f(w_up_e)[0],
        "wde": f(w_down_e)[0],
    }
    in_maps = []
    for core in range(8):
        b, j = core // 4, core % 4
        xo = np.zeros((NSLOT, 130, D), np.float32)
        hm = np.ones((128, NSLOT), np.float32)
        for i in range(NSLOT):
            st = (4 * i + j) * 128
            if st == 0:
                xo[i, 2:] = x[b, 0:128]
                hm[:, i] = 0.0
            else:
                xo[i] = x[b, st - 2:st + 128]
        m = dict(shared)
        m["xb"] = x[b]
        m["xo"] = xo
        m["c8"] = f(c[b].reshape(8, 128).T)
        m["ej"] = _structural_tables(j)
        m["tq"] = (j * 128 + np.arange(128, dtype=np.float32)).reshape(128, 1)
        m["hmask"] = hm
        in_maps.append(m)
    if "nc" not in _NC_CACHE:
        _NC_CACHE["nc"] = build_nc()
    res = run_bass_kernel_spmd(_NC_CACHE["nc"], in_maps, core_ids=list(range(8)))
    out = np.empty((2, S, D), np.float32)
    for core in range(8):
        b, j = core // 4, core % 4
        y = res.results[core]["y"]
        for i in range(NSLOT):
            st = (4 * i + j) * 128
            out[b, st:st + 128] = y[i * 128:(i + 1) * 128]
    return out
```
